# Optimizing a Trainium2 kernel written in Bass

```python
import math
import jax
import jax.numpy as jnp
from jax import lax
import numpy as np

D_MODEL = 1024
BATCH = 8
SEQ = 4096
DEPTH = 2

GRID_W = 64
CTX_LEN = 256
SHORT_CONV = 3
NORM_EPS = 1e-6
N_BRANCH = 3
MIX_WIDTH = 512

HY_WIDTH = MIX_WIDTH
HY_ORDER = 2
HY_FILTER_DIM = 64
HY_BANDS = 16
HY_EMB = 2 * HY_BANDS + 1
HY_DECAY_TARGET = 1e-2
HY_FAST_PCT = 0.3
HY_SLOW_PCT = 1.5

RW_HEAD = 64
RW_HEADS = MIX_WIDTH // RW_HEAD
RW_WIDTH = RW_HEADS * RW_HEAD
RW_DECAY_RANK = 64
RW_ICLR_RANK = 64
RW_GATE_RANK = 128
RW_LN_EPS = 64e-5

S5_GROUP = 16
S5_GROUPS = MIX_WIDTH // S5_GROUP
S5_WIDTH = S5_GROUPS * S5_GROUP
S5_STATE = 64

N_EXPERTS = 32
TOP_K = 4
D_EXPERT = 1024
SWIGLU_LIMIT = 7.0
SWIGLU_ALPHA = 1.702
MOE_BLOCK = 128

HY_COLS = (HY_ORDER + 1) * HY_WIDTH
RW_COLS = 3 * RW_WIDTH + RW_DECAY_RANK + RW_ICLR_RANK + RW_GATE_RANK
S5_COLS = S5_WIDTH
GATE_COLS = N_BRANCH * D_MODEL
IN_COLS = HY_COLS + RW_COLS + S5_COLS + GATE_COLS
RW_SPLITS = (RW_WIDTH, 2 * RW_WIDTH, 3 * RW_WIDTH, 3 * RW_WIDTH + RW_DECAY_RANK,
             3 * RW_WIDTH + RW_DECAY_RANK + RW_ICLR_RANK)

kernel_name = 'hybrid_hyena_rwkv7_s5_moe_dit'


def rmsnorm(x, g):
    xf = x.astype(jnp.float32)
    y = xf * lax.rsqrt(jnp.mean(xf * xf, axis=-1, keepdims=True) + NORM_EPS)
    return (y * g.astype(jnp.float32)).astype(x.dtype)


def modulate(x, g, shift, scale):
    return rmsnorm(x, g) * (1 + scale) + shift


def short_conv(z, w, b):
    n = z.shape[1]
    half = SHORT_CONV // 2
    zp = jnp.pad(z, ((0, 0), (half, half), (0, 0)))
    return sum(zp[:, j:j + n] * w[j] for j in range(SHORT_CONV)) + b


def grid_pos_embed(n_tokens):
    rows = n_tokens // GRID_W
    row_id, col_id = jnp.meshgrid(jnp.arange(rows), jnp.arange(GRID_W), indexing='ij')
    quarter = D_MODEL // 4
    omega = 1.0 / (10000.0 ** (jnp.arange(quarter, dtype=jnp.float32) / quarter))

    def enc(p):
        ang = p.reshape(-1)[:, None].astype(jnp.float32) * omega
        return jnp.concatenate([jnp.sin(ang), jnp.cos(ang)], axis=-1)

    return jnp.concatenate([enc(row_id), enc(col_id)], axis=-1)


def hyena_filters(n, w1, b1, f1, w2, b2, f2, w3, b3):
    f32 = jnp.float32
    t = jnp.linspace(0.0, 1.0, n, dtype=f32)[:, None]
    bands = jnp.linspace(1e-4, HY_BANDS - 1, HY_BANDS, dtype=f32)
    ang = (2 * math.pi * jnp.arange(n, dtype=f32) / n)[:, None] * bands
    feats = jnp.concatenate([t, jnp.cos(ang), -jnp.sin(ang)], axis=-1)
    hf = jnp.sin(f1.astype(f32) * (feats @ w1.astype(f32) + b1.astype(f32)))
    hf = jnp.sin(f2.astype(f32) * (hf @ w2.astype(f32) + b2.astype(f32)))
    hf = (hf @ w3.astype(f32) + b3.astype(f32)).reshape(n, HY_ORDER, 2, HY_WIDTH)
    deltas = jnp.abs(jnp.linspace(math.log(HY_DECAY_TARGET) / HY_SLOW_PCT,
                                  math.log(HY_DECAY_TARGET) / HY_FAST_PCT, HY_WIDTH, dtype=f32))
    hf = hf * jnp.exp(-t * deltas)[:, None, None, :]
    fwd = hf[:, :, 0]
    bwd = hf[1:, :, 1][::-1]
    return jnp.concatenate([fwd, jnp.zeros((1, HY_ORDER, HY_WIDTH), f32), bwd], axis=0)


def fft_long_conv(z, filt, bias):
    n = z.shape[1]
    zf = z.astype(jnp.float32)
    spec = jnp.fft.rfft(zf, n=2 * n, axis=1) * jnp.fft.rfft(filt, n=2 * n, axis=0)[None]
    y = jnp.fft.irfft(spec, n=2 * n, axis=1)[:, :n]
    return (y + zf * bias.astype(jnp.float32)).astype(z.dtype)


def hyena_branch(z, conv_w, conv_b, w1, b1, f1, w2, b2, f2, w3, b3, bias):
    u = short_conv(z, conv_w, conv_b)
    x1, x2, v = jnp.split(u, 3, axis=-1)
    filt = hyena_filters(z.shape[1], w1, b1, f1, w2, b2, f2, w3, b3)
    y = x1 * fft_long_conv(v, filt[:, 0], bias[0])
    return x2 * fft_long_conv(y, filt[:, 1], bias[1])


def _heads(t):
    return t.reshape(t.shape[:-1] + (RW_HEADS, RW_HEAD))


def rwkv_prep(z, conv_w, conv_b, w_up, w0, a_up, a0, g_up, k_k, k_a):
    u = short_conv(z, conv_w, conv_b).astype(jnp.float32)
    r, k, v, xw, xa, xg = jnp.split(u, RW_SPLITS, axis=-1)
    g = jax.nn.sigmoid(xg) @ g_up.astype(jnp.float32)
    kk = _heads(k * k_k)
    kk = kk * lax.rsqrt(jnp.maximum(jnp.sum(kk * kk, axis=-1, keepdims=True), 1e-24))
    w = -jax.nn.softplus(-(w0[:, None, None, :] + jnp.einsum('blr,drc->dblc', jnp.tanh(xw), w_up))) - 0.5
    decay = _heads(jnp.exp(-jnp.exp(w)))
    a = _heads(jax.nn.sigmoid(a0[:, None, None, :] + jnp.einsum('blr,drc->dblc', xa, a_up)))
    k_dir = _heads(k) * (1 + (a - 1) * _heads(k_a))
    return _heads(r), _heads(v), g, kk, decay, a, k_dir


def wkv_scan(r, decay, k, v, kk, a, s0, reverse, with_output):
    xs = tuple(jnp.moveaxis(t, 1, 0) for t in (r, decay, k, v, -kk, kk * a))

    def step(s, inp):
        r_t, w_t, k_t, v_t, a_t, b_t = inp
        sa = jnp.einsum('bhvk,bhk->bhv', s, a_t)
        s = s * w_t[:, :, None, :] + sa[..., None] * b_t[:, :, None, :] + v_t[..., None] * k_t[:, :, None, :]
        y = jnp.einsum('bhvk,bhk->bhv', s, r_t) if with_output else None
        return s, y

    s, ys = lax.scan(step, s0, xs, reverse=reverse)
    return s, (jnp.moveaxis(ys, 0, 1) if with_output else None)


def rwkv_readout(r, v, g, k_dir, y_dirs, r_k, ln_g, ln_b, dtype):
    bonus = jnp.sum(r * k_dir * r_k, axis=(0, -1))[..., None] * v
    y = y_dirs[0] + y_dirs[1] + bonus
    mu = jnp.mean(y, axis=-1, keepdims=True)
    var = jnp.mean(jnp.square(y - mu), axis=-1, keepdims=True)
    y = ((y - mu) * lax.rsqrt(var + RW_LN_EPS)).reshape(y.shape[:2] + (RW_WIDTH,))
    return ((y * ln_g + ln_b) * g).astype(dtype)


def rwkv_branch(z, zc, need_ctx, conv_w, conv_b, w_up, w0, a_up, a0, g_up, k_k, k_a, r_k, ln_g, ln_b):
    r, v, g, kk, decay, a, k_dir = rwkv_prep(z, conv_w, conv_b, w_up, w0, a_up, a0, g_up, k_k, k_a)
    rc, vc, gc, kkc, decayc, ac, k_dirc = rwkv_prep(zc, conv_w, conv_b, w_up, w0, a_up, a0, g_up, k_k, k_a)
    s0 = jnp.zeros((z.shape[0], RW_HEADS, RW_HEAD, RW_HEAD), jnp.float32)
    y_lat, y_ctx = [], []
    for d, rev in enumerate((False, True)):
        s_ctx, yc_d = wkv_scan(rc, decayc[d], k_dirc[d], vc, kkc, ac[d], s0, rev, need_ctx)
        _, yl_d = wkv_scan(r, decay[d], k_dir[d], v, kk, a[d], s_ctx, rev, True)
        y_lat.append(yl_d)
        y_ctx.append(yc_d)
    out = rwkv_readout(r, v, g, k_dir, y_lat, r_k, ln_g, ln_b, z.dtype)
    out_c = rwkv_readout(rc, vc, gc, k_dirc, y_ctx, r_k, ln_g, ln_b, zc.dtype) if need_ctx else None
    return out, out_c


def s5_discretise(a_re, a_im, log_dt, b_re, b_im):
    f32 = jnp.float32
    a_re, a_im, b_re, b_im = (t.astype(f32) for t in (a_re, a_im, b_re, b_im))
    dt = jnp.exp(log_dt.astype(f32))[:, None]
    mag = jnp.exp(a_re * dt)
    lb_re, lb_im = mag * jnp.cos(a_im * dt), mag * jnp.sin(a_im * dt)
    den = a_re * a_re + a_im * a_im
    nr = lb_re - 1.0
    cr = (nr * a_re + lb_im * a_im) / den
    ci = (lb_im * a_re - nr * a_im) / den
    bb_re = cr[..., None] * b_re - ci[..., None] * b_im
    bb_im = cr[..., None] * b_im + ci[..., None] * b_re
    return lb_re, lb_im, bb_re, bb_im


def _linear_recurrence_combine(e_i, e_j):
    ar_i, ai_i, br_i, bi_i = e_i
    ar_j, ai_j, br_j, bi_j = e_j
    return (ar_j * ar_i - ai_j * ai_i,
            ar_j * ai_i + ai_j * ar_i,
            ar_j * br_i - ai_j * bi_i + br_j,
            ar_j * bi_i + ai_j * br_i + bi_j)


def s5_scan(u, lb_re, lb_im, bb_re, bb_im, s0_re, s0_im, reverse):
    n = u.shape[1]
    bu_re = jnp.einsum('blgh,gph->blgp', u, bb_re)
    bu_im = jnp.einsum('blgh,gph->blgp', u, bb_im)
    edge = n - 1 if reverse else 0
    bu_re = bu_re.at[:, edge].add(lb_re * s0_re - lb_im * s0_im)
    bu_im = bu_im.at[:, edge].add(lb_re * s0_im + lb_im * s0_re)
    shape = (1, n) + lb_re.shape
    elems = (jnp.broadcast_to(lb_re, shape), jnp.broadcast_to(lb_im, shape), bu_re, bu_im)
    _, _, h_re, h_im = lax.associative_scan(_linear_recurrence_combine, elems, reverse=reverse, axis=1)
    return h_re, h_im


def s5_readout(h_re, h_im, u_g, c_re, c_im, d_skip, glu_w, glu_b, dtype):
    f32 = jnp.float32
    y = (jnp.einsum('blgp,ghp->blgh', h_re, c_re.astype(f32))
         - jnp.einsum('blgp,ghp->blgh', h_im, c_im.astype(f32))
         + u_g * d_skip.astype(f32).reshape(S5_GROUPS, S5_GROUP))
    y = jax.nn.gelu(y.reshape(y.shape[:2] + (S5_WIDTH,)))
    lin, gate = jnp.split(y @ glu_w.astype(f32) + glu_b.astype(f32), 2, axis=-1)
    return (lin * jax.nn.sigmoid(gate)).astype(dtype)


def s5_branch(u, uc, need_ctx, a_re, a_im, log_dt, b_re, b_im, c_re, c_im, d_skip, glu_w, glu_b):
    def groups(t):
        return t.astype(jnp.float32).reshape(t.shape[:2] + (S5_GROUPS, S5_GROUP))

    ul, ucg = groups(u), groups(uc)
    s0 = jnp.zeros((u.shape[0], S5_GROUPS, S5_STATE), jnp.float32)
    lat_re, lat_im, ctx_re, ctx_im = [], [], [], []
    for d, rev in enumerate((False, True)):
        lb_re, lb_im, bb_re, bb_im = s5_discretise(a_re[d], a_im[d], log_dt[d], b_re, b_im)
        hc_re, hc_im = s5_scan(ucg, lb_re, lb_im, bb_re, bb_im, s0, s0, rev)
        edge = 0 if rev else -1
        hl_re, hl_im = s5_scan(ul, lb_re, lb_im, bb_re, bb_im, hc_re[:, edge], hc_im[:, edge], rev)
        lat_re.append(hl_re)
        lat_im.append(hl_im)
        ctx_re.append(hc_re)
        ctx_im.append(hc_im)
    out = s5_readout(lat_re[0] + lat_re[1], lat_im[0] + lat_im[1], ul, c_re, c_im, d_skip, glu_w, glu_b, u.dtype)
    if not need_ctx:
        return out, None
    out_c = s5_readout(ctx_re[0] + ctx_re[1], ctx_im[0] + ctx_im[1], ucg, c_re, c_im, d_skip, glu_w, glu_b, uc.dtype)
    return out, out_c


def merge_branches(zg, y_hy, y_rw, y_s5, w_branch, w_out):
    g_hy, g_rw, g_s5 = jnp.split(jax.nn.sigmoid(zg), N_BRANCH, axis=-1)
    m = g_hy * (y_hy @ w_branch[0]) + g_rw * (y_rw @ w_branch[1]) + g_s5 * (y_s5 @ w_branch[2])
    return m @ w_out


def token_mixer(h, hc, need_ctx, w_in, hy_conv_w, hy_conv_b, hy_w1, hy_b1, hy_f1, hy_w2, hy_b2, hy_f2,
                hy_w3, hy_b3, hy_bias, rw_conv_w, rw_conv_b, rw_w_up, rw_w0, rw_a_up, rw_a0, rw_g_up,
                rw_k_k, rw_k_a, rw_r_k, rw_ln_g, rw_ln_b, s5_a_re, s5_a_im, s5_log_dt, s5_b_re, s5_b_im,
                s5_c_re, s5_c_im, s5_d, s5_glu_w, s5_glu_b, w_branch, w_out):
    bounds = (HY_COLS, HY_COLS + RW_COLS, HY_COLS + RW_COLS + S5_COLS)
    z_hy, z_rw, z_s5, z_gate = jnp.split(h @ w_in, bounds, axis=-1)
    zc_hy, zc_rw, zc_s5, zc_gate = jnp.split(hc @ w_in, bounds, axis=-1)
    hyena_args = (hy_conv_w, hy_conv_b, hy_w1, hy_b1, hy_f1, hy_w2, hy_b2, hy_f2, hy_w3, hy_b3, hy_bias)
    y_hy = hyena_branch(z_hy, *hyena_args)
    y_rw, yc_rw = rwkv_branch(z_rw, zc_rw, need_ctx, rw_conv_w, rw_conv_b, rw_w_up, rw_w0, rw_a_up, rw_a0,
                              rw_g_up, rw_k_k, rw_k_a, rw_r_k, rw_ln_g, rw_ln_b)
    y_s5, yc_s5 = s5_branch(z_s5, zc_s5, need_ctx, s5_a_re, s5_a_im, s5_log_dt, s5_b_re, s5_b_im,
                            s5_c_re, s5_c_im, s5_d, s5_glu_w, s5_glu_b)
    y = merge_branches(z_gate, y_hy, y_rw, y_s5, w_branch, w_out)
    if not need_ctx:
        return y, None
    yc = merge_branches(zc_gate, hyena_branch(zc_hy, *hyena_args), yc_rw, yc_s5, w_branch, w_out)
    return y, yc


def moe_ffn(h, router_w, router_b, w1, b1, w2, b2):
    bsz, n, dm = h.shape
    xt = h.reshape(-1, dm)
    n_tok = xt.shape[0]
    n_asg = n_tok * TOP_K
    logits = (xt @ router_w + router_b).astype(jnp.float32)
    top_val, top_idx = lax.top_k(logits, TOP_K)
    gate = jax.nn.softmax(top_val, axis=-1).astype(h.dtype)
    flat_e = top_idx.reshape(-1)
    flat_tok = jnp.repeat(jnp.arange(n_tok, dtype=jnp.int32), TOP_K)
    flat_w = gate.reshape(-1)
    order = jnp.argsort(flat_e)
    sorted_e = flat_e[order]
    counts = jnp.bincount(flat_e, length=N_EXPERTS)
    padded = (counts + MOE_BLOCK - 1) // MOE_BLOCK * MOE_BLOCK
    pad_end = jnp.cumsum(padded)
    pad_start = pad_end - padded
    grp_start = jnp.cumsum(counts) - counts
    dest = pad_start[sorted_e] + jnp.arange(n_asg, dtype=jnp.int32) - grp_start[sorted_e]
    n_blocks = -(-n_asg // MOE_BLOCK) + N_EXPERTS
    n_slots = n_blocks * MOE_BLOCK
    slot_tok = jnp.zeros((n_slots,), jnp.int32).at[dest].set(flat_tok[order])
    slot_w = jnp.zeros((n_slots,), h.dtype).at[dest].set(flat_w[order])
    blk_e = jnp.minimum(jnp.searchsorted(pad_end, jnp.arange(n_blocks) * MOE_BLOCK, side='right'), N_EXPERTS - 1)
    xs = xt[slot_tok].reshape(n_blocks, MOE_BLOCK, dm)

    def expert_block(args):
        xb, e = args
        gl, up = jnp.split(xb @ w1[e] + b1[e], 2, axis=-1)
        gl = jnp.minimum(gl, SWIGLU_LIMIT)
        up = jnp.clip(up, -SWIGLU_LIMIT, SWIGLU_LIMIT)
        act = (up + 1) * gl * jax.nn.sigmoid(SWIGLU_ALPHA * gl)
        return act @ w2[e] + b2[e]

    ys = lax.map(expert_block, (xs, blk_e)).reshape(n_slots, dm)
    out = jax.ops.segment_sum(ys * slot_w[:, None], slot_tok, num_segments=n_tok)
    return out.reshape(bsz, n, dm)


def setup_inputs(seed: int = 0) -> dict:
    key = jax.random.key(seed)
    ks = iter(jax.random.split(key, 64))
    f32 = jnp.float32

    def nrm(shape, scale):
        return jax.random.normal(next(ks), shape, f32) * scale

    def uni(shape, lo, hi):
        return jax.random.uniform(next(ks), shape, f32, minval=lo, maxval=hi)

    L, D = DEPTH, D_MODEL
    G, P, H = S5_GROUPS, S5_STATE, S5_GROUP
    return {
        'x': nrm((BATCH, SEQ, D), 1.0),
        'c': nrm((BATCH, D), 1.0),
        'ctx': nrm((BATCH, CTX_LEN, D), 1.0),
        'c_ctx': nrm((D,), 1.0),
        'ada_w': nrm((L, D, 6 * D), D ** -0.5),
        'ada_b': nrm((L, 6 * D), 0.02),
        'norm1_g': 1.0 + nrm((L, D), 0.02),
        'norm2_g': 1.0 + nrm((L, D), 0.02),
        'w_in': nrm((L, D, IN_COLS), D ** -0.5),
        'hy_conv_w': nrm((L, SHORT_CONV, HY_COLS), SHORT_CONV ** -0.5),
        'hy_conv_b': nrm((L, HY_COLS), 0.02),
        'hy_w1': nrm((L, HY_EMB, HY_FILTER_DIM), HY_EMB ** -0.5),
        'hy_b1': nrm((L, HY_FILTER_DIM), 0.02),
        'hy_f1': 1.0 + nrm((L, HY_FILTER_DIM), 0.02),
        'hy_w2': nrm((L, HY_FILTER_DIM, HY_FILTER_DIM), HY_FILTER_DIM ** -0.5),
        'hy_b2': nrm((L, HY_FILTER_DIM), 0.02),
        'hy_f2': 1.0 + nrm((L, HY_FILTER_DIM), 0.02),
        'hy_w3': nrm((L, HY_FILTER_DIM, HY_ORDER * 2 * HY_WIDTH), 0.005),
        'hy_b3': nrm((L, HY_ORDER * 2 * HY_WIDTH), 0.002),
        'hy_bias': nrm((L, HY_ORDER, HY_WIDTH), 0.1),
        'rw_conv_w': nrm((L, SHORT_CONV, RW_COLS), SHORT_CONV ** -0.5),
        'rw_conv_b': nrm((L, RW_COLS), 0.02),
        'rw_w_up': nrm((L, 2, RW_DECAY_RANK, RW_WIDTH), 0.5 * RW_DECAY_RANK ** -0.5),
        'rw_w0': uni((L, 2, RW_WIDTH), -6.0, 0.0),
        'rw_a_up': nrm((L, 2, RW_ICLR_RANK, RW_WIDTH), 0.5 * RW_ICLR_RANK ** -0.5),
        'rw_a0': nrm((L, 2, RW_WIDTH), 0.1),
        'rw_g_up': nrm((L, RW_GATE_RANK, RW_WIDTH), RW_GATE_RANK ** -0.5),
        'rw_k_k': 0.85 + nrm((L, RW_WIDTH), 0.02),
        'rw_k_a': 1.0 + nrm((L, RW_WIDTH), 0.02),
        'rw_r_k': nrm((L, RW_HEADS, RW_HEAD), 0.1),
        'rw_ln_g': 1.0 + nrm((L, RW_WIDTH), 0.02),
        'rw_ln_b': nrm((L, RW_WIDTH), 0.02),
        's5_a_re': -0.5 + nrm((L, 2, G, P), 0.01),
        's5_a_im': math.pi * jnp.arange(P, dtype=f32) + nrm((L, 2, G, P), 0.01),
        's5_log_dt': uni((L, 2, G), math.log(1e-3), math.log(1e-1)),
        's5_b_re': nrm((L, G, P, H), H ** -0.5),
        's5_b_im': nrm((L, G, P, H), H ** -0.5),
        's5_c_re': nrm((L, G, H, P), P ** -0.5),
        's5_c_im': nrm((L, G, H, P), P ** -0.5),
        's5_d': nrm((L, S5_WIDTH), 0.5),
        's5_glu_w': nrm((L, S5_WIDTH, 2 * S5_WIDTH), S5_WIDTH ** -0.5),
        's5_glu_b': nrm((L, 2 * S5_WIDTH), 0.02),
        'w_branch': nrm((L, N_BRANCH, MIX_WIDTH, D), MIX_WIDTH ** -0.5),
        'w_out': nrm((L, D, D), D ** -0.5),
        'router_w': nrm((L, D, N_EXPERTS), D ** -0.5),
        'router_b': nrm((L, N_EXPERTS), 0.01),
        'moe_w1': nrm((L, N_EXPERTS, D, 2 * D_EXPERT), D ** -0.5),
        'moe_b1': nrm((L, N_EXPERTS, 2 * D_EXPERT), 0.02),
        'moe_w2': nrm((L, N_EXPERTS, D_EXPERT, D), D_EXPERT ** -0.5),
        'moe_b2': nrm((L, N_EXPERTS, D), 0.02),
        'final_g': 1.0 + nrm((D,), 0.02),
    }


def reference(x, c, ctx, c_ctx, ada_w, ada_b, norm1_g, norm2_g, w_in, hy_conv_w, hy_conv_b, hy_w1, hy_b1,
              hy_f1, hy_w2, hy_b2, hy_f2, hy_w3, hy_b3, hy_bias, rw_conv_w, rw_conv_b, rw_w_up, rw_w0,
              rw_a_up, rw_a0, rw_g_up, rw_k_k, rw_k_a, rw_r_k, rw_ln_g, rw_ln_b, s5_a_re, s5_a_im,
              s5_log_dt, s5_b_re, s5_b_im, s5_c_re, s5_c_im, s5_d, s5_glu_w, s5_glu_b, w_branch, w_out,
              router_w, router_b, moe_w1, moe_b1, moe_w2, moe_b2, final_g):
    x = x + grid_pos_embed(x.shape[1]).astype(x.dtype)
    xc = ctx
    cond = jax.nn.silu(c)[:, None, :]
    cond_ctx = jax.nn.silu(c_ctx)
    for l in range(DEPTH):
        need_ctx = l < DEPTH - 1
        sh1, sc1, g1, sh2, sc2, g2 = jnp.split(cond @ ada_w[l] + ada_b[l], 6, axis=-1)
        csh1, csc1, cg1, csh2, csc2, cg2 = jnp.split(cond_ctx @ ada_w[l] + ada_b[l], 6, axis=-1)
        y, yc = token_mixer(
            modulate(x, norm1_g[l], sh1, sc1), modulate(xc, norm1_g[l], csh1, csc1), need_ctx,
            w_in[l], hy_conv_w[l], hy_conv_b[l], hy_w1[l], hy_b1[l], hy_f1[l], hy_w2[l], hy_b2[l], hy_f2[l],
            hy_w3[l], hy_b3[l], hy_bias[l], rw_conv_w[l], rw_conv_b[l], rw_w_up[l], rw_w0[l], rw_a_up[l],
            rw_a0[l], rw_g_up[l], rw_k_k[l], rw_k_a[l], rw_r_k[l], rw_ln_g[l], rw_ln_b[l], s5_a_re[l],
            s5_a_im[l], s5_log_dt[l], s5_b_re[l], s5_b_im[l], s5_c_re[l], s5_c_im[l], s5_d[l], s5_glu_w[l],
            s5_glu_b[l], w_branch[l], w_out[l])
        x = x + g1 * y
        x = x + g2 * moe_ffn(modulate(x, norm2_g[l], sh2, sc2), router_w[l], router_b[l],
                             moe_w1[l], moe_b1[l], moe_w2[l], moe_b2[l])
        if need_ctx:
            xc = xc + cg1 * yc
            xc = xc + cg2 * moe_ffn(modulate(xc, norm2_g[l], csh2, csc2), router_w[l], router_b[l],
                                    moe_w1[l], moe_b1[l], moe_w2[l], moe_b2[l])
    return rmsnorm(x, final_g)
```

```python
import math
from contextlib import ExitStack
import numpy as np
import ml_dtypes
import concourse.bass as bass
import concourse.mybir as mybir
from concourse.bass_utils import run_bass_kernel_spmd

F32 = mybir.dt.float32
BF16 = mybir.dt.bfloat16
I32 = mybir.dt.int32
AF = mybir.ActivationFunctionType
ALU = mybir.AluOpType
AX = mybir.AxisListType

L = 2
D = 1024
TC = 256
TL = 4096
T = TC + TL
IN_COLS = 6912
NE = 32
CHUNKS = [(0, 256)] + [(256 + 512 * i, 512) for i in range(8)]
ENGS = ("pe", "dve", "act", "pool", "sp")
NDMA_SEM = 8


class Prog:
    CHECK_DMA = False
    DBG = 0
    dma_multi = []

    def __init__(self, nc):
        self.nc = nc
        self.ops = {e: [] for e in ENGS}
        self.cnt = {}
        self.seen = {e: {} for e in ENGS}
        self.lw = {}
        self.lr = {}
        self.dma_rr = {e: 0 for e in ENGS}
        self.flushed = {}
        self.act_scratch = None
        self.sem_names = []
        for e in ENGS:
            self._mk(f"c_{e}")
            for i in range(NDMA_SEM):
                self._mk(f"d_{e}{i}")

    def _mk(self, name):
        self.cnt[name] = 0
        self.sem_names.append(name)

    def _deps(self, eng, reads, writes):
        need = {}

        def add(tok):
            if tok is None:
                return
            s, v = tok
            if need.get(s, 0) < v:
                need[s] = v
        for k in reads:
            add(self.lw.get(k))
        for k in writes:
            add(self.lw.get(k))
            for t in self.lr.get(k, ()):
                add(t)
        waits = []
        seen = self.seen[eng]
        for s, v in need.items():
            if s == "c_pe" and eng == "pe":
                continue
            if seen.get(s, 0) >= v:
                continue
            seen[s] = v
            waits.append((s, v))
        return waits

    def _commit(self, tok, reads, writes):
        for k in writes:
            self.lw[k] = tok
            self.lr[k] = []
        for k in reads:
            lst = self.lr.setdefault(k, [])
            lst.append(tok)
            if len(lst) > 12:
                m = {}
                for s, v in lst:
                    m[s] = max(m.get(s, 0), v)
                self.lr[k] = list(m.items())

    def op(self, eng, fn, reads=(), writes=()):
        waits = self._deps(eng, reads, writes)
        s = f"c_{eng}"
        self.cnt[s] += 1
        tok = (s, self.cnt[s])
        self.ops[eng].append((waits, fn, s, 1))
        self._commit(tok, reads, writes)
        return tok

    def _flush_producers(self, reads):
        for k in reads:
            tok = self.lw.get(k)
            if tok is None or not tok[0].startswith("c_") or tok[0] == "c_pe":
                continue
            pe_ = tok[0][2:]
            if self.flushed.get(pe_, 0) >= tok[1]:
                ftok = (tok[0], self.flushed[pe_])
            else:
                s = tok[0]
                self.cnt[s] += 1
                ftok = (s, self.cnt[s])
                self.ops[pe_].append(([], lambda e: e.drain(), s, 1))
                self.flushed[pe_] = self.cnt[s]
            self.lw[k] = ftok

    def dma(self, eng, fn, reads=(), writes=()):
        if eng == "act":
            eng = "sp"
        self._flush_producers(reads)
        waits = self._deps(eng, reads, writes)
        i = self.dma_rr[eng]
        self.dma_rr[eng] = (i + 1) % NDMA_SEM
        s = f"d_{eng}{i}"
        if self.cnt[s] > 0 and self.seen[eng].get(s, 0) < self.cnt[s]:
            self.seen[eng][s] = self.cnt[s]
            waits = waits + [(s, self.cnt[s])]
        self.cnt[s] += 16
        tok = (s, self.cnt[s])
        self.ops[eng].append((waits, fn, s, 16))
        self._commit(tok, reads, writes)
        return tok

    def barrier(self):
        for e in ENGS:
            waits = []
            for s, v in self.cnt.items():
                if v == 0 or self.seen[e].get(s, 0) >= v:
                    continue
                if s == f"c_{e}" and e == "pe":
                    pass
                self.seen[e][s] = v
                waits.append((s, v))
            self.ops[e].append((waits, None, None, 0))
        self.lw = {}
        self.lr = {}

    def emit(self):
        nc = self.nc
        with ExitStack() as es:
            sems = {n: es.enter_context(nc.semaphore(n)) for n in self.sem_names}
            block = es.enter_context(nc.Block())
            handles = {"pe": block.tensor, "dve": block.vector, "act": block.scalar,
                       "pool": block.gpsimd, "sp": block.sync}
            for e in ENGS:
                ops = self.ops[e]
                if not ops:
                    continue

                def body(eh, ops=ops, e=e):
                    if e == "act" and self.act_scratch is not None:
                        eh = _ActProxy(eh, self.act_scratch)
                    for waits, fn, s, inc in ops:
                        for (ws, wv) in waits:
                            eh.wait_ge(sems[ws], wv)
                        if fn is not None:
                            if inc == 16 and Prog.CHECK_DMA:
                                n_before = nc.n_instructions()
                                ins = fn(eh)
                                dn = nc.n_instructions() - n_before
                                if dn != 1:
                                    Prog.dma_multi.append((dn, str(ins)[:200]))
                                ins.then_inc(sems[s], inc)
                            else:
                                fn(eh).then_inc(sems[s], inc)
                handles[e](body)


class _ActProxy:
    def __init__(self, eh, scr):
        self._eh = eh
        self._scr = scr
        self._last = None

    def activation(self, *a, **kw):
        f = kw.get("func")
        if f != self._last:
            self._last = f
            for _ in range(2):
                self._eh.activation(out=self._scr[:, 0:64], in_=self._scr[:, 64:128], func=f)
        return self._eh.activation(*a, **kw)

    def __getattr__(self, n):
        return getattr(self._eh, n)


class Ctx:
    def __init__(self, nc, feed=(), dump=()):
        self.nc = nc
        self.P = Prog(nc)
        self.feed = set(feed)
        self.dump = set(dump)
        self.drams = {}
        self.ext_in = set()
        self.ext_out = set()

    def dram(self, name, shape, dtype=F32, kind=None):
        if name in self.drams:
            return self.drams[name]
        if kind is None:
            kind = "ExternalInput" if name in self.feed else ("ExternalOutput" if name in self.dump else "Internal")
        t = self.nc.dram_tensor(name, list(shape), dtype, kind=kind).ap()
        self.drams[name] = t
        if kind == "ExternalInput":
            self.ext_in.add(name)
        elif kind == "ExternalOutput":
            self.ext_out.add(name)
        return t

    def inp(self, name, shape, dtype=F32):
        return self.dram(name, shape, dtype, kind="ExternalInput")


_SBT_UID = [0]


def sbt(es, nc, name, shape, dtype=F32):
    _SBT_UID[0] += 1
    return es.enter_context(nc.sbuf_tensor(f"{name}_u{_SBT_UID[0]}", list(shape), dtype))


def stage_consts(C, es):
    nc, P = C.nc, C.P
    K = {}
    K["ones_bf"] = sbt(es, nc, "ones_bf", [128, 128], BF16)
    K["ident"] = sbt(es, nc, "ident", [128, 128], F32)
    K["sel"] = sbt(es, nc, "sel", [32, NE * 128], F32)
    K["cond"] = sbt(es, nc, "cond", [128, 8, 2], F32)
    P.op("pool", lambda e: e.memset(K["ones_bf"][:], 1.0), writes=["ones_bf"])
    identd = C.inp("ident_in", [128, 128])
    seld = C.inp("sel_in", [32, NE * 128])
    ccT = C.inp("ccT", [128, 16])
    P.dma("sp", lambda e: e.dma_start(out=K["ident"][:], in_=identd[:, :]), writes=["ident"])
    P.dma("sp", lambda e: e.dma_start(out=K["sel"][:], in_=seld[:, :]), writes=["sel"])
    P.dma("sp", lambda e: e.dma_start(out=K["cond"][:].rearrange("p j s -> p (j s)"), in_=ccT[:, :]), writes=["cond"])
    P.op("act", lambda e: e.activation(out=K["cond"][:], in_=K["cond"][:], func=AF.Silu), reads=["cond"], writes=["cond"])
    for l in range(L):
        K[f"ada{l}"] = sbt(es, nc, f"ada{l}", [128, 48, 2], F32)
        K[f"gs1_{l}"] = sbt(es, nc, f"gs1_{l}", [128, 8, 2], F32)
        K[f"gs2_{l}"] = sbt(es, nc, f"gs2_{l}", [128, 8, 2], F32)
    K["fing"] = sbt(es, nc, "fing", [128, 8], F32)
    fg = C.inp("final_g_t", [128, 8])
    P.dma("sp", lambda e: e.dma_start(out=K["fing"][:], in_=fg[:, :]), writes=["fing"])
    return K


def stage_ada(C, K, PS, l):
    nc, P = C.nc, C.P
    ada_w = C.inp("ada_w", [L, D, 6 * D])
    ada_b = C.inp("ada_b_t", [L, 128, 48])
    n1g = C.inp("norm1_g_t", [L, 128, 8])
    n2g = C.inp("norm2_g_t", [L, 128, 8])
    with ExitStack() as es:
        aw = [sbt(es, nc, f"aw{i}_{l}", [128, 8, 128], F32) for i in range(3)]
        bt = sbt(es, nc, f"adab_{l}", [128, 48], F32)
        g1t = sbt(es, nc, f"n1g_{l}", [128, 8], F32)
        g2t = sbt(es, nc, f"n2g_{l}", [128, 8], F32)
        tmp = sbt(es, nc, f"adatmp_{l}", [128, 8, 2], F32)
        P.dma("sp", lambda e: e.dma_start(out=bt[:], in_=ada_b[l]), writes=["adab"])
        P.dma("sp", lambda e: e.dma_start(out=g1t[:], in_=n1g[l]), writes=["n1g"])
        P.dma("sp", lambda e: e.dma_start(out=g2t[:], in_=n2g[l]), writes=["n2g"])
        pa = PS[0]
        wv = ada_w[l].rearrange("(k p) c -> p k c", p=128)
        for j in range(48):
            a = aw[j % 3]
            key = f"aw{j % 3}"
            P.dma("sp" if j % 2 == 0 else "act", lambda e, a=a, j=j: e.dma_start(out=a[:], in_=wv[:, :, j * 128:(j + 1) * 128]), writes=[key])
            for k in range(8):
                P.op("pe", lambda e, a=a, j=j, k=k: e.matmul(pa[:, 2 * j:2 * j + 2], lhsT=a[:, k, :], rhs=K["cond"][:, k, :],
                                                              start=(k == 0), stop=(k == 7)), reads=[key, "cond"], writes=["ps0"])
        ada = K[f"ada{l}"]
        P.op("dve", lambda e: e.tensor_tensor(out=ada[:], in0=pa[:, 0:96].rearrange("p (j s) -> p j s", s=2),
                                               in1=bt[:, :].unsqueeze(2).broadcast_to([128, 48, 2]), op=ALU.add),
             reads=["ps0", "adab"], writes=[f"ada{l}"])
        for (gs, sc0, gt, gk) in ((K[f"gs1_{l}"], 8, g1t, "n1g"), (K[f"gs2_{l}"], 32, g2t, "n2g")):
            P.op("dve", lambda e, sc0=sc0: e.tensor_scalar(out=tmp[:], in0=ada[:, sc0:sc0 + 8, :], scalar1=1.0, scalar2=None, op0=ALU.add),
                 reads=[f"ada{l}"], writes=["adatmp"])
            P.op("dve", lambda e, gs=gs, gt=gt: e.tensor_tensor(out=gs[:], in0=tmp[:], in1=gt[:, :].unsqueeze(2).broadcast_to([128, 8, 2]), op=ALU.mult),
                 reads=["adatmp", gk], writes=[f"gs{l}"])
        P.barrier()


def stage_resid_init(C, K, PS):
    nc, P = C.nc, C.P
    xT = C.inp("xT", [D, T])
    posT = C.inp("posT", [D, T])
    R = C.dram("R", [D, T])
    with ExitStack() as es:
        a = [sbt(es, nc, f"ri_a{i}", [128, T], F32) for i in range(2)]
        b = [sbt(es, nc, f"ri_b{i}", [128, T], F32) for i in range(2)]
        for j in range(8):
            i = j % 2
            P.dma("sp", lambda e, i=i, j=j: e.dma_start(out=a[i][:], in_=xT[j * 128:(j + 1) * 128, :]), writes=[f"ri_a{i}"])
            P.dma("act", lambda e, i=i, j=j: e.dma_start(out=b[i][:], in_=posT[j * 128:(j + 1) * 128, :]), writes=[f"ri_b{i}"])
            P.op("pool", lambda e, i=i: e.tensor_tensor(out=a[i][:], in0=a[i][:], in1=b[i][:], op=ALU.add), reads=[f"ri_a{i}", f"ri_b{i}"], writes=[f"ri_a{i}"])
            P.dma("sp", lambda e, i=i, j=j: e.dma_start(out=R[j * 128:(j + 1) * 128, :], in_=a[i][:]), reads=[f"ri_a{i}"], writes=[f"R{j}"])
        P.barrier()


def norm_chunk(C, K, xs, xs_key, rstd, rstd_key, sq, sq_key, ln, psum, pskey, tmp, tmp_keys=None):
    P = C.P
    ta, tb = tmp
    for j in range(8):
        P.op("pool", lambda e, j=j: e.tensor_tensor(out=sq[:, j, :ln], in0=xs[:, j, :ln], in1=xs[:, j, :ln], op=ALU.mult), reads=[xs_key], writes=[sq_key])
    for j in range(8):
        P.op("pe", lambda e, j=j: e.matmul(psum[:, :ln], lhsT=K["ones_bf"][:], rhs=sq[:, j, :ln], start=(j == 0), stop=(j == 7)),
             reads=[sq_key, "ones_bf"], writes=[pskey])
    ka, kb = tmp_keys if tmp_keys else (rstd_key + "_ta", rstd_key + "_tb")
    P.op("dve", lambda e: e.tensor_scalar(out=ta[:, :ln], in0=psum[:, :ln], scalar1=1.0 / D, scalar2=1e-6, op0=ALU.mult, op1=ALU.add), reads=[pskey], writes=[ka])
    P.op("dve", lambda e: e.reciprocal(out=rstd[:, :ln], in_=ta[:, :ln]), reads=[ka], writes=[rstd_key])
    P.op("dve", lambda e: e.tensor_scalar(out=rstd[:, :ln], in0=rstd[:, :ln], scalar1=1.0, scalar2=None, op0=ALU.min), reads=[rstd_key], writes=[rstd_key])
    for it in range(12):
        P.op("dve", lambda e: e.tensor_tensor(out=tb[:, :ln], in0=rstd[:, :ln], in1=rstd[:, :ln], op=ALU.mult), reads=[rstd_key], writes=[kb])
        P.op("dve", lambda e: e.scalar_tensor_tensor(out=tb[:, :ln], in0=tb[:, :ln], scalar=-0.5, in1=ta[:, :ln], op0=ALU.mult, op1=ALU.mult), reads=[kb, ka], writes=[kb])
        P.op("dve", lambda e: e.scalar_tensor_tensor(out=rstd[:, :ln], in0=tb[:, :ln], scalar=1.5, in1=rstd[:, :ln], op0=ALU.add, op1=ALU.mult), reads=[kb, rstd_key], writes=[rstd_key])


def stage_norm1_inproj(C, K, PS, l, need_ctx):
    nc, P = C.nc, C.P
    R = C.dram("R", [D, T])
    Rv = R.rearrange("(j p) t -> p j t", p=128)
    w_in = C.inp("w_in", [L, D, IN_COLS])
    convw = C.inp("conv_t", [L, 128, 26, 4])
    UHY = C.dram("UHY", [T, 1536])
    URW = C.dram("URW", [1792, T])
    VTM = C.dram("VTM", [T, 512])
    VTMR = C.dram("VTMR", [T, 512])
    US5 = C.dram("US5", [512, T])
    GATE = C.dram("GATE", [3072, T], BF16)
    ada, gs1 = K[f"ada{l}"], K[f"gs1_{l}"]
    with ExitStack() as es:
        hT = sbt(es, nc, "hT", [128, 8, T], BF16)
        cw = sbt(es, nc, "convw", [128, 26, 4], F32)
        es_n = ExitStack()
        xs = [sbt(es_n, nc, f"n1xs{i}", [128, 8, 512], F32) for i in range(2)]
        sq = sbt(es_n, nc, "n1sq", [128, 8, 512], BF16)
        rstd = sbt(es_n, nc, "n1rstd", [128, 512], F32)
        tmp = [sbt(es_n, nc, f"n1tmp{i}", [128, 512], F32) for i in range(2)]
        ntmp = [sbt(es_n, nc, f"n1nt{i}", [128, 512], F32) for i in range(2)]
        P.dma("sp", lambda e: e.dma_start(out=cw[:], in_=convw[l]), writes=["convw"])
        for ci, (c0, ln) in enumerate(CHUNKS):
            s = 1 if ci == 0 else 0
            x = xs[ci % 2]
            tg = f"n1_{ci % 2}"
            P.dma("sp", lambda e, x=x, c0=c0, ln=ln: e.dma_start(out=x[:, :, :ln], in_=Rv[:, :, c0:c0 + ln]), reads=[f"R{j}" for j in range(8)], writes=[tg + "xs"])
            norm_chunk(C, K, x, tg + "xs", rstd, "n1rstd", sq, "n1sq", ln, PS[1], "ps1", ntmp)
            for j in range(8):
                t_ = tmp[j % 2]
                P.op("dve", lambda e, t_=t_, x=x, j=j, ln=ln, s=s: e.scalar_tensor_tensor(out=t_[:, :ln], in0=x[:, j, :ln], scalar=gs1[:, j, s:s + 1], in1=rstd[:, :ln], op0=ALU.mult, op1=ALU.mult),
                     reads=[tg + "xs", "n1rstd", f"gs{l}"], writes=[f"n1tmp{j % 2}"])
                P.op("act", lambda e, t_=t_, j=j, c0=c0, ln=ln, s=s: e.activation(out=hT[:, j, c0:c0 + ln], in_=t_[:, :ln], func=AF.Identity, bias=ada[:, j, s:s + 1], scale=1.0),
                     reads=[f"n1tmp{j % 2}", f"ada{l}"], writes=[f"hT{ci}"])
        P.barrier()
        es_n.close()
        wv = w_in[l].rearrange("(k p) c -> p k c", p=128)
        wst = [sbt(es, nc, f"ipw_s{i}", [128, 8, 128], F32) for i in range(2)]
        wb = [sbt(es, nc, f"ipw_b{i}", [128, 8, 128], BF16) for i in range(2)]
        zt = [sbt(es, nc, f"zt{i}", [128, T], F32) for i in range(2)]
        ut = [sbt(es, nc, "ut0", [128, T], F32)] * 2
        stg = [sbt(es, nc, "tstg0", [128, 34, 128], F32)] * 2
        sg2 = sbt(es, nc, "tstg2", [128, 34, 128], F32)
        gtb = [sbt(es, nc, f"gtb{i}", [128, T], BF16) for i in range(2)]
        nct = IN_COLS // 128
        psi = 2
        for ct in range(nct):
            i = ct % 2
            P.dma("sp" if i == 0 else "act", lambda e, i=i, ct=ct: e.dma_start(out=wst[i][:], in_=wv[:, :, ct * 128:(ct + 1) * 128]), writes=[f"ipw_s{i}"])
            P.op("pool", lambda e, i=i: e.tensor_copy(out=wb[i][:], in_=wst[i][:]), reads=[f"ipw_s{i}"], writes=[f"ipw_b{i}"])
            z = zt[i]
            for ci, (c0, ln) in enumerate(CHUNKS):
                ps = PS[2 + (psi % 4)]
                pk = f"ps{2 + (psi % 4)}"
                psi += 1
                for k in range(8):
                    P.op("pe", lambda e, ps=ps, i=i, k=k, c0=c0, ln=ln: e.matmul(ps[:, :ln], lhsT=wb[i][:, k, :], rhs=hT[:, k, c0:c0 + ln], start=(k == 0), stop=(k == 7)),
                         reads=[f"ipw_b{i}", f"hT{ci}"], writes=[pk])
                if ct < 30:
                    eng = "act" if ci % 2 else "dve"
                    if eng == "act":
                        P.op("act", lambda e, ps=ps, z=z, c0=c0, ln=ln: e.activation(out=z[:, c0:c0 + ln], in_=ps[:, :ln], func=AF.Copy), reads=[pk], writes=[f"zt{i}"])
                    else:
                        P.op("dve", lambda e, ps=ps, z=z, c0=c0, ln=ln: e.tensor_copy(out=z[:, c0:c0 + ln], in_=ps[:, :ln]), reads=[pk], writes=[f"zt{i}"])
                else:
                    P.op("act", lambda e, ps=ps, i=i, c0=c0, ln=ln: e.activation(out=gtb[i][:, c0:c0 + ln], in_=ps[:, :ln], func=AF.Sigmoid), reads=[pk], writes=[f"gtb{i}"])
            if ct < 26:
                u = ut[i]
                for (s0, sl) in ((0, TC), (TC, TL)):
                    P.op("dve", lambda e, u=u, z=z, s0=s0, sl=sl, ct=ct: e.tensor_scalar(out=u[:, s0:s0 + sl], in0=z[:, s0:s0 + sl], scalar1=cw[:, ct, 1:2], scalar2=cw[:, ct, 3:4], op0=ALU.mult, op1=ALU.add),
                         reads=[f"zt{i}", "convw"], writes=["ut0"])
                    P.op("dve", lambda e, u=u, z=z, s0=s0, sl=sl, ct=ct: e.scalar_tensor_tensor(out=u[:, s0 + 1:s0 + sl], in0=z[:, s0:s0 + sl - 1], scalar=cw[:, ct, 0:1], in1=u[:, s0 + 1:s0 + sl], op0=ALU.mult, op1=ALU.add),
                         reads=[f"zt{i}", "ut0", "convw"], writes=["ut0"])
                    P.op("dve", lambda e, u=u, z=z, s0=s0, sl=sl, ct=ct: e.scalar_tensor_tensor(out=u[:, s0:s0 + sl - 1], in0=z[:, s0 + 1:s0 + sl], scalar=cw[:, ct, 2:3], in1=u[:, s0:s0 + sl - 1], op0=ALU.mult, op1=ALU.add),
                         reads=[f"zt{i}", "ut0", "convw"], writes=["ut0"])
                tm_dst = None
                if ct < 12:
                    tm_dst = UHY[:, ct * 128:(ct + 1) * 128]
                    tmk = f"UHY{ct}"
                elif 20 <= ct < 24:
                    tm_dst = VTM[:, (ct - 20) * 128:(ct - 19) * 128]
                    tmk = f"VTM{ct - 20}"
                if ct >= 12:
                    P.dma("sp", lambda e, u=u, ct=ct: e.dma_start(out=URW[(ct - 12) * 128:(ct - 11) * 128, :], in_=u[:]), reads=["ut0"], writes=[f"URW{ct - 12}"])
                if tm_dst is not None:
                    sg = stg[i]
                    for b0 in range(0, 34, 4):
                        nb = min(4, 34 - b0)
                        ps = PS[6 + (b0 // 4) % 2]
                        pk = f"ps{6 + (b0 // 4) % 2}"
                        for bb in range(nb):
                            P.op("pe", lambda e, ps=ps, u=u, b0=b0, bb=bb: e.transpose(ps[:, bb * 128:(bb + 1) * 128], u[:, (b0 + bb) * 128:(b0 + bb + 1) * 128], K["ident"][:]),
                                 reads=["ut0", "ident"], writes=[pk])
                        P.op("act", lambda e, ps=ps, sg=sg, b0=b0, nb=nb: e.activation(out=sg[:, b0:b0 + nb, :], in_=ps[:, :nb * 128].rearrange("p (b c) -> p b c", c=128), func=AF.Copy),
                             reads=[pk], writes=["tstg0"])
                    for b0 in range(0, 34, 6):
                        nb = min(6, 34 - b0)
                        P.dma("sp", lambda e, sg=sg, tm_dst=tm_dst, b0=b0, nb=nb: e.dma_start(out=tm_dst[b0 * 128:(b0 + nb) * 128, :].rearrange("(b p) c -> p b c", p=128), in_=sg[:, b0:b0 + nb, :]), reads=["tstg0"], writes=[tmk])
                if 20 <= ct < 24:
                    vr_dst = VTMR[:, (ct - 20) * 128:(ct - 19) * 128]
                    for b0 in range(0, 34, 4):
                        nb = min(4, 34 - b0)
                        ps = PS[6 + (b0 // 4) % 2]
                        pk = f"ps{6 + (b0 // 4) % 2}"
                        for bb in range(nb):
                            P.op("pe", lambda e, ps=ps, sg=sg, b0=b0, bb=bb: e.matmul(ps[:, bb * 128:(bb + 1) * 128], lhsT=K["Jm"][:], rhs=sg[:, b0 + bb, :], start=True, stop=True),
                                 reads=["tstg0", "Jm"], writes=[pk])
                        for bb in range(nb):
                            P.op("act", lambda e, ps=ps, b0=b0, bb=bb: e.activation(out=sg2[:, 33 - (b0 + bb), :], in_=ps[:, bb * 128:(bb + 1) * 128], func=AF.Copy), reads=[pk], writes=["tstg2"])
                    for b0 in range(0, 34, 6):
                        nb = min(6, 34 - b0)
                        P.dma("sp", lambda e, vr_dst=vr_dst, b0=b0, nb=nb: e.dma_start(out=vr_dst[b0 * 128:(b0 + nb) * 128, :].rearrange("(b p) c -> p b c", p=128), in_=sg2[:, b0:b0 + nb, :]), reads=["tstg2"], writes=[f"VTMR{ct - 20}"])
            elif ct < 30:
                P.dma("sp", lambda e, z=z, ct=ct: e.dma_start(out=US5[(ct - 26) * 128:(ct - 25) * 128, :], in_=z[:]), reads=[f"zt{i}"], writes=[f"US5{ct - 26}"])
            else:
                P.dma("sp", lambda e, i=i, ct=ct: e.dma_start(out=GATE[(ct - 30) * 128:(ct - 29) * 128, :], in_=gtb[i][:]), reads=[f"gtb{i}"], writes=[f"GATE{ct - 30}"])
        P.barrier()


def stage_merge(C, K, PS, l, need_ctx):
    nc, P = C.nc, C.P
    R = C.dram("R", [D, T])
    Rv = R.rearrange("(j p) t -> p j t", p=128)
    GATE = C.dram("GATE", [3072, T], BF16)
    Gv = GATE.rearrange("(b j p) t -> p b j t", p=128, j=8)
    Y = [C.dram(n, [512, T], BF16) for n in ("YHY", "YRW", "YS5")]
    wbr = C.inp("w_branch", [L, 3, 512, D])
    wout = C.inp("w_out", [L, D, D])
    ada = K[f"ada{l}"]
    with ExitStack() as es:
        wbb = sbt(es, nc, "wbb", [128, 12, D], BF16)
        wob = sbt(es, nc, "wob", [128, 8, D], BF16)
        wst = [sbt(es, nc, f"mwst{i}", [128, D], F32) for i in range(2)]
        n = 0
        for b in range(3):
            for k in range(4):
                i = n % 2
                P.dma("sp", lambda e, i=i, b=b, k=k: e.dma_start(out=wst[i][:], in_=wbr[l, b, k * 128:(k + 1) * 128, :]), writes=[f"mwst{i}"])
                P.op("pool", lambda e, i=i, b=b, k=k: e.tensor_copy(out=wbb[:, b * 4 + k, :], in_=wst[i][:]), reads=[f"mwst{i}"], writes=["wbb"])
                n += 1
        for k in range(8):
            i = n % 2
            P.dma("sp", lambda e, i=i, k=k: e.dma_start(out=wst[i][:], in_=wout[l, k * 128:(k + 1) * 128, :]), writes=[f"mwst{i}"])
            P.op("pool", lambda e, i=i, k=k: e.tensor_copy(out=wob[:, k, :], in_=wst[i][:]), reads=[f"mwst{i}"], writes=["wob"])
            n += 1
        yb = [sbt(es, nc, f"myb{i}", [128, 12, 512], BF16) for i in range(2)]
        gb = [sbt(es, nc, f"mgb{i}", [128, 3, 8, 512], BF16) for i in range(2)]
        xr = [sbt(es, nc, f"mxr{i}", [128, 8, 512], F32) for i in range(2)]
        mT = sbt(es, nc, "mT", [128, 8, 512], BF16)
        t1 = [sbt(es, nc, f"mt1_{i}", [128, 512], F32) for i in range(2)]
        for ci, (c0, ln) in enumerate(CHUNKS):
            if ci == 0 and not need_ctx:
                continue
            s = 1 if ci == 0 else 0
            i = ci % 2
            for b in range(3):
                P.dma("sp", lambda e, i=i, b=b, c0=c0, ln=ln: e.dma_start(out=yb[i][:, b * 4:(b + 1) * 4, :ln], in_=Y[b].rearrange("(k p) t -> p k t", p=128)[:, :, c0:c0 + ln]),
                      reads=[f"Y{b}"], writes=[f"myb{i}"])
                P.dma("act", lambda e, i=i, b=b, c0=c0, ln=ln: e.dma_start(out=gb[i][:, b, :, :ln], in_=Gv[:, b, :, c0:c0 + ln]),
                      reads=[f"GATE{b * 8 + j}" for j in range(8)], writes=[f"mgb{i}"])
            P.dma("sp", lambda e, i=i, c0=c0, ln=ln: e.dma_start(out=xr[i][:, :, :ln], in_=Rv[:, :, c0:c0 + ln]), reads=[f"R{j}" for j in range(8)], writes=[f"mxr{i}"])
            for dt_ in range(8):
                for b in range(3):
                    ps = PS[b]
                    for k in range(4):
                        P.op("pe", lambda e, ps=ps, i=i, b=b, k=k, dt_=dt_, ln=ln: e.matmul(ps[:, :ln], lhsT=wbb[:, b * 4 + k, dt_ * 128:(dt_ + 1) * 128], rhs=yb[i][:, b * 4 + k, :ln], start=(k == 0), stop=(k == 3)),
                             reads=["wbb", f"myb{i}"], writes=[f"ps{b}"])
                ta, tb = t1
                P.op("dve", lambda e, i=i, dt_=dt_, ln=ln: e.tensor_tensor(out=ta[:, :ln], in0=PS[0][:, :ln], in1=gb[i][:, 0, dt_, :ln], op=ALU.mult), reads=["ps0", f"mgb{i}"], writes=["mt1_0"])
                P.op("dve", lambda e, i=i, dt_=dt_, ln=ln: e.tensor_tensor(out=tb[:, :ln], in0=PS[1][:, :ln], in1=gb[i][:, 1, dt_, :ln], op=ALU.mult), reads=["ps1", f"mgb{i}"], writes=["mt1_1"])
                P.op("pool", lambda e, ln=ln: e.tensor_tensor(out=ta[:, :ln], in0=ta[:, :ln], in1=tb[:, :ln], op=ALU.add), reads=["mt1_0", "mt1_1"], writes=["mt1_0"])
                P.op("dve", lambda e, i=i, dt_=dt_, ln=ln: e.tensor_tensor(out=tb[:, :ln], in0=PS[2][:, :ln], in1=gb[i][:, 2, dt_, :ln], op=ALU.mult), reads=["ps2", f"mgb{i}"], writes=["mt1_1"])
                P.op("pool", lambda e, dt_=dt_, ln=ln: e.tensor_tensor(out=mT[:, dt_, :ln], in0=ta[:, :ln], in1=tb[:, :ln], op=ALU.add), reads=["mt1_0", "mt1_1"], writes=[f"mT{dt_}"])
            for dt_ in range(8):
                ps = PS[4 + dt_ % 2]
                pk = f"ps{4 + dt_ % 2}"
                for k in range(8):
                    P.op("pe", lambda e, ps=ps, k=k, dt_=dt_, ln=ln: e.matmul(ps[:, :ln], lhsT=wob[:, k, dt_ * 128:(dt_ + 1) * 128], rhs=mT[:, k, :ln], start=(k == 0), stop=(k == 7)),
                         reads=["wob", f"mT{k}"], writes=[pk])
                P.op("dve", lambda e, ps=ps, i=i, dt_=dt_, ln=ln, s=s: e.scalar_tensor_tensor(out=xr[i][:, dt_, :ln], in0=ps[:, :ln], scalar=ada[:, 16 + dt_, s:s + 1], in1=xr[i][:, dt_, :ln], op0=ALU.mult, op1=ALU.add),
                     reads=[pk, f"mxr{i}", f"ada{l}"], writes=[f"mxr{i}"])
            P.dma("sp", lambda e, i=i, c0=c0, ln=ln: e.dma_start(out=Rv[:, :, c0:c0 + ln], in_=xr[i][:, :, :ln]), reads=[f"mxr{i}"], writes=[f"R{j}" for j in range(8)])
        P.barrier()


def stage_moe(C, K, PS, l, need_ctx, final):
    nc, P = C.nc, C.P
    R = C.dram("R", [D, T])
    Rv = R.rearrange("(j p) t -> p j t", p=128)
    H2 = C.dram("H2", [D, T], BF16)
    H2v = H2.rearrange("(j p) t -> p j t", p=128)
    rw = C.inp("router_w_t", [L, 128, 8, NE])
    rb = C.inp("router_b", [L, NE])
    w1 = C.inp("moe_w1", [L, NE, D, 2 * D])
    w2 = C.inp("moe_w2", [L, NE, D, D])
    b1 = C.inp("moe_b1_t", [L, 128, NE, 16])
    b2 = C.inp("moe_b2_t", [L, 128, NE, 8])
    outT = C.dram("outT", [D, TL], kind="ExternalOutput") if final else None
    ada, gs2 = K[f"ada{l}"], K[f"gs2_{l}"]
    chunks = [(ci, c0, ln) for ci, (c0, ln) in enumerate(CHUNKS) if not (ci == 0 and not need_ctx)]
    with ExitStack() as es:
        GT = sbt(es, nc, "GT", [32, T], F32)
        b1t = sbt(es, nc, "b1t", [128, NE, 16], F32)
        b2t = sbt(es, nc, "b2t", [128, NE, 8], F32)
        P.dma("sp", lambda e: e.dma_start(out=b1t[:], in_=b1[l]), writes=["b1t"])
        P.dma("sp", lambda e: e.dma_start(out=b2t[:], in_=b2[l]), writes=["b2t"])
        with ExitStack() as es2:
            xs = [sbt(es2, nc, f"n2xs{i}", [128, 8, 512], F32) for i in range(2)]
            sq = sbt(es2, nc, "n2sq", [128, 8, 512], BF16)
            rstd = sbt(es2, nc, "n2rstd", [128, 512], F32)
            hf = sbt(es2, nc, "n2hf", [128, 8, 512], F32)
            ntmp = [sbt(es2, nc, f"n2nt{i}", [128, 512], F32) for i in range(2)]
            hb = [sbt(es2, nc, f"n2hb{i}", [128, 8, 512], BF16) for i in range(2)]
            rwt = sbt(es2, nc, "rwt", [128, 8, NE], F32)
            rbt = sbt(es2, nc, "rbt", [128, NE], F32)
            lg = sbt(es2, nc, "lg", [128, NE], F32)
            m8 = sbt(es2, nc, "m8", [128, 8], F32)
            nm = sbt(es2, nc, "nm", [128, 1], F32)
            msk = sbt(es2, nc, "msk", [128, NE], F32)
            ex = sbt(es2, nc, "ex", [128, NE], F32)
            ssum = sbt(es2, nc, "ssum", [128, 1], F32)
            P.dma("sp", lambda e: e.dma_start(out=rwt[:], in_=rw[l]), writes=["rwt"])
            P.dma("sp", lambda e: e.dma_start(out=rbt[:], in_=rb[l:l + 1, :].partition_broadcast(128)), writes=["rbt"])
            for (ci, c0, ln) in chunks:
                s = 1 if ci == 0 else 0
                i = ci % 2
                x = xs[i]
                tg = f"n2_{i}"
                P.dma("sp", lambda e, x=x, c0=c0, ln=ln: e.dma_start(out=x[:, :, :ln], in_=Rv[:, :, c0:c0 + ln]), reads=[f"R{j}" for j in range(8)], writes=[tg + "xs"])
                norm_chunk(C, K, x, tg + "xs", rstd, "n2_rstd", sq, "n2sq", ln, PS[0], "ps0", ntmp)
                for j in range(8):
                    P.op("dve", lambda e, x=x, j=j, ln=ln, s=s: e.scalar_tensor_tensor(out=hf[:, j, :ln], in0=x[:, j, :ln], scalar=gs2[:, j, s:s + 1], in1=rstd[:, :ln], op0=ALU.mult, op1=ALU.mult),
                         reads=[tg + "xs", "n2_rstd", f"gs{l}"], writes=["n2hf"])
                    P.op("act", lambda e, j=j, ln=ln, s=s: e.activation(out=hf[:, j, :ln], in_=hf[:, j, :ln], func=AF.Identity, bias=ada[:, 24 + j, s:s + 1], scale=1.0),
                         reads=["n2hf", f"ada{l}"], writes=["n2hf"])
                    P.op("pool", lambda e, i=i, j=j, ln=ln: e.tensor_copy(out=hb[i][:, j, :ln], in_=hf[:, j, :ln]), reads=["n2hf"], writes=[f"n2hb{i}"])
                P.dma("sp", lambda e, i=i, c0=c0, ln=ln: e.dma_start(out=H2v[:, :, c0:c0 + ln], in_=hb[i][:, :, :ln]), reads=[f"n2hb{i}"], writes=["H2"])
                if ci == 0 and "dbgRS" in C.dump:
                    d1 = C.dram("dbgRS", [128, 512]); d2 = C.dram("dbgHF0", [128, 8, 512]); d3 = C.dram("dbgXS", [128, 8, 512])
                    P.dma("sp", lambda e: e.dma_start(out=d1[:, :], in_=rstd[:]), reads=["n2_rstd"], writes=["dbgRS"])
                    P.dma("sp", lambda e: e.dma_start(out=d2[:, :, :], in_=hf[:]), reads=["n2hf"], writes=["dbgHF0"])
                    P.dma("sp", lambda e, x=x: e.dma_start(out=d3[:, :, :], in_=x[:]), reads=[tg + "xs"], writes=["dbgXS"])
                for tb in range(ln // 128):
                    ps = PS[1 + tb % 2]
                    pk = f"ps{1 + tb % 2}"
                    for j in range(8):
                        P.op("pe", lambda e, ps=ps, j=j, tb=tb: e.matmul(ps[:, :NE], lhsT=hf[:, j, tb * 128:(tb + 1) * 128], rhs=rwt[:, j, :], start=(j == 0), stop=(j == 7)),
                             reads=["n2hf", "rwt"], writes=[pk])
                    P.op("dve", lambda e, ps=ps: e.tensor_tensor(out=lg[:], in0=ps[:, :NE], in1=rbt[:], op=ALU.add), reads=[pk, "rbt"], writes=["lg"])
                    P.op("dve", lambda e: e.max(out=m8[:], in_=lg[:]), reads=["lg"], writes=["m8"])
                    P.op("dve", lambda e: e.tensor_scalar(out=nm[:], in0=m8[:, 0:1], scalar1=-1.0, scalar2=None, op0=ALU.mult), reads=["m8"], writes=["nm"])
                    P.op("dve", lambda e: e.tensor_scalar(out=msk[:], in0=lg[:], scalar1=m8[:, 3:4], scalar2=None, op0=ALU.is_ge), reads=["lg", "m8"], writes=["msk"])
                    P.op("act", lambda e: e.activation(out=ex[:], in_=lg[:], func=AF.Exp, bias=nm[:, 0:1], scale=1.0), reads=["lg", "nm"], writes=["ex"])
                    P.op("dve", lambda e: e.tensor_tensor(out=ex[:], in0=ex[:], in1=msk[:], op=ALU.mult), reads=["ex", "msk"], writes=["ex"])
                    P.op("dve", lambda e: e.reduce_sum(out=ssum[:], in_=ex[:], axis=AX.X), reads=["ex"], writes=["ssum"])
                    P.op("dve", lambda e: e.reciprocal(out=ssum[:], in_=ssum[:]), reads=["ssum"], writes=["ssum"])
                    P.op("dve", lambda e: e.tensor_scalar(out=ex[:], in0=ex[:], scalar1=ssum[:, 0:1], scalar2=None, op0=ALU.mult), reads=["ex", "ssum"], writes=["ex"])
                    ps3 = PS[3]
                    P.op("pe", lambda e, ps3=ps3: e.transpose(ps3[:NE, :128], ex[:], K["ident"][:]), reads=["ex", "ident"], writes=["ps3"])
                    P.op("act", lambda e, ps3=ps3, c0=c0, tb=tb: e.activation(out=GT[:, c0 + tb * 128:c0 + (tb + 1) * 128], in_=ps3[:NE, :128], func=AF.Copy), reads=["ps3"], writes=["GT"])
            if "dbgGT" in C.dump:
                dg_ = C.dram("dbgGT", [32, T])
                for q0 in range(0, T, 256):
                    P.dma("sp", lambda e, q0=q0: e.dma_start(out=dg_[:, q0:q0 + 256], in_=GT[:, q0:q0 + 256]), reads=["GT"], writes=["dbgGT"])
            if "dbgLG" in C.dump:
                for nm_, t_ in (("dbgLG", lg), ("dbgEX", ex), ("dbgM8", m8)):
                    dd_ = C.dram(nm_, list(t_.shape))
                    P.dma("sp", lambda e, dd_=dd_, t_=t_: e.dma_start(out=dd_[:, :], in_=t_[:]), reads=["lg", "ex", "m8", "msk"], writes=[nm_])
                dd_ = C.dram("dbgHF", [128, 8, 512])
                P.dma("sp", lambda e, dd_=dd_: e.dma_start(out=dd_[:, :, :], in_=hf[:]), reads=["n2hf"], writes=["dbgHF"])
            P.barrier()
            if Prog.DBG & 8:
                return
        groups = [chunks[i:i + 2] for i in range(0, len(chunks), 2)]
        w1v = w1[l].rearrange("e (k p) c -> e p k c", p=128)
        w2v = w2[l].rearrange("e (k p) c -> e p k c", p=128)
        with ExitStack() as es3:
            w1b = [sbt(es3, nc, "w1b0", [128, 8, 2048], BF16)] * 2
            w2b = [sbt(es3, nc, "w2b0", [128, 8, 1024], BF16)] * 2
            wst = [sbt(es3, nc, f"xwst{i}", [128, 1024], F32) for i in range(2)]
            acc = sbt(es3, nc, "acc", [128, 8, 1024], F32)
            h2c = [sbt(es3, nc, f"h2c{i}", [128, 8, 512], BF16) for i in range(2)]
            actT = sbt(es3, nc, "actT", [128, 8, 512], BF16)
            gsb = sbt(es3, nc, "gsb", [128, 8, 512], BF16)
            gbs = sbt(es3, nc, "gbs", [128, 512], F32)
            glc = [sbt(es3, nc, f"glc{i}", [128, 512], F32) for i in range(2)]
            sig = [sbt(es3, nc, f"sig{i}", [128, 512], F32) for i in range(2)]
            upc = [sbt(es3, nc, f"upc{i}", [128, 512], F32) for i in range(2)]
            otmp = [sbt(es3, nc, f"otmp{i}", [128, 512], F32) for i in range(2)]
            xr = sbt(es3, nc, "moxr", [128, 8, 512], F32)
            fsq_t = sbt(es3, nc, "fsq", [128, 8, 512], BF16)
            frstd_t = sbt(es3, nc, "frstd", [128, 512], F32)
            wsi = 0
            psi = 0
            for grp in groups:
                goff = {}
                o = 0
                for (ci, c0, ln) in grp:
                    goff[ci] = o
                    o += ln
                glen = o
                P.op("pool", lambda e, glen=glen: e.memset(acc[:, :, :glen], 0.0), writes=["acc"])
                hci = 0
                for ex_ in range(NE):
                    wi = ex_ % 2
                    for k in range(8):
                        for hh in range(2):
                            si = wsi % 2
                            wsi += 1
                            P.dma("sp" if hh == 0 else "act", lambda e, si=si, ex_=ex_, k=k, hh=hh: e.dma_start(out=wst[si][:], in_=w1v[ex_, :, k, hh * 1024:(hh + 1) * 1024]), writes=[f"xwst{si}"])
                            P.op("pool", lambda e, si=si, wi=wi, k=k, hh=hh: e.tensor_copy(out=w1b[wi][:, k, hh * 1024:(hh + 1) * 1024], in_=wst[si][:]), reads=[f"xwst{si}"], writes=["w1b0"])
                    for k in range(8):
                        si = wsi % 2
                        wsi += 1
                        P.dma("sp" if k % 2 == 0 else "act", lambda e, si=si, ex_=ex_, k=k: e.dma_start(out=wst[si][:], in_=w2v[ex_, :, k, :]), writes=[f"xwst{si}"])
                        P.op("pool", lambda e, si=si, k=k: e.tensor_copy(out=w2b[0][:, k, :], in_=wst[si][:]), reads=[f"xwst{si}"], writes=["w2b0"])
                    for (ci, c0, ln) in grp:
                        hi = hci % 2
                        hci += 1
                        P.dma("sp", lambda e, hi=hi, c0=c0, ln=ln: e.dma_start(out=h2c[hi][:, :, :ln], in_=H2v[:, :, c0:c0 + ln]), reads=["H2"], writes=[f"h2c{hi}"])
                        psg = PS[7]
                        P.op("pe", lambda e, psg=psg, ex_=ex_, c0=c0, ln=ln: e.matmul(psg[:, :ln], lhsT=K["sel"][:, ex_ * 128:(ex_ + 1) * 128], rhs=GT[:, c0:c0 + ln], start=True, stop=True),
                             reads=["sel", "GT"], writes=["ps7"])
                        P.op("act", lambda e, psg=psg, ln=ln: e.activation(out=gbs[:, :ln], in_=psg[:, :ln], func=AF.Copy), reads=["ps7"], writes=["gbs"])
                        for ht in range(16):
                            ps = PS[psi % 6]
                            pk = f"ps{psi % 6}"
                            psi += 1
                            for k in range(8):
                                P.op("pe", lambda e, ps=ps, wi=wi, hi=hi, k=k, ht=ht, ln=ln: e.matmul(ps[:, :ln], lhsT=w1b[wi][:, k, ht * 128:(ht + 1) * 128], rhs=h2c[hi][:, k, :ln], start=(k == 0), stop=(k == 7)),
                                     reads=["w1b0", f"h2c{hi}"], writes=[pk])
                            q = ht % 2
                            if ht < 8:
                                P.op("dve", lambda e, ps=ps, q=q, ex_=ex_, ht=ht, ln=ln: e.tensor_scalar(out=glc[q][:, :ln], in0=ps[:, :ln], scalar1=b1t[:, ex_, ht:ht + 1], scalar2=7.0, op0=ALU.add, op1=ALU.min),
                                     reads=[pk, "b1t"], writes=[f"glc{q}"])
                                P.op("act", lambda e, q=q, ln=ln: e.activation(out=sig[q][:, :ln], in_=glc[q][:, :ln], func=AF.Sigmoid, scale=1.702), reads=[f"glc{q}"], writes=[f"sig{q}"])
                                P.op("pool", lambda e, q=q, ht=ht, ln=ln: e.tensor_tensor(out=gsb[:, ht, :ln], in0=glc[q][:, :ln], in1=sig[q][:, :ln], op=ALU.mult), reads=[f"glc{q}", f"sig{q}"], writes=[f"gsb{ht}"])
                            else:
                                P.op("dve", lambda e, ps=ps, q=q, ex_=ex_, ht=ht, ln=ln: e.tensor_scalar(out=upc[q][:, :ln], in0=ps[:, :ln], scalar1=b1t[:, ex_, ht:ht + 1], scalar2=7.0, op0=ALU.add, op1=ALU.min),
                                     reads=[pk, "b1t"], writes=[f"upc{q}"])
                                P.op("pool", lambda e, q=q, ln=ln: e.tensor_scalar(out=upc[q][:, :ln], in0=upc[q][:, :ln], scalar1=-7.0, scalar2=1.0, op0=ALU.max, op1=ALU.add), reads=[f"upc{q}"], writes=[f"upc{q}"])
                                P.op("pool", lambda e, q=q, ht=ht, ln=ln: e.tensor_tensor(out=actT[:, ht - 8, :ln], in0=upc[q][:, :ln], in1=gsb[:, ht - 8, :ln], op=ALU.mult), reads=[f"upc{q}", f"gsb{ht - 8}"], writes=[f"actT{ht - 8}"])
                        for dt_ in range(8):
                            ps = PS[psi % 6]
                            pk = f"ps{psi % 6}"
                            psi += 1
                            for k in range(8):
                                P.op("pe", lambda e, ps=ps, wi=wi, k=k, dt_=dt_, ln=ln: e.matmul(ps[:, :ln], lhsT=w2b[wi][:, k, dt_ * 128:(dt_ + 1) * 128], rhs=actT[:, k, :ln], start=(k == 0), stop=(k == 7)),
                                     reads=["w2b0", f"actT{k}"], writes=[pk])
                            q = dt_ % 2
                            o0 = goff[ci]
                            P.op("dve", lambda e, ps=ps, q=q, ex_=ex_, dt_=dt_, ln=ln: e.scalar_tensor_tensor(out=otmp[q][:, :ln], in0=ps[:, :ln], scalar=b2t[:, ex_, dt_:dt_ + 1], in1=gbs[:, :ln], op0=ALU.add, op1=ALU.mult),
                                 reads=[pk, "b2t", "gbs"], writes=[f"otmp{q}"])
                            P.op("pool", lambda e, q=q, dt_=dt_, o0=o0, ln=ln: e.tensor_tensor(out=acc[:, dt_, o0:o0 + ln], in0=acc[:, dt_, o0:o0 + ln], in1=otmp[q][:, :ln], op=ALU.add),
                                 reads=["acc", f"otmp{q}"], writes=["acc"])
                for (ci, c0, ln) in grp:
                    s = 1 if ci == 0 else 0
                    o0 = goff[ci]
                    P.dma("sp", lambda e, c0=c0, ln=ln: e.dma_start(out=xr[:, :, :ln], in_=Rv[:, :, c0:c0 + ln]), reads=[f"R{j}" for j in range(8)], writes=["moxs"])
                    for j in range(8):
                        P.op("dve", lambda e, j=j, o0=o0, ln=ln, s=s: e.scalar_tensor_tensor(out=xr[:, j, :ln], in0=acc[:, j, o0:o0 + ln], scalar=ada[:, 40 + j, s:s + 1], in1=xr[:, j, :ln], op0=ALU.mult, op1=ALU.add),
                             reads=["acc", "moxs", f"ada{l}"], writes=["moxs"])
                    if not final:
                        P.dma("sp", lambda e, c0=c0, ln=ln: e.dma_start(out=Rv[:, :, c0:c0 + ln], in_=xr[:, :, :ln]), reads=["moxs"], writes=[f"R{j}" for j in range(8)])
                    elif ci > 0:
                        norm_chunk(C, K, xr, "moxs", frstd_t, "morstd", fsq_t, "mosq", ln, PS[6], "ps6", (glc[0], glc[1]), ("glc0", "glc1"))
                        for j in range(8):
                            P.op("dve", lambda e, j=j, ln=ln: e.scalar_tensor_tensor(out=xr[:, j, :ln], in0=xr[:, j, :ln], scalar=K["fing"][:, j:j + 1], in1=frstd_t[:, :ln], op0=ALU.mult, op1=ALU.mult),
                                 reads=["moxs", "morstd", "fing"], writes=["moxs"])
                        P.dma("sp", lambda e, c0=c0, ln=ln: e.dma_start(out=outT.rearrange("(j p) t -> p j t", p=128)[:, :, c0 - TC:c0 - TC + ln], in_=xr[:, :, :ln]), reads=["moxs"], writes=["outT"])
            P.barrier()


def build_program(stages, feed=(), dump=()):
    nc = bass.Bass("TRN2", target_bir_lowering=False)
    C = Ctx(nc, feed, dump)
    P = C.P
    with ExitStack() as es:
        PS = [es.enter_context(nc.psum_tensor(f"ps{i}", [128, 512], F32)) for i in range(8)]
        K = stage_consts(C, es)
        rw_consts(C, K, es)
        K["actscr"] = sbt(es, nc, "actscr", [128, 128], F32)
        P.op("pool", lambda e: e.memset(K["actscr"][:], 1.0), writes=["actscr"])
        P.act_scratch = K["actscr"]
        K["eps"] = sbt(es, nc, "eps", [128, 1], F32)
        P.op("pool", lambda e: e.memset(K["eps"][:], 1e-6), writes=["eps"])
        P.barrier()
        for st in stages:
            st(C, K, PS)
        P.barrier()
        P.emit()
    return nc, C


def _vt(v, n):
    return np.ascontiguousarray(v.reshape(v.shape[0], n, 128).transpose(0, 2, 1))


def grid_pos_T():
    rows = TL // 64
    quarter = D // 4
    omega = 1.0 / (10000.0 ** (np.arange(quarter, dtype=np.float32) / np.float32(quarter)))
    rid, cid = np.meshgrid(np.arange(rows), np.arange(64), indexing="ij")

    def enc(p):
        ang = p.reshape(-1)[:, None].astype(np.float32) * omega.astype(np.float32)
        return np.concatenate([np.sin(ang), np.cos(ang)], axis=-1)
    pe = np.concatenate([enc(rid), enc(cid)], axis=-1).astype(np.float32)
    out = np.zeros((D, T), np.float32)
    out[:, TC:] = pe.T
    return out


def prep_shared(inp):
    f = np.float32
    S = {}
    S["ident_in"] = np.eye(128, dtype=f)
    sel = np.zeros((32, NE * 128), f)
    for e in range(NE):
        sel[e, e * 128:(e + 1) * 128] = 1.0
    S["sel_in"] = sel
    S["final_g_t"] = _vt(inp["final_g"][None], 8)[0]
    S["ada_w"] = inp["ada_w"]
    S["ada_b_t"] = _vt(inp["ada_b"], 48)
    S["norm1_g_t"] = _vt(inp["norm1_g"], 8)
    S["norm2_g_t"] = _vt(inp["norm2_g"], 8)
    S["posT"] = grid_pos_T()
    S["w_in"] = inp["w_in"]
    cw = np.concatenate([inp["hy_conv_w"], inp["rw_conv_w"]], axis=-1)
    cb = np.concatenate([inp["hy_conv_b"], inp["rw_conv_b"]], axis=-1)[:, None]
    c4 = np.concatenate([cw, cb], axis=1)
    S["conv_t"] = np.ascontiguousarray(c4.reshape(L, 4, 26, 128).transpose(0, 3, 2, 1))
    S["w_branch"] = inp["w_branch"]
    S["w_out"] = inp["w_out"]
    S["router_w_t"] = np.ascontiguousarray(inp["router_w"].reshape(L, 8, 128, NE).transpose(0, 2, 1, 3))
    S["router_b"] = inp["router_b"]
    S["moe_w1"] = inp["moe_w1"]
    S["moe_w2"] = inp["moe_w2"]
    S["moe_b1_t"] = np.ascontiguousarray(inp["moe_b1"].reshape(L, NE, 16, 128).transpose(0, 3, 1, 2))
    S["moe_b2_t"] = np.ascontiguousarray(inp["moe_b2"].reshape(L, NE, 8, 128).transpose(0, 3, 1, 2))
    def st_layout(a):
        return np.ascontiguousarray(a.reshape(L, 2, 16, 2, 64).transpose(0, 1, 3, 4, 2).reshape(L, 2, 128, 16))
    S["s5_a_re_t"] = st_layout(inp["s5_a_re"])
    S["s5_a_im_t"] = st_layout(inp["s5_a_im"])
    S["s5_ldt_t"] = st_layout(np.broadcast_to(inp["s5_log_dt"][..., None], (L, 2, 32, 64)))
    BT = np.zeros((L, 16, 32, 256), f)
    CT = np.zeros((L, 16, 128, 64), f)
    for ri, (bb, cc) in enumerate(((inp["s5_b_re"], inp["s5_c_re"]), (inp["s5_b_im"], inp["s5_c_im"]))):
        for gg in range(2):
            bg = bb.reshape(L, 16, 2, 64, 16)[:, :, gg]
            cg = cc.reshape(L, 16, 2, 16, 64)[:, :, gg]
            BT[:, :, gg * 16:(gg + 1) * 16, ri * 128 + gg * 64: ri * 128 + (gg + 1) * 64] = bg.transpose(0, 1, 3, 2)
            CT[:, :, gg * 64:(gg + 1) * 64, ri * 32 + gg * 16: ri * 32 + (gg + 1) * 16] = cg.transpose(0, 1, 3, 2)
    S["s5_BT"] = BT
    S["s5_CT"] = CT
    S["s5_d_t"] = _vt(inp["s5_d"], 4)
    S["s5_glu_w"] = inp["s5_glu_w"]
    S["s5_glu_b_t"] = _vt(inp["s5_glu_b"], 8)
    S["jiota"] = np.ascontiguousarray(np.broadcast_to(np.arange(512, dtype=f)[None], (128, 512)))
    S.update(hyena_consts(TL))
    S.update(hyena_consts(TC))
    for k in ("hy_w1", "hy_w2", "hy_w3", "hy_b3", "hy_bias"):
        S[k] = inp[k]
    S["hy_pv_t"] = np.ascontiguousarray(np.stack([inp["hy_b1"], inp["hy_f1"], inp["hy_b2"], inp["hy_f2"]], axis=-1))
    S["Jm_in"] = np.ascontiguousarray(np.eye(128, dtype=f)[::-1])
    bo = np.zeros((128, 128), f); bo[:64, :64] = 1.0; bo[64:, 64:] = 1.0
    S["BO_in"] = bo
    m16 = np.zeros((16, 512), f)
    for r_ in range(16):
        d_, j_ = r_ // 8, (r_ % 8) // 2
        m16[r_, d_ * 256 + j_ * 64: d_ * 256 + (j_ + 1) * 64] = 1.0
    S["mask16_in"] = m16
    mh = np.zeros((128, 2), f); mh[:64, 0] = 1.0; mh[64:, 1] = 1.0
    S["maskhh_in"] = mh
    rwc = np.zeros((128, 4), f); rwc[:, 0] = 1.0; rwc[:, 1] = -0.5; rwc[:, 2] = 64e-5
    S["rwc_in"] = rwc
    S["rw_vec_t"] = np.ascontiguousarray(np.stack([_vt(inp["rw_k_k"], 4), _vt(inp["rw_k_a"], 4), _vt(inp["rw_r_k"].reshape(L, 512), 4),
                                                    _vt(inp["rw_ln_g"], 4), _vt(inp["rw_ln_b"], 4)], axis=-1))
    S["rw_w0a0_t"] = np.ascontiguousarray(np.stack([_vt(inp["rw_w0"][:, 0], 4), _vt(inp["rw_w0"][:, 1], 4),
                                                     _vt(inp["rw_a0"][:, 0], 4), _vt(inp["rw_a0"][:, 1], 4)], axis=-1))
    for k in ("rw_w_up", "rw_a_up", "rw_g_up"):
        S[k] = inp[k]
    return S


def prep_core(inp, b):
    f = np.float32
    Cc = {}
    xcat = np.concatenate([inp["ctx"][b], inp["x"][b]], axis=0)
    Cc["xT"] = np.ascontiguousarray(xcat.T)
    cc = np.stack([inp["c"][b], inp["c_ctx"]], axis=-1)
    Cc["ccT"] = np.ascontiguousarray(cc.reshape(8, 128, 2).transpose(1, 0, 2).reshape(128, 16))
    return Cc


TWO_PI = 2.0 * math.pi


def emit_sincos(P, eng_sel, ang, ang_key, sin_t, cos_t, out_key, tmpf, tmpi, shape_sl, consts=None):
    sl = shape_sl
    for (dst, shift) in ((sin_t, 0.0), (cos_t, 0.5 * math.pi)):
        P.op("dve", lambda e, shift=shift: e.tensor_scalar(out=tmpi[sl], in0=ang[sl], scalar1=shift, scalar2=1.0 / TWO_PI, op0=ALU.add, op1=ALU.mult),
             reads=[ang_key], writes=[out_key + "_ti"])
        P.op("dve", lambda e: e.tensor_copy(out=tmpf[sl], in_=tmpi[sl]), reads=[out_key + "_ti"], writes=[out_key + "_tf"])
        P.op("dve", lambda e: e.scalar_tensor_tensor(out=tmpf[sl], in0=tmpf[sl], scalar=-TWO_PI, in1=ang[sl], op0=ALU.mult, op1=ALU.add),
             reads=[out_key + "_tf", ang_key], writes=[out_key + "_tf"])
        P.op("dve", lambda e, shift=shift: e.tensor_scalar(out=tmpf[sl], in0=tmpf[sl], scalar1=shift, scalar2=-3.1415925, op0=ALU.add, op1=ALU.max),
             reads=[out_key + "_tf"], writes=[out_key + "_tf"])
        P.op("dve", lambda e: e.tensor_scalar(out=tmpf[sl], in0=tmpf[sl], scalar1=3.1415925, scalar2=None, op0=ALU.min),
             reads=[out_key + "_tf"], writes=[out_key + "_tf"])
        P.op("act", lambda e, dst=dst: e.activation(out=dst[sl], in_=tmpf[sl], func=AF.Sin), reads=[out_key + "_tf"], writes=[out_key])


def stage_s5(C, K, PS, l, need_ctx):
    nc, P = C.nc, C.P
    US5 = C.dram("US5", [512, T])
    YPRE = C.dram("YS5PRE", [512, T])
    YS5 = C.dram("YS5", [512, T], BF16)
    a_re = C.inp("s5_a_re_t", [L, 2, 128, 16])
    a_im = C.inp("s5_a_im_t", [L, 2, 128, 16])
    ldt = C.inp("s5_ldt_t", [L, 2, 128, 16])
    BTd = C.inp("s5_BT", [L, 16, 32, 256])
    CTd = C.inp("s5_CT", [L, 16, 128, 64])
    dsk = C.inp("s5_d_t", [L, 128, 4])
    gw = C.inp("s5_glu_w", [L, 512, 1024])
    gbd = C.inp("s5_glu_b_t", [L, 128, 8])
    jio = C.inp("jiota", [128, 512])
    with ExitStack() as es:
        J = sbt(es, nc, "jio", [128, 512], F32)
        P.dma("sp", lambda e: e.dma_start(out=J[:], in_=jio[:, :]), writes=["jio"])
        sm = {}
        for nm in ("are", "aim", "dt", "rho", "phi", "sn", "cs", "lbr", "lbi", "den", "nr", "cr", "ci", "ncr", "t1", "t2"):
            sm[nm] = [sbt(es, nc, f"s5{nm}{d}", [128, 16], F32) for d in range(2)]
        smi = sbt(es, nc, "s5smi", [128, 16], I32)
        for d in range(2):
            g = lambda nm: sm[nm][d]
            P.dma("sp", lambda e, d=d: e.dma_start(out=sm["are"][d][:], in_=a_re[l, d]), writes=[f"are{d}"])
            P.dma("sp", lambda e, d=d: e.dma_start(out=sm["aim"][d][:], in_=a_im[l, d]), writes=[f"aim{d}"])
            P.dma("sp", lambda e, d=d: e.dma_start(out=sm["dt"][d][:], in_=ldt[l, d]), writes=[f"dt{d}"])
            P.op("act", lambda e, d=d: e.activation(out=sm["dt"][d][:], in_=sm["dt"][d][:], func=AF.Exp), reads=[f"dt{d}"], writes=[f"dt{d}"])
            P.op("dve", lambda e, d=d: e.tensor_tensor(out=sm["t1"][d][:], in0=sm["are"][d][:], in1=sm["dt"][d][:], op=ALU.mult), reads=[f"are{d}", f"dt{d}"], writes=[f"t1{d}"])
            P.op("act", lambda e, d=d: e.activation(out=sm["rho"][d][:], in_=sm["t1"][d][:], func=AF.Exp), reads=[f"t1{d}"], writes=[f"rho{d}"])
            P.op("dve", lambda e, d=d: e.tensor_tensor(out=sm["phi"][d][:], in0=sm["aim"][d][:], in1=sm["dt"][d][:], op=ALU.mult), reads=[f"aim{d}", f"dt{d}"], writes=[f"phi{d}"])
            emit_sincos(P, None, sm["phi"][d], f"phi{d}", sm["sn"][d], sm["cs"][d], f"sc{d}", sm["t2"][d], smi, slice(None))
            P.op("dve", lambda e, d=d: e.tensor_tensor(out=sm["lbr"][d][:], in0=sm["rho"][d][:], in1=sm["cs"][d][:], op=ALU.mult), reads=[f"rho{d}", f"sc{d}"], writes=[f"lbr{d}"])
            P.op("dve", lambda e, d=d: e.tensor_tensor(out=sm["lbi"][d][:], in0=sm["rho"][d][:], in1=sm["sn"][d][:], op=ALU.mult), reads=[f"rho{d}", f"sc{d}"], writes=[f"lbi{d}"])
            P.op("dve", lambda e, d=d: e.tensor_tensor(out=sm["den"][d][:], in0=sm["are"][d][:], in1=sm["are"][d][:], op=ALU.mult), reads=[f"are{d}"], writes=[f"den{d}"])
            P.op("dve", lambda e, d=d: e.tensor_tensor(out=sm["t1"][d][:], in0=sm["aim"][d][:], in1=sm["aim"][d][:], op=ALU.mult), reads=[f"aim{d}"], writes=[f"t1{d}"])
            P.op("dve", lambda e, d=d: e.tensor_tensor(out=sm["den"][d][:], in0=sm["den"][d][:], in1=sm["t1"][d][:], op=ALU.add), reads=[f"den{d}", f"t1{d}"], writes=[f"den{d}"])
            P.op("dve", lambda e, d=d: e.reciprocal(out=sm["den"][d][:], in_=sm["den"][d][:]), reads=[f"den{d}"], writes=[f"den{d}"])
            P.op("dve", lambda e, d=d: e.tensor_scalar(out=sm["nr"][d][:], in0=sm["lbr"][d][:], scalar1=-1.0, scalar2=None, op0=ALU.add), reads=[f"lbr{d}"], writes=[f"nr{d}"])
            P.op("dve", lambda e, d=d: e.tensor_tensor(out=sm["cr"][d][:], in0=sm["nr"][d][:], in1=sm["are"][d][:], op=ALU.mult), reads=[f"nr{d}", f"are{d}"], writes=[f"cr{d}"])
            P.op("dve", lambda e, d=d: e.tensor_tensor(out=sm["t1"][d][:], in0=sm["lbi"][d][:], in1=sm["aim"][d][:], op=ALU.mult), reads=[f"lbi{d}", f"aim{d}", f"den{d}"], writes=[f"t1{d}"])
            P.op("dve", lambda e, d=d: e.tensor_tensor(out=sm["cr"][d][:], in0=sm["cr"][d][:], in1=sm["t1"][d][:], op=ALU.add), reads=[f"cr{d}", f"t1{d}"], writes=[f"cr{d}"])
            P.op("dve", lambda e, d=d: e.tensor_tensor(out=sm["cr"][d][:], in0=sm["cr"][d][:], in1=sm["den"][d][:], op=ALU.mult), reads=[f"cr{d}", f"den{d}"], writes=[f"cr{d}"])
            P.op("dve", lambda e, d=d: e.tensor_tensor(out=sm["ci"][d][:], in0=sm["lbi"][d][:], in1=sm["are"][d][:], op=ALU.mult), reads=[f"lbi{d}", f"are{d}"], writes=[f"ci{d}"])
            P.op("dve", lambda e, d=d: e.tensor_tensor(out=sm["t1"][d][:], in0=sm["nr"][d][:], in1=sm["aim"][d][:], op=ALU.mult), reads=[f"nr{d}", f"aim{d}", f"cr{d}"], writes=[f"t1{d}"])
            P.op("dve", lambda e, d=d: e.tensor_tensor(out=sm["ci"][d][:], in0=sm["ci"][d][:], in1=sm["t1"][d][:], op=ALU.subtract), reads=[f"ci{d}", f"t1{d}"], writes=[f"ci{d}"])
            P.op("dve", lambda e, d=d: e.tensor_tensor(out=sm["ci"][d][:], in0=sm["ci"][d][:], in1=sm["den"][d][:], op=ALU.mult), reads=[f"ci{d}", f"den{d}"], writes=[f"ci{d}"])
            P.op("dve", lambda e, d=d: e.tensor_scalar(out=sm["ncr"][d][:], in0=sm["cr"][d][:], scalar1=-1.0, scalar2=None, op0=ALU.mult), reads=[f"cr{d}"], writes=[f"ncr{d}"])
        ang = sbt(es, nc, "s5ang", [128, 512], F32)
        tf = sbt(es, nc, "s5tf", [128, 512], F32)
        ti_ = sbt(es, nc, "s5ti", [128, 512], I32)
        sn = sbt(es, nc, "s5sn", [128, 512], F32)
        cs = sbt(es, nc, "s5cs", [128, 512], F32)
        trt = sbt(es, nc, "s5tr", [128, 512], F32)
        tit = sbt(es, nc, "s5tit", [128, 512], F32)
        rho_t = sbt(es, nc, "s5rhot", [128, 512], F32)
        u32 = sbt(es, nc, "s5u", [32, T], F32)
        u32r = sbt(es, nc, "s5ur", [32, T], F32)
        bt = sbt(es, nc, "s5bt", [32, 256], F32)
        ctf = sbt(es, nc, "s5ctf", [128, 64], F32)
        ctb = sbt(es, nc, "s5ctb", [128, 64], BF16)
        H = {(d, ri): sbt(es, nc, f"s5H{d}{ri}", [128, T], BF16) for d in range(2) for ri in range(2)}
        w = [sbt(es, nc, f"s5w{i}", [128, 512], F32) for i in range(6)]
        q = [sbt(es, nc, f"s5q{i}", [128, 512], F32) for i in range(2)]
        qi0 = [sbt(es, nc, f"s5qi{i}", [128, 1], F32) for i in range(2)]
        hl = [sbt(es, nc, f"s5hl{i}", [128, 1], F32) for i in range(2)]
        ns1 = sbt(es, nc, "s5ns1", [128, 1], F32)
        sm1 = sbt(es, nc, "s5sm1", [128, 1], F32)
        y32 = [sbt(es, nc, f"s5y32_{i}", [32, 512], F32) for i in range(2)]
        fchunks = list(CHUNKS)
        bchunks = [(TL, TC)] + [(512 * i, 512) for i in range(8)]
        for st in range(16):
            P.dma("sp", lambda e, st=st: e.dma_start(out=u32[:], in_=US5[32 * st:32 * st + 32, :]), reads=[f"US5{st // 4}"], writes=["s5u"])
            P.op("pool", lambda e: e.tensor_copy(out=u32r[:], in_=u32[:, ::-1]), reads=["s5u"], writes=["s5ur"])
            P.dma("act", lambda e, st=st: e.dma_start(out=bt[:], in_=BTd[l, st]), writes=["s5bt"])
            P.dma("act", lambda e, st=st: e.dma_start(out=ctf[:], in_=CTd[l, st]), writes=["s5ctf"])
            P.op("pool", lambda e: e.tensor_copy(out=ctb[:, 0:32], in_=ctf[:, 0:32]), reads=["s5ctf"], writes=["s5ctb"])
            P.op("pool", lambda e: e.tensor_scalar(out=ctb[:, 32:64], in0=ctf[:, 32:64], scalar1=-1.0, scalar2=None, op0=ALU.mult), reads=["s5ctf"], writes=["s5ctb"])
            for d in range(2):
                phi = sm["phi"][d][:, st:st + 1]
                P.op("dve", lambda e, phi=phi: e.tensor_scalar(out=ang[:], in0=J[:], scalar1=phi, scalar2=None, op0=ALU.mult), reads=["jio", f"phi{d}"], writes=["s5ang"])
                emit_sincos(P, None, ang, "s5ang", sn, cs, "s5sc", tf, ti_, slice(None))
                P.op("dve", lambda e, d=d, st=st: e.tensor_scalar(out=rho_t[:], in0=J[:], scalar1=0.0, scalar2=sm["rho"][d][:, st:st + 1], op0=ALU.mult, op1=ALU.add), reads=["jio", f"rho{d}"], writes=["s5rhot"])
                P.op("dve", lambda e, d=d, st=st: e.tensor_scalar(out=trt[:], in0=cs[:], scalar1=sm["cr"][d][:, st:st + 1], scalar2=None, op0=ALU.mult), reads=["s5sc", f"cr{d}"], writes=["s5tr"])
                P.op("dve", lambda e, d=d, st=st: e.scalar_tensor_tensor(out=trt[:], in0=sn[:], scalar=sm["ci"][d][:, st:st + 1], in1=trt[:], op0=ALU.mult, op1=ALU.add), reads=["s5sc", f"ci{d}", "s5tr"], writes=["s5tr"])
                P.op("dve", lambda e, d=d, st=st: e.tensor_scalar(out=tit[:], in0=cs[:], scalar1=sm["ci"][d][:, st:st + 1], scalar2=None, op0=ALU.mult), reads=["s5sc", f"ci{d}"], writes=["s5tit"])
                P.op("dve", lambda e, d=d, st=st: e.scalar_tensor_tensor(out=tit[:], in0=sn[:], scalar=sm["ncr"][d][:, st:st + 1], in1=tit[:], op0=ALU.mult, op1=ALU.add), reads=["s5sc", f"ncr{d}", "s5tit"], writes=["s5tit"])
                P.op("dve", lambda e: e.tensor_scalar(out=ns1[:], in0=sn[:, 1:2], scalar1=-1.0, scalar2=None, op0=ALU.mult), reads=["s5sc"], writes=["s5ns1"])
                usrc = u32 if d == 0 else u32r
                ukey = "s5u" if d == 0 else "s5ur"
                chunks = fchunks if d == 0 else bchunks
                for cidx, (c0, ln) in enumerate(chunks):
                    pX, pY = PS[(2 * cidx) % 4], PS[(2 * cidx + 1) % 4]
                    kX, kY = f"ps{(2 * cidx) % 4}", f"ps{(2 * cidx + 1) % 4}"
                    P.op("pe", lambda e, pX=pX, c0=c0, ln=ln, usrc=usrc: e.matmul(pX[:, :ln], lhsT=bt[:, 0:128], rhs=usrc[:, c0:c0 + ln], start=True, stop=True), reads=["s5bt", ukey], writes=[kX])
                    P.op("pe", lambda e, pY=pY, c0=c0, ln=ln, usrc=usrc: e.matmul(pY[:, :ln], lhsT=bt[:, 128:256], rhs=usrc[:, c0:c0 + ln], start=True, stop=True), reads=["s5bt", ukey], writes=[kY])
                    P.op("dve", lambda e, pX=pX, ln=ln: e.tensor_tensor(out=w[0][:, :ln], in0=pX[:, :ln], in1=trt[:, :ln], op=ALU.mult), reads=[kX, "s5tr"], writes=["s5w0"])
                    P.op("dve", lambda e, pY=pY, ln=ln: e.tensor_tensor(out=w[1][:, :ln], in0=pY[:, :ln], in1=tit[:, :ln], op=ALU.mult), reads=[kY, "s5tit"], writes=["s5w1"])
                    P.op("dve", lambda e, pY=pY, ln=ln: e.tensor_tensor(out=w[2][:, :ln], in0=pY[:, :ln], in1=trt[:, :ln], op=ALU.mult), reads=[kY, "s5tr"], writes=["s5w2"])
                    P.op("dve", lambda e, pX=pX, ln=ln: e.tensor_tensor(out=w[3][:, :ln], in0=pX[:, :ln], in1=tit[:, :ln], op=ALU.mult), reads=[kX, "s5tit"], writes=["s5w3"])
                    P.op("pool", lambda e, ln=ln: e.tensor_tensor(out=w[0][:, :ln], in0=w[0][:, :ln], in1=w[1][:, :ln], op=ALU.subtract), reads=["s5w0", "s5w1"], writes=["s5w0"])
                    P.op("pool", lambda e, ln=ln: e.tensor_tensor(out=w[2][:, :ln], in0=w[2][:, :ln], in1=w[3][:, :ln], op=ALU.add), reads=["s5w2", "s5w3"], writes=["s5w2"])
                    if cidx == 0:
                        P.op("pool", lambda e: e.memset(qi0[0][:], 0.0), writes=["s5qi0"])
                        P.op("pool", lambda e: e.memset(qi0[1][:], 0.0), writes=["s5qi1"])
                    else:
                        P.op("dve", lambda e: e.tensor_tensor(out=sm1[:], in0=hl[0][:], in1=cs[:, 1:2], op=ALU.mult), reads=["s5hl0", "s5sc"], writes=["s5sm1"])
                        P.op("dve", lambda e: e.scalar_tensor_tensor(out=qi0[0][:], in0=hl[1][:], scalar=ns1[:, 0:1], in1=sm1[:], op0=ALU.mult, op1=ALU.add), reads=["s5hl1", "s5ns1", "s5sm1"], writes=["s5qi0"])
                        P.op("dve", lambda e: e.tensor_tensor(out=sm1[:], in0=hl[1][:], in1=cs[:, 1:2], op=ALU.mult), reads=["s5hl1", "s5sc", "s5qi0"], writes=["s5sm1"])
                        P.op("dve", lambda e: e.scalar_tensor_tensor(out=qi0[1][:], in0=hl[0][:], scalar=sn[:, 1:2], in1=sm1[:], op0=ALU.mult, op1=ALU.add), reads=["s5hl0", "s5sc", "s5sm1"], writes=["s5qi1"])
                    P.op("dve", lambda e, ln=ln: e.tensor_tensor_scan(out=q[0][:, :ln], data0=rho_t[:, :ln], data1=w[0][:, :ln], initial=qi0[0][:, 0:1], op0=ALU.mult, op1=ALU.add), reads=["s5rhot", "s5w0", "s5qi0"], writes=["s5q0"])
                    P.op("dve", lambda e, ln=ln: e.tensor_tensor_scan(out=q[1][:, :ln], data0=rho_t[:, :ln], data1=w[2][:, :ln], initial=qi0[1][:, 0:1], op0=ALU.mult, op1=ALU.add), reads=["s5rhot", "s5w2", "s5qi1"], writes=["s5q1"])
                    P.op("pool", lambda e, ln=ln: e.tensor_tensor(out=w[1][:, :ln], in0=q[0][:, :ln], in1=cs[:, :ln], op=ALU.mult), reads=["s5q0", "s5sc", "s5w1"], writes=["s5w1"])
                    P.op("pool", lambda e, ln=ln: e.tensor_tensor(out=w[3][:, :ln], in0=q[1][:, :ln], in1=sn[:, :ln], op=ALU.mult), reads=["s5q1", "s5sc", "s5w3"], writes=["s5w3"])
                    P.op("pool", lambda e, ln=ln: e.tensor_tensor(out=w[4][:, :ln], in0=q[0][:, :ln], in1=sn[:, :ln], op=ALU.mult), reads=["s5q0", "s5sc"], writes=["s5w4"])
                    P.op("pool", lambda e, ln=ln: e.tensor_tensor(out=w[5][:, :ln], in0=q[1][:, :ln], in1=cs[:, :ln], op=ALU.mult), reads=["s5q1", "s5sc"], writes=["s5w5"])
                    P.op("pool", lambda e, ln=ln: e.tensor_tensor(out=w[1][:, :ln], in0=w[1][:, :ln], in1=w[3][:, :ln], op=ALU.subtract), reads=["s5w1", "s5w3"], writes=["s5w1"])
                    P.op("pool", lambda e, ln=ln: e.tensor_tensor(out=w[4][:, :ln], in0=w[4][:, :ln], in1=w[5][:, :ln], op=ALU.add), reads=["s5w4", "s5w5"], writes=["s5w4"])
                    P.op("act", lambda e, ln=ln: e.activation(out=hl[0][:], in_=w[1][:, ln - 1:ln], func=AF.Copy), reads=["s5w1"], writes=["s5hl0"])
                    P.op("act", lambda e, ln=ln: e.activation(out=hl[1][:], in_=w[4][:, ln - 1:ln], func=AF.Copy), reads=["s5w4"], writes=["s5hl1"])
                    if d == 0:
                        dr, di = H[(0, 0)][:, c0:c0 + ln], H[(0, 1)][:, c0:c0 + ln]
                    else:
                        n0 = T - c0 - ln
                        dr, di = H[(1, 0)][:, n0:n0 + ln][:, ::-1], H[(1, 1)][:, n0:n0 + ln][:, ::-1]
                    P.op("act", lambda e, dr=dr, ln=ln: e.activation(out=dr, in_=w[1][:, :ln], func=AF.Copy), reads=["s5w1"], writes=[f"s5H{d}0"])
                    P.op("act", lambda e, di=di, ln=ln: e.activation(out=di, in_=w[4][:, :ln], func=AF.Copy), reads=["s5w4"], writes=[f"s5H{d}1"])
            for ci, (c0, ln) in enumerate(CHUNKS):
                if ci == 0 and not need_ctx:
                    continue
                ps = PS[4 + ci % 2]
                pk = f"ps{4 + ci % 2}"
                n = 0
                for d in range(2):
                    for ri in range(2):
                        P.op("pe", lambda e, ps=ps, d=d, ri=ri, c0=c0, ln=ln, n=n: e.matmul(ps[:32, :ln], lhsT=ctb[:, ri * 32:(ri + 1) * 32], rhs=H[(d, ri)][:, c0:c0 + ln], start=(n == 0), stop=(n == 3)),
                             reads=["s5ctb", f"s5H{d}{ri}"], writes=[pk])
                        n += 1
                yy = y32[ci % 2]
                P.op("act", lambda e, ps=ps, yy=yy, ln=ln: e.activation(out=yy[:, :ln], in_=ps[:32, :ln], func=AF.Copy), reads=[pk], writes=[f"s5y32_{ci % 2}"])
                P.dma("sp", lambda e, yy=yy, st=st, c0=c0, ln=ln: e.dma_start(out=YPRE[32 * st:32 * st + 32, c0:c0 + ln], in_=yy[:, :ln]), reads=[f"s5y32_{ci % 2}"], writes=["YPRE"])
        P.barrier()
    with ExitStack() as es:
        gwst = [sbt(es, nc, f"s5gws{i}", [128, 1024], F32) for i in range(2)]
        gwb = sbt(es, nc, "s5gwb", [128, 4, 1024], BF16)
        gbt = sbt(es, nc, "s5gbt", [128, 8], F32)
        dt_ = sbt(es, nc, "s5dsk", [128, 4], F32)
        P.dma("sp", lambda e: e.dma_start(out=gbt[:], in_=gbd[l]), writes=["s5gbt"])
        P.dma("sp", lambda e: e.dma_start(out=dt_[:], in_=dsk[l]), writes=["s5dsk"])
        for k in range(4):
            P.dma("sp", lambda e, k=k: e.dma_start(out=gwst[k % 2][:], in_=gw[l, k * 128:(k + 1) * 128, :]), writes=[f"s5gws{k % 2}"])
            P.op("pool", lambda e, k=k: e.tensor_copy(out=gwb[:, k, :], in_=gwst[k % 2][:]), reads=[f"s5gws{k % 2}"], writes=["s5gwb"])
        yp = [sbt(es, nc, f"s5yp{i}", [128, 4, 512], F32) for i in range(2)]
        uu = [sbt(es, nc, f"s5uu{i}", [128, 4, 512], F32) for i in range(2)]
        yg = sbt(es, nc, "s5yg", [128, 4, 512], BF16)
        lin = [sbt(es, nc, f"s5lin{i}", [128, 512], F32) for i in range(2)]
        sg = [sbt(es, nc, f"s5sg{i}", [128, 512], F32) for i in range(2)]
        yo = [sbt(es, nc, f"s5yo{i}", [128, 4, 512], BF16) for i in range(2)]
        YPv = YPRE.rearrange("(k p) t -> p k t", p=128)
        USv = US5.rearrange("(k p) t -> p k t", p=128)
        YSv = YS5.rearrange("(k p) t -> p k t", p=128)
        for ci, (c0, ln) in enumerate(CHUNKS):
            if ci == 0 and not need_ctx:
                continue
            i = ci % 2
            P.dma("sp", lambda e, i=i, c0=c0, ln=ln: e.dma_start(out=yp[i][:, :, :ln], in_=YPv[:, :, c0:c0 + ln]), reads=["YPRE"], writes=[f"s5yp{i}"])
            P.dma("act", lambda e, i=i, c0=c0, ln=ln: e.dma_start(out=uu[i][:, :, :ln], in_=USv[:, :, c0:c0 + ln]), reads=[f"US5{k}" for k in range(4)], writes=[f"s5uu{i}"])
            for k in range(4):
                P.op("dve", lambda e, i=i, k=k, ln=ln: e.scalar_tensor_tensor(out=yp[i][:, k, :ln], in0=uu[i][:, k, :ln], scalar=dt_[:, k:k + 1], in1=yp[i][:, k, :ln], op0=ALU.mult, op1=ALU.add),
                     reads=[f"s5yp{i}", f"s5uu{i}", "s5dsk"], writes=[f"s5yp{i}"])
                P.op("act", lambda e, i=i, k=k, ln=ln: e.activation(out=yg[:, k, :ln], in_=yp[i][:, k, :ln], func=AF.Gelu), reads=[f"s5yp{i}"], writes=[f"s5yg{k}"])
            for ot in range(4):
                pl, pg = PS[ot % 2], PS[2 + ot % 2]
                kl, kg = f"ps{ot % 2}", f"ps{2 + ot % 2}"
                for k in range(4):
                    P.op("pe", lambda e, pl=pl, k=k, ot=ot, ln=ln: e.matmul(pl[:, :ln], lhsT=gwb[:, k, ot * 128:(ot + 1) * 128], rhs=yg[:, k, :ln], start=(k == 0), stop=(k == 3)), reads=["s5gwb", f"s5yg{k}"], writes=[kl])
                for k in range(4):
                    P.op("pe", lambda e, pg=pg, k=k, ot=ot, ln=ln: e.matmul(pg[:, :ln], lhsT=gwb[:, k, (ot + 4) * 128:(ot + 5) * 128], rhs=yg[:, k, :ln], start=(k == 0), stop=(k == 3)), reads=["s5gwb", f"s5yg{k}"], writes=[kg])
                P.op("act", lambda e, pl=pl, ot=ot, ln=ln: e.activation(out=lin[ot % 2][:, :ln], in_=pl[:, :ln], func=AF.Identity, bias=gbt[:, ot:ot + 1], scale=1.0), reads=[kl, "s5gbt"], writes=[f"s5lin{ot % 2}"])
                P.op("act", lambda e, pg=pg, ot=ot, ln=ln: e.activation(out=sg[ot % 2][:, :ln], in_=pg[:, :ln], func=AF.Sigmoid, bias=gbt[:, ot + 4:ot + 5], scale=1.0), reads=[kg, "s5gbt"], writes=[f"s5sg{ot % 2}"])
                P.op("dve", lambda e, i=i, ot=ot, ln=ln: e.tensor_tensor(out=yo[i][:, ot, :ln], in0=lin[ot % 2][:, :ln], in1=sg[ot % 2][:, :ln], op=ALU.mult), reads=[f"s5lin{ot % 2}", f"s5sg{ot % 2}"], writes=[f"s5yo{i}"])
            P.dma("sp", lambda e, i=i, c0=c0, ln=ln: e.dma_start(out=YSv[:, :, c0:c0 + ln], in_=yo[i][:, :, :ln]), reads=[f"s5yo{i}"], writes=["Y2"])
        P.barrier()


def hyena_consts(n):
    f64 = np.float64
    N2 = 2 * n
    nch = n // 128
    t = np.arange(n, dtype=f64)
    th = 2 * np.pi * (np.arange(n, dtype=f64) + 0.5) / N2
    ang = np.outer(t, th)
    Cm, Sm = np.cos(ang), np.sin(ang)

    def tile_fwd(M):
        return np.ascontiguousarray(M.reshape(nch, 128, nch, 128).transpose(2, 1, 0, 3)).astype(ml_dtypes.bfloat16)

    def tile_inv(M):
        return np.ascontiguousarray((M.T * (2.0 / N2)).reshape(nch, 128, nch, 128).transpose(2, 1, 0, 3)).astype(ml_dtypes.bfloat16)
    out = {f"hyCf{n}": tile_fwd(Cm), f"hySf{n}": tile_fwd(Sm), f"hyCi{n}": tile_inv(Cm), f"hySi{n}": tile_inv(Sm)}
    f32 = np.float32
    tt = np.linspace(0.0, 1.0, n, dtype=f32)[:, None]
    bands = np.linspace(1e-4, 15, 16, dtype=f32)
    a2 = (f32(2 * math.pi) * np.arange(n, dtype=f32) / f32(n))[:, None] * bands
    feats = np.concatenate([tt, np.cos(a2), -np.sin(a2)], axis=-1).astype(f32)
    out[f"hyfeat{n}"] = np.ascontiguousarray(feats.T)
    deltas = np.abs(np.linspace(math.log(1e-2) / 1.5, math.log(1e-2) / 0.3, 512, dtype=f32))
    out[f"hywin{n}"] = np.exp(-tt * deltas).astype(f32)
    return out


def emit_sin(P, src, src_key, dst, dst_key, tf, ti, sl):
    P.op("dve", lambda e: e.tensor_scalar(out=ti[sl], in0=src[sl], scalar1=1.0 / TWO_PI, scalar2=None, op0=ALU.mult), reads=[src_key], writes=[dst_key + "_ti"])
    P.op("dve", lambda e: e.tensor_copy(out=tf[sl], in_=ti[sl]), reads=[dst_key + "_ti"], writes=[dst_key + "_tf"])
    P.op("dve", lambda e: e.scalar_tensor_tensor(out=tf[sl], in0=tf[sl], scalar=-TWO_PI, in1=src[sl], op0=ALU.mult, op1=ALU.add), reads=[dst_key + "_tf", src_key], writes=[dst_key + "_tf"])
    P.op("dve", lambda e: e.tensor_scalar(out=tf[sl], in0=tf[sl], scalar1=-3.1415925, scalar2=3.1415925, op0=ALU.max, op1=ALU.min), reads=[dst_key + "_tf"], writes=[dst_key + "_tf"])
    P.op("act", lambda e: e.activation(out=dst[sl], in_=tf[sl], func=AF.Sin), reads=[dst_key + "_tf"], writes=[dst_key])


def stage_hyena(C, K, PS, l, n, row0):
    nc, P = C.nc, C.P
    nch = n // 128
    UHY = C.dram("UHY", [T, 1536])
    YHY = C.dram("YHY", [512, T], BF16)
    Cf = C.inp(f"hyCf{n}", [nch, 128, nch, 128], BF16)
    Sf = C.inp(f"hySf{n}", [nch, 128, nch, 128], BF16)
    Ci = C.inp(f"hyCi{n}", [nch, 128, nch, 128], BF16)
    Si = C.inp(f"hySi{n}", [nch, 128, nch, 128], BF16)
    featd = C.inp(f"hyfeat{n}", [33, n])
    wind = C.inp(f"hywin{n}", [n, 512])
    w1d = C.inp("hy_w1", [L, 33, 64]); w2d = C.inp("hy_w2", [L, 64, 64]); w3d = C.inp("hy_w3", [L, 64, 2048])
    pv = C.inp("hy_pv_t", [L, 64, 4])
    b3d = C.inp("hy_b3", [L, 2048])
    biasd = C.inp("hy_bias", [L, 2, 512])
    FA = [C.dram(f"hyFA{o}_{n}", [n, 512], BF16) for o in range(2)]
    FD = [C.dram(f"hyFD{o}_{n}", [n, 512], BF16) for o in range(2)]
    HR = [C.dram(f"hyHR{o}_{n}", [n, 512]) for o in range(2)]
    HI = [C.dram(f"hyHI{o}_{n}", [n, 512]) for o in range(2)]
    tg = f"hy{n}"
    with ExitStack() as es:
        ft_ = sbt(es, nc, tg + "feat", [33, n], F32)
        w1 = sbt(es, nc, tg + "w1", [33, 64], F32); w2 = sbt(es, nc, tg + "w2", [64, 64], F32); w3 = sbt(es, nc, tg + "w3", [64, 2048], F32)
        pvt = sbt(es, nc, tg + "pv", [64, 4], F32)
        b3 = sbt(es, nc, tg + "b3", [1, 2048], F32)
        on1 = sbt(es, nc, tg + "on1", [1, 128], F32)
        a1 = sbt(es, nc, tg + "a1", [64, 512], F32); h1 = sbt(es, nc, tg + "h1", [64, 512], F32); h2 = sbt(es, nc, tg + "h2", [64, 512], F32)
        tf = sbt(es, nc, tg + "tf", [64, 512], F32); ti = sbt(es, nc, tg + "ti", [64, 512], I32)
        win = [sbt(es, nc, tg + f"win{i}", [128, 512], F32) for i in range(2)]
        fw = [sbt(es, nc, tg + f"fw{i}", [128, 512], F32) for i in range(2)]
        bw = [sbt(es, nc, tg + f"bw{i}", [128, 512], F32) for i in range(2)]
        ao = [sbt(es, nc, tg + f"ao{i}", [128, 512], BF16) for i in range(2)]
        do = [sbt(es, nc, tg + f"do{i}", [128, 512], BF16) for i in range(2)]
        P.dma("sp", lambda e: e.dma_start(out=ft_[:], in_=featd[:, :]), writes=[tg + "feat"])
        P.dma("sp", lambda e: e.dma_start(out=w1[:], in_=w1d[l]), writes=[tg + "w"])
        P.dma("sp", lambda e: e.dma_start(out=w2[:], in_=w2d[l]), writes=[tg + "w"])
        P.dma("sp", lambda e: e.dma_start(out=w3[:], in_=w3d[l]), writes=[tg + "w"])
        P.dma("sp", lambda e: e.dma_start(out=pvt[:], in_=pv[l]), writes=[tg + "w"])
        P.dma("sp", lambda e: e.dma_start(out=b3[:], in_=b3d[l:l + 1, :]), writes=[tg + "w"])
        P.op("pool", lambda e: e.memset(on1[:], 1.0), writes=[tg + "on1"])
        P.barrier()
        for c5 in range(max(1, n // 512)):
            ln = min(512, n)
            p0 = c5 * 512
            sl = (slice(None), slice(0, ln))
            P.op("pe", lambda e, p0=p0, ln=ln: e.matmul(PS[0][:64, :ln], lhsT=w1[:, :], rhs=ft_[:, p0:p0 + ln], start=True, stop=True), reads=[tg + "feat"], writes=["ps0"])
            P.op("dve", lambda e, ln=ln: e.tensor_scalar(out=a1[:, :ln], in0=PS[0][:64, :ln], scalar1=pvt[:, 0:1], scalar2=pvt[:, 1:2], op0=ALU.add, op1=ALU.mult), reads=["ps0"], writes=[tg + "a1"])
            emit_sin(P, a1, tg + "a1", h1, tg + "h1", tf, ti, sl)
            P.op("pe", lambda e, ln=ln: e.matmul(PS[1][:64, :ln], lhsT=w2[:, :], rhs=h1[:, :ln], start=True, stop=True), reads=[tg + "h1"], writes=["ps1"])
            P.op("dve", lambda e, ln=ln: e.tensor_scalar(out=a1[:, :ln], in0=PS[1][:64, :ln], scalar1=pvt[:, 2:3], scalar2=pvt[:, 3:4], op0=ALU.add, op1=ALU.mult), reads=["ps1", tg + "h1_tf"], writes=[tg + "a1"])
            emit_sin(P, a1, tg + "a1", h2, tg + "h2", tf, ti, sl)
            for bl in range(ln // 128):
                blk = c5 * 4 + bl
                i = blk % 2
                P.dma("act", lambda e, i=i, blk=blk: e.dma_start(out=win[i][:], in_=wind[blk * 128:(blk + 1) * 128, :]), writes=[tg + f"win{i}"])
                for cc in range(4):
                    ps = PS[2 + cc]
                    P.op("pe", lambda e, ps=ps, bl=bl, cc=cc: e.matmul(ps[:, :], lhsT=h2[:, bl * 128:(bl + 1) * 128], rhs=w3[:, cc * 512:(cc + 1) * 512], start=True, stop=False), reads=[tg + "h2"], writes=[f"ps{2 + cc}"])
                    P.op("pe", lambda e, ps=ps, cc=cc: e.matmul(ps[:, :], lhsT=on1[:, :], rhs=b3[:, cc * 512:(cc + 1) * 512], start=False, stop=True), reads=[tg + "on1"], writes=[f"ps{2 + cc}"])
                for o in range(2):
                    P.op("dve", lambda e, i=i, o=o: e.tensor_tensor(out=fw[o][:], in0=PS[2 + 2 * o][:, :], in1=win[i][:], op=ALU.mult), reads=[f"ps{2 + 2 * o}", tg + f"win{i}"], writes=[tg + f"fw{o}"])
                    P.op("dve", lambda e, i=i, o=o: e.tensor_tensor(out=bw[o][:], in0=PS[3 + 2 * o][:, :], in1=win[i][:], op=ALU.mult), reads=[f"ps{3 + 2 * o}", tg + f"win{i}"], writes=[tg + f"bw{o}"])
                    if blk == 0:
                        P.op("dve", lambda e, o=o: e.memset(bw[o][0:1, :], 0.0), reads=[tg + f"bw{o}"], writes=[tg + f"bw{o}"])
                    P.op("pool", lambda e, o=o: e.tensor_tensor(out=ao[o][:], in0=fw[o][:], in1=bw[o][:], op=ALU.add), reads=[tg + f"fw{o}", tg + f"bw{o}"], writes=[tg + f"ao{o}"])
                    P.op("pool", lambda e, o=o: e.tensor_tensor(out=do[o][:], in0=fw[o][:], in1=bw[o][:], op=ALU.subtract), reads=[tg + f"fw{o}", tg + f"bw{o}"], writes=[tg + f"do{o}"])
                    P.dma("sp", lambda e, o=o, blk=blk: e.dma_start(out=FA[o][blk * 128:(blk + 1) * 128, :], in_=ao[o][:]), reads=[tg + f"ao{o}"], writes=[tg + "FA"])
                    P.dma("sp", lambda e, o=o, blk=blk: e.dma_start(out=FD[o][blk * 128:(blk + 1) * 128, :], in_=do[o][:]), reads=[tg + f"do{o}"], writes=[tg + "FD"])
        P.barrier()
    with ExitStack() as es:
        X = sbt(es, nc, tg + "X", [128, nch, 512], BF16)
        X2 = sbt(es, nc, tg + "X2", [128, nch, 512], BF16)
        cm = [sbt(es, nc, tg + f"cm{i}", [128, nch, 128], BF16) for i in range(2)]
        smm = [sbt(es, nc, tg + f"sm{i}", [128, nch, 128], BF16) for i in range(2)]
        ho = [sbt(es, nc, tg + f"ho{i}", [128, 512], F32) for i in range(4)]
        for o in range(2):
            for ch in range(nch):
                P.dma("sp", lambda e, o=o, ch=ch: e.dma_start(out=X[:, ch, :], in_=FA[o][ch * 128:(ch + 1) * 128, :]), reads=[tg + "FA"], writes=[tg + "X"])
                P.dma("sp" if Prog.DBG & 4 else "act", lambda e, o=o, ch=ch: e.dma_start(out=X2[:, ch, :], in_=FD[o][ch * 128:(ch + 1) * 128, :]), reads=[tg + "FD"], writes=[tg + "X2"])
            for ft in range(nch):
                i = ft % 2
                P.dma("sp", lambda e, i=i, ft=ft: e.dma_start(out=cm[i][:], in_=Cf[ft]), writes=[tg + f"cm{i}"])
                P.dma("sp" if Prog.DBG & 4 else "act", lambda e, i=i, ft=ft: e.dma_start(out=smm[i][:], in_=Sf[ft]), writes=[tg + f"sm{i}"])
                pr, pi_ = PS[2 * i], PS[2 * i + 1]
                for ch in range(nch):
                    P.op("pe", lambda e, pr=pr, i=i, ch=ch: e.matmul(pr[:, :], lhsT=cm[i][:, ch, :], rhs=X[:, ch, :], start=(ch == 0), stop=(ch == nch - 1)), reads=[tg + f"cm{i}", tg + "X"], writes=[f"ps{2 * i}"])
                for ch in range(nch):
                    P.op("pe", lambda e, pi_=pi_, i=i, ch=ch: e.matmul(pi_[:, :], lhsT=smm[i][:, ch, :], rhs=X2[:, ch, :], start=(ch == 0), stop=(ch == nch - 1)), reads=[tg + f"sm{i}", tg + "X2"], writes=[f"ps{2 * i + 1}"])
                if Prog.DBG & 1:
                    P.barrier()
                P.op("dve", lambda e, pr=pr, i=i: e.tensor_copy(out=ho[2 * i][:], in_=pr[:, :]), reads=[f"ps{2 * i}"], writes=[tg + f"ho{2 * i}"])
                P.op("dve", lambda e, pi_=pi_, i=i: e.tensor_copy(out=ho[2 * i + 1][:], in_=pi_[:, :]), reads=[f"ps{2 * i + 1}"], writes=[tg + f"ho{2 * i + 1}"])
                if Prog.DBG & 2:
                    P.barrier()
                P.dma("sp", lambda e, i=i, o=o, ft=ft: e.dma_start(out=HR[o][ft * 128:(ft + 1) * 128, :], in_=ho[2 * i][:]), reads=[tg + f"ho{2 * i}"], writes=[tg + "HR"])
                P.dma("sp", lambda e, i=i, o=o, ft=ft: e.dma_start(out=HI[o][ft * 128:(ft + 1) * 128, :], in_=ho[2 * i + 1][:]), reads=[tg + f"ho{2 * i + 1}"], writes=[tg + "HI"])
        P.barrier()
    with ExitStack() as es:
        cX = sbt(es, nc, tg + "cX", [128, nch, 512], BF16)
        Yr = sbt(es, nc, tg + "Yr", [128, nch, 512], BF16)
        Yi = sbt(es, nc, tg + "Yi", [128, nch, 512], BF16)
        ccm = [sbt(es, nc, tg + f"ccm{i}", [128, nch, 128], BF16) for i in range(2)]
        csm = [sbt(es, nc, tg + f"csm{i}", [128, nch, 128], BF16) for i in range(2)]
        hr = [sbt(es, nc, tg + f"hr{i}", [128, 512], F32) for i in range(2)]
        hi = [sbt(es, nc, tg + f"hi{i}", [128, 512], F32) for i in range(2)]
        tt = [sbt(es, nc, tg + f"t{i}", [128, 512], F32) for i in range(4)]
        ux = [sbt(es, nc, tg + f"ux{i}", [128, 1536], F32) for i in range(2)]
        bb = [sbt(es, nc, tg + f"bb{o}", [128, 512], F32) for o in range(2)]
        yo = sbt(es, nc, tg + "yo", [128, 512], F32)
        yst = [sbt(es, nc, tg + f"yst{i}", [128, 4, 128], BF16) for i in range(2)]
        for o in range(2):
            P.dma("sp", lambda e, o=o: e.dma_start(out=bb[o][:], in_=biasd[l, o:o + 1, :].partition_broadcast(128)), writes=[tg + f"bb{o}"])
        for ch in range(nch):
            P.dma("pool", lambda e, ch=ch: e.dma_start(out=cX[:, ch, :], in_=UHY[row0 + ch * 128:row0 + (ch + 1) * 128, 1024:1536]), reads=[f"UHY{c_}" for c_ in range(8, 12)], writes=[tg + f"cX{ch}"])
        for o in range(2):
            for ft in range(nch):
                i = ft % 2
                P.dma("sp", lambda e, i=i, ft=ft: e.dma_start(out=ccm[i][:], in_=Cf[ft]), writes=[tg + f"ccm{i}"])
                P.dma("act", lambda e, i=i, ft=ft: e.dma_start(out=csm[i][:], in_=Sf[ft]), writes=[tg + f"csm{i}"])
                P.dma("sp", lambda e, i=i, o=o, ft=ft: e.dma_start(out=hr[i][:], in_=HR[o][ft * 128:(ft + 1) * 128, :]), reads=[tg + "HR"], writes=[tg + f"hr{i}"])
                P.dma("act", lambda e, i=i, o=o, ft=ft: e.dma_start(out=hi[i][:], in_=HI[o][ft * 128:(ft + 1) * 128, :]), reads=[tg + "HI"], writes=[tg + f"hi{i}"])
                pr, pi_ = PS[2 * i], PS[2 * i + 1]
                for ch in range(nch):
                    P.op("pe", lambda e, pr=pr, i=i, ch=ch: e.matmul(pr[:, :], lhsT=ccm[i][:, ch, :], rhs=cX[:, ch, :], start=(ch == 0), stop=(ch == nch - 1)), reads=[tg + f"ccm{i}", tg + f"cX{ch}"], writes=[f"ps{2 * i}"])
                for ch in range(nch):
                    P.op("pe", lambda e, pi_=pi_, i=i, ch=ch: e.matmul(pi_[:, :], lhsT=csm[i][:, ch, :], rhs=cX[:, ch, :], start=(ch == 0), stop=(ch == nch - 1)), reads=[tg + f"csm{i}", tg + f"cX{ch}"], writes=[f"ps{2 * i + 1}"])
                P.op("dve", lambda e, pr=pr, i=i: e.tensor_tensor(out=tt[0][:], in0=pr[:, :], in1=hr[i][:], op=ALU.mult), reads=[f"ps{2 * i}", tg + f"hr{i}"], writes=[tg + "t0"])
                P.op("dve", lambda e, pi_=pi_, i=i: e.tensor_tensor(out=tt[1][:], in0=pi_[:, :], in1=hi[i][:], op=ALU.mult), reads=[f"ps{2 * i + 1}", tg + f"hi{i}"], writes=[tg + "t1"])
                P.op("dve", lambda e, pr=pr, i=i: e.tensor_tensor(out=tt[2][:], in0=pr[:, :], in1=hi[i][:], op=ALU.mult), reads=[f"ps{2 * i}", tg + f"hi{i}"], writes=[tg + "t2"])
                P.op("dve", lambda e, pi_=pi_, i=i: e.tensor_tensor(out=tt[3][:], in0=pi_[:, :], in1=hr[i][:], op=ALU.mult), reads=[f"ps{2 * i + 1}", tg + f"hr{i}"], writes=[tg + "t3"])
                P.op("pool", lambda e, ft=ft: e.tensor_tensor(out=Yr[:, ft, :], in0=tt[0][:], in1=tt[1][:], op=ALU.subtract), reads=[tg + "t0", tg + "t1"], writes=[tg + f"Yr{ft}"])
                P.op("pool", lambda e, ft=ft: e.tensor_tensor(out=Yi[:, ft, :], in0=tt[2][:], in1=tt[3][:], op=ALU.add), reads=[tg + "t2", tg + "t3"], writes=[tg + f"Yi{ft}"])
            for tb in range(nch):
                i = tb % 2
                P.dma("sp", lambda e, i=i, tb=tb: e.dma_start(out=ccm[i][:], in_=Ci[tb]), writes=[tg + f"ccm{i}"])
                P.dma("act", lambda e, i=i, tb=tb: e.dma_start(out=csm[i][:], in_=Si[tb]), writes=[tg + f"csm{i}"])
                P.dma("sp", lambda e, i=i, tb=tb: e.dma_start(out=ux[i][:], in_=UHY[row0 + tb * 128:row0 + (tb + 1) * 128, :]), reads=[f"UHY{c_}" for c_ in range(12)], writes=[tg + f"ux{i}"])
                ps = PS[4 + i]
                pk = f"ps{4 + i}"
                for ch in range(nch):
                    P.op("pe", lambda e, ps=ps, i=i, ch=ch: e.matmul(ps[:, :], lhsT=ccm[i][:, ch, :], rhs=Yr[:, ch, :], start=(ch == 0), stop=False), reads=[tg + f"ccm{i}", tg + f"Yr{ch}"], writes=[pk])
                for ch in range(nch):
                    P.op("pe", lambda e, ps=ps, i=i, ch=ch: e.matmul(ps[:, :], lhsT=csm[i][:, ch, :], rhs=Yi[:, ch, :], start=False, stop=(ch == nch - 1)), reads=[tg + f"csm{i}", tg + f"Yi{ch}"], writes=[pk])
                if o == 0:
                    P.op("pool", lambda e, i=i: e.tensor_tensor(out=tt[0][:], in0=ux[i][:, 1024:1536], in1=bb[0][:], op=ALU.mult), reads=[tg + f"ux{i}", tg + "bb0"], writes=[tg + "t0"])
                    P.op("dve", lambda e, ps=ps: e.tensor_tensor(out=tt[0][:], in0=ps[:, :], in1=tt[0][:], op=ALU.add), reads=[pk, tg + "t0"], writes=[tg + "t0"])
                    P.op("pool", lambda e, i=i, tb=tb: e.tensor_tensor(out=cX[:, tb, :], in0=tt[0][:], in1=ux[i][:, 0:512], op=ALU.mult), reads=[tg + "t0", tg + f"ux{i}"] + [tg + f"Yr{c_}" for c_ in range(nch)], writes=[tg + f"cX{tb}"])
                else:
                    P.op("pool", lambda e, tb=tb: e.tensor_tensor(out=tt[1][:], in0=cX[:, tb, :], in1=bb[1][:], op=ALU.mult), reads=[tg + f"cX{tb}", tg + "bb1"], writes=[tg + "t1"])
                    P.op("dve", lambda e, ps=ps: e.tensor_tensor(out=tt[1][:], in0=ps[:, :], in1=tt[1][:], op=ALU.add), reads=[pk, tg + "t1"], writes=[tg + "t1"])
                    P.op("pool", lambda e, i=i: e.tensor_tensor(out=yo[:], in0=tt[1][:], in1=ux[i][:, 512:1024], op=ALU.mult), reads=[tg + "t1", tg + f"ux{i}"], writes=[tg + "yo"])
                    pt = PS[6 + i]
                    for k in range(4):
                        P.op("pe", lambda e, pt=pt, k=k: e.transpose(pt[:, k * 128:(k + 1) * 128], yo[:, k * 128:(k + 1) * 128], K["ident"][:]), reads=[tg + "yo", "ident"], writes=[f"ps{6 + i}"])
                    P.op("act", lambda e, pt=pt, i=i: e.activation(out=yst[i][:], in_=pt[:, :].rearrange("p (k t) -> p k t", t=128), func=AF.Copy), reads=[f"ps{6 + i}"], writes=[tg + f"yst{i}"])
                    P.dma("sp", lambda e, i=i, tb=tb: e.dma_start(out=YHY.rearrange("(k p) t -> p k t", p=128)[:, :, row0 + tb * 128:row0 + (tb + 1) * 128], in_=yst[i][:]), reads=[tg + f"yst{i}"], writes=["Y0"])
        P.barrier()


def stage_zero_yrw(C, K, PS):
    nc, P = C.nc, C.P
    YRW = C.dram("YRW", [512, T], BF16)
    with ExitStack() as es:
        z = sbt(es, nc, "zrw", [128, T], BF16)
        P.op("pool", lambda e: e.memset(z[:], 0.0), writes=["zrw"])
        for k in range(4):
            P.dma("sp", lambda e, k=k: e.dma_start(out=YRW[k * 128:(k + 1) * 128, :], in_=z[:]), reads=["zrw"], writes=["Y1"])
        P.barrier()


def full_stages():
    st = []
    for l in range(L):
        need_ctx = l < L - 1
        st.append(lambda C, K, PS, l=l: stage_ada(C, K, PS, l))
        if l == 0:
            st.append(stage_resid_init)
        st.append(lambda C, K, PS, l=l, nctx=need_ctx: stage_norm1_inproj(C, K, PS, l, nctx))
        st.append(lambda C, K, PS, l=l: stage_hyena(C, K, PS, l, TL, TC))
        if need_ctx:
            st.append(lambda C, K, PS, l=l: stage_hyena(C, K, PS, l, TC, 0))
        st.append(lambda C, K, PS, l=l, nctx=need_ctx: stage_s5(C, K, PS, l, nctx))
        st.append(lambda C, K, PS, l=l: stage_rw_prep(C, K, PS, l))
        st.append(lambda C, K, PS, l=l, nctx=need_ctx: stage_rw_scan(C, K, PS, l, nctx))
        st.append(lambda C, K, PS, l=l, nctx=need_ctx: stage_rw_out(C, K, PS, l, nctx))
        st.append(lambda C, K, PS, l=l, nctx=need_ctx: stage_merge(C, K, PS, l, nctx))
        st.append(lambda C, K, PS, l=l, nctx=need_ctx: stage_moe(C, K, PS, l, nctx, l == L - 1))
    return st


def kernel(**inputs):
    inp = {k: np.asarray(v) for k, v in inputs.items()}
    B = inp["x"].shape[0]
    nc, C = build_program(full_stages())
    S = prep_shared(inp)
    in_maps = []
    for b in range(B):
        allin = {**S, **prep_core(inp, b)}
        in_maps.append({k: allin[k] for k in C.ext_in})
    res = run_bass_kernel_spmd(nc, in_maps, core_ids=list(range(B)))
    out = np.stack([np.ascontiguousarray(np.asarray(r["outT"]).T) for r in res.results], axis=0)
    return out.astype(np.float32)


NS = 32
SC = 256


def rw_consts(C, K, es):
    nc, P = C.nc, C.P
    for nm, shp in (("Jm", [128, 128]), ("BO", [128, 128]), ("mask16", [16, 512]), ("maskhh", [128, 2]), ("rwc", [128, 4])):
        K[nm] = sbt(es, nc, nm, shp, F32)
        src = C.inp(nm + "_in", shp)
        P.dma("sp", lambda e, nm=nm, src=src: e.dma_start(out=K[nm][:], in_=src[:, :]), writes=[nm])


def stage_rw_prep(C, K, PS, l):
    nc, P = C.nc, C.P
    URW = C.dram("URW", [1792, T])
    Uv = URW.rearrange("(j p) t -> p j t", p=128)
    names = ("AKK", "BD0", "BD1", "KD0", "KD1", "WD0", "WD1", "GG", "BON")
    DR = {n: C.dram("rw" + n, [512, T]).rearrange("(j p) t -> p j t", p=128) for n in names}
    vec = C.inp("rw_vec_t", [L, 128, 4, 5])
    w0a0 = C.inp("rw_w0a0_t", [L, 128, 4, 4])
    wupd = C.inp("rw_w_up", [L, 2, 64, 512])
    aupd = C.inp("rw_a_up", [L, 2, 64, 512])
    gupd = C.inp("rw_g_up", [L, 128, 512])
    with ExitStack() as es:
        vt = sbt(es, nc, "rwvec", [128, 4, 5], F32)
        wa = sbt(es, nc, "rww0a0", [128, 4, 4], F32)
        nw0 = sbt(es, nc, "rwnw0", [128, 4, 2], F32)
        wup = sbt(es, nc, "rwwup", [64, 2, 512], F32)
        aup = sbt(es, nc, "rwaup", [128, 2, 512], F32)
        gup = sbt(es, nc, "rwgup", [128, 512], F32)
        P.dma("sp", lambda e: e.dma_start(out=vt[:], in_=vec[l]), writes=["rwvec"])
        P.dma("sp", lambda e: e.dma_start(out=wa[:], in_=w0a0[l]), writes=["rww0a0"])
        for d in range(2):
            P.dma("sp", lambda e, d=d: e.dma_start(out=wup[:, d, :], in_=wupd[l, d]), writes=["rwwup"])
            P.dma("sp", lambda e, d=d: e.dma_start(out=aup[64:128, d, :], in_=aupd[l, d]), writes=["rwaup"])
        P.dma("sp", lambda e: e.dma_start(out=gup[:], in_=gupd[l]), writes=["rwgup"])
        P.op("dve", lambda e: e.tensor_scalar(out=nw0[:], in0=wa[:, :, 0:2], scalar1=-1.0, scalar2=None, op0=ALU.mult), reads=["rww0a0"], writes=["rwnw0"])
        xwa = sbt(es, nc, "rwxwa", [128, 512], F32)
        txw = sbt(es, nc, "rwtxw", [64, 512], F32)
        xg = sbt(es, nc, "rwxg", [128, 512], F32)
        rr = sbt(es, nc, "rwr", [128, 512], F32)
        kk_ = sbt(es, nc, "rwk", [128, 512], F32)
        kk0 = sbt(es, nc, "rwkk0", [128, 512], F32)
        sq = sbt(es, nc, "rwsq", [128, 512], F32)
        kkn = sbt(es, nc, "rwkkn", [128, 512], F32)
        akk = sbt(es, nc, "rwakk", [128, 512], F32)
        gg = sbt(es, nc, "rwgg", [128, 512], F32)
        e1 = sbt(es, nc, "rwe1", [128, 512], F32)
        dec = [sbt(es, nc, f"rwdec{d}", [128, 512], F32) for d in range(2)]
        aa = sbt(es, nc, "rwaa", [128, 512], F32)
        tt_ = sbt(es, nc, "rwtt", [128, 512], F32)
        kd = [sbt(es, nc, f"rwkd{d}", [128, 512], F32) for d in range(2)]
        bd = [sbt(es, nc, f"rwbd{d}", [128, 512], F32) for d in range(2)]
        bon = sbt(es, nc, "rwbon", [128, 512], F32)
        for ci, (c0, ln) in enumerate(CHUNKS):
            P.dma("sp", lambda e, c0=c0, ln=ln: e.dma_start(out=xwa[:, :ln], in_=Uv[:, 12, c0:c0 + ln]), reads=["URW12"], writes=["rwxwa"])
            P.dma("sp", lambda e, c0=c0, ln=ln: e.dma_start(out=xg[:, :ln], in_=Uv[:, 13, c0:c0 + ln]), reads=["URW13"], writes=["rwxg"])
            P.op("act", lambda e, ln=ln: e.activation(out=txw[:, :ln], in_=xwa[0:64, :ln], func=AF.Tanh), reads=["rwxwa"], writes=["rwtxw"])
            P.op("act", lambda e, ln=ln: e.activation(out=xg[:, :ln], in_=xg[:, :ln], func=AF.Sigmoid), reads=["rwxg"], writes=["rwxg"])
            for j in range(4):
                js = slice(j * 128, (j + 1) * 128)
                P.dma("sp", lambda e, j=j, c0=c0, ln=ln: e.dma_start(out=rr[:, :ln], in_=Uv[:, j, c0:c0 + ln]), reads=[f"URW{j}"], writes=["rwr"])
                P.dma("act", lambda e, j=j, c0=c0, ln=ln: e.dma_start(out=kk_[:, :ln], in_=Uv[:, 4 + j, c0:c0 + ln]), reads=[f"URW{4 + j}"], writes=["rwk"])
                P.op("pe", lambda e, js=js, ln=ln: e.matmul(PS[0][:, :ln], lhsT=gup[:, js], rhs=xg[:, :ln], start=True, stop=True), reads=["rwgup", "rwxg"], writes=["ps0"])
                P.op("act", lambda e, ln=ln: e.activation(out=gg[:, :ln], in_=PS[0][:, :ln], func=AF.Copy), reads=["ps0"], writes=["rwgg"])
                P.dma("sp", lambda e, j=j, c0=c0, ln=ln: e.dma_start(out=DR["GG"][:, j, c0:c0 + ln], in_=gg[:, :ln]), reads=["rwgg"], writes=["rwGG"])
                P.op("dve", lambda e, j=j, ln=ln: e.tensor_scalar(out=kk0[:, :ln], in0=kk_[:, :ln], scalar1=vt[:, j, 0:1], scalar2=None, op0=ALU.mult), reads=["rwk", "rwvec"], writes=["rwkk0"])
                P.op("act", lambda e, ln=ln: e.activation(out=sq[:, :ln], in_=kk0[:, :ln], func=AF.Square), reads=["rwkk0"], writes=["rwsq"])
                P.op("pe", lambda e, ln=ln: e.matmul(PS[1][:, :ln], lhsT=K["BO"][:], rhs=sq[:, :ln], start=True, stop=True), reads=["BO", "rwsq"], writes=["ps1"])
                P.op("dve", lambda e, ln=ln: e.tensor_scalar(out=sq[:, :ln], in0=PS[1][:, :ln], scalar1=1e-24, scalar2=None, op0=ALU.max), reads=["ps1"], writes=["rwsq"])
                P.op("act", lambda e, ln=ln: e.activation(out=sq[:, :ln], in_=sq[:, :ln], func=AF.Sqrt), reads=["rwsq"], writes=["rwsq"])
                P.op("dve", lambda e, ln=ln: e.reciprocal(out=sq[:, :ln], in_=sq[:, :ln]), reads=["rwsq"], writes=["rwsq"])
                P.op("dve", lambda e, ln=ln: e.tensor_tensor(out=kkn[:, :ln], in0=kk0[:, :ln], in1=sq[:, :ln], op=ALU.mult), reads=["rwkk0", "rwsq"], writes=["rwkkn"])
                P.op("pool", lambda e, ln=ln: e.tensor_scalar(out=akk[:, :ln], in0=kkn[:, :ln], scalar1=-1.0, scalar2=None, op0=ALU.mult), reads=["rwkkn"], writes=["rwakk"])
                P.dma("sp", lambda e, j=j, c0=c0, ln=ln: e.dma_start(out=DR["AKK"][:, j, c0:c0 + ln], in_=akk[:, :ln]), reads=["rwakk"], writes=["rwAKK"])
                for d in range(2):
                    P.op("pe", lambda e, d=d, js=js, ln=ln: e.matmul(PS[2][:, :ln], lhsT=wup[:, d, js], rhs=txw[:, :ln], start=True, stop=True), reads=["rwwup", "rwtxw"], writes=["ps2"])
                    P.op("act", lambda e, d=d, j=j, ln=ln: e.activation(out=e1[:, :ln], in_=PS[2][:, :ln], func=AF.Exp, bias=nw0[:, j, d:d + 1], scale=-1.0), reads=["ps2", "rwnw0"], writes=["rwe1"])
                    P.op("act", lambda e, ln=ln: e.activation(out=e1[:, :ln], in_=e1[:, :ln], func=AF.Ln, bias=K["rwc"][:, 0:1], scale=1.0), reads=["rwe1", "rwc"], writes=["rwe1"])
                    P.op("act", lambda e, ln=ln: e.activation(out=e1[:, :ln], in_=e1[:, :ln], func=AF.Exp, bias=K["rwc"][:, 1:2], scale=-1.0), reads=["rwe1", "rwc"], writes=["rwe1"])
                    P.op("act", lambda e, d=d, ln=ln: e.activation(out=dec[d][:, :ln], in_=e1[:, :ln], func=AF.Exp, scale=-1.0), reads=["rwe1"], writes=[f"rwdec{d}"])
                    P.dma("sp", lambda e, d=d, j=j, c0=c0, ln=ln: e.dma_start(out=DR[f"WD{d}"][:, j, c0:c0 + ln], in_=dec[d][:, :ln]), reads=[f"rwdec{d}"], writes=[f"rwWD{d}"])
                    P.op("pe", lambda e, d=d, js=js, ln=ln: e.matmul(PS[3][:, :ln], lhsT=aup[64:128, d, js], rhs=xwa[64:128, :ln], start=True, stop=True), reads=["rwaup", "rwxwa"], writes=["ps3"])
                    P.op("act", lambda e, d=d, j=j, ln=ln: e.activation(out=aa[:, :ln], in_=PS[3][:, :ln], func=AF.Sigmoid, bias=wa[:, j, 2 + d:3 + d], scale=1.0), reads=["ps3", "rww0a0"], writes=["rwaa"])
                    P.op("dve", lambda e, j=j, ln=ln: e.tensor_scalar(out=tt_[:, :ln], in0=aa[:, :ln], scalar1=-1.0, scalar2=vt[:, j, 1:2], op0=ALU.add, op1=ALU.mult), reads=["rwaa", "rwvec"], writes=["rwtt"])
                    P.op("dve", lambda e, d=d, ln=ln: e.scalar_tensor_tensor(out=kd[d][:, :ln], in0=tt_[:, :ln], scalar=1.0, in1=kk_[:, :ln], op0=ALU.add, op1=ALU.mult), reads=["rwtt", "rwk"], writes=[f"rwkd{d}"])
                    P.dma("sp", lambda e, d=d, j=j, c0=c0, ln=ln: e.dma_start(out=DR[f"KD{d}"][:, j, c0:c0 + ln], in_=kd[d][:, :ln]), reads=[f"rwkd{d}"], writes=[f"rwKD{d}"])
                    P.op("pool", lambda e, d=d, ln=ln: e.tensor_tensor(out=bd[d][:, :ln], in0=kkn[:, :ln], in1=aa[:, :ln], op=ALU.mult), reads=["rwkkn", "rwaa"], writes=[f"rwbd{d}"])
                    P.dma("act", lambda e, d=d, j=j, c0=c0, ln=ln: e.dma_start(out=DR[f"BD{d}"][:, j, c0:c0 + ln], in_=bd[d][:, :ln]), reads=[f"rwbd{d}"], writes=[f"rwBD{d}"])
                P.op("pool", lambda e, ln=ln: e.tensor_tensor(out=tt_[:, :ln], in0=kd[0][:, :ln], in1=kd[1][:, :ln], op=ALU.add), reads=["rwkd0", "rwkd1", "rwtt"], writes=["rwtt"])
                P.op("dve", lambda e, j=j, ln=ln: e.scalar_tensor_tensor(out=tt_[:, :ln], in0=rr[:, :ln], scalar=vt[:, j, 2:3], in1=tt_[:, :ln], op0=ALU.mult, op1=ALU.mult), reads=["rwr", "rwvec", "rwtt"], writes=["rwtt"])
                P.op("pe", lambda e, ln=ln: e.matmul(PS[4][:, :ln], lhsT=K["BO"][:], rhs=tt_[:, :ln], start=True, stop=True), reads=["BO", "rwtt"], writes=["ps4"])
                P.op("act", lambda e, ln=ln: e.activation(out=bon[:, :ln], in_=PS[4][:, :ln], func=AF.Copy), reads=["ps4"], writes=["rwbon"])
                P.dma("sp", lambda e, j=j, c0=c0, ln=ln: e.dma_start(out=DR["BON"][:, j, c0:c0 + ln], in_=bon[:, :ln]), reads=["rwbon"], writes=["rwBON"])
        P.barrier()


def stage_rw_scan(C, K, PS, l, need_ctx):
    nc, P = C.nc, C.P
    URW = C.dram("URW", [1792, T])
    Uv = URW.rearrange("(j p) t -> p j t", p=128)
    DR = {n: C.dram("rw" + n, [512, T]).rearrange("(j p) t -> p j t", p=128) for n in ("AKK", "BD0", "BD1", "KD0", "KD1", "WD0", "WD1")}
    VT = [C.dram("VTM", [T, 512]), C.dram("VTMR", [T, 512])]
    YD = [C.dram("rwYD0", [T, 512]), C.dram("rwYDR", [T, 512])]
    with ExitStack() as es:
        sct = {(d, nm): sbt(es, nc, f"sc{nm}{d}", [128, 4, SC], F32) for d in range(2) for nm in ("A", "R", "B", "K", "W")}
        Abd = sbt(es, nc, "Abd", [128, NS, 16], F32)
        Rbd = sbt(es, nc, "Rbd", [128, NS, 16], F32)
        BKbd = sbt(es, nc, "BKbd", [128, NS, 32], F32)
        Wt = sbt(es, nc, "Wt", [128, NS, 8], F32)
        LT = sbt(es, nc, "LT", [32, NS, 128], F32)
        RH = sbt(es, nc, "RH", [32, NS, 512], F32)
        MA = sbt(es, nc, "MA", [128, 512], F32)
        MB = sbt(es, nc, "MB", [128, 512], F32)
        ym = [sbt(es, nc, f"ym{i}", [16, 512], F32) for i in range(2)]
        Ys = sbt(es, nc, "Ysteps", [16, NS, 64], F32)
        P.op("pool", lambda e: e.memset(RH[:], 0.0), writes=["RH"])
        P.op("pool", lambda e: e.memset(MA[:], 0.0), writes=["MA"])
        srcs = {"A": lambda d: DR["AKK"], "R": lambda d: Uv, "B": lambda d: DR[f"BD{d}"], "K": lambda d: DR[f"KD{d}"], "W": lambda d: DR[f"WD{d}"]}
        skeys = {"A": lambda d: "rwAKK", "R": lambda d: None, "B": lambda d: f"rwBD{d}", "K": lambda d: f"rwKD{d}", "W": lambda d: f"rwWD{d}"}
        step = 0
        for s0sc in range(0, T, SC):
            n0 = [s0sc, (256 - s0sc - SC) if s0sc < 256 else (4608 - s0sc - SC)]
            for d in range(2):
                for nm in ("A", "R", "B", "K", "W"):
                    src = srcs[nm](d)
                    rk = [skeys[nm](d)] if skeys[nm](d) else [f"URW{j}" for j in range(4)]
                    P.dma("sp" if d == 0 else "act", lambda e, d=d, nm=nm, src=src, nd=n0[d]: e.dma_start(out=sct[(d, nm)][:], in_=src[:, 0:4, nd:nd + SC]),
                          reads=rk, writes=[f"sc{nm}{d}"])
            for i0 in range(0, SC, NS):
                s0 = s0sc + i0
                need_y = need_ctx or s0 >= 256

                def view(d, nm, j):
                    t_ = sct[(d, nm)]
                    if d == 0:
                        return t_[:, j, i0:i0 + NS]
                    return t_[:, j, SC - i0 - NS:SC - i0][:, ::-1]
                for d in range(2):
                    for j in range(4):
                        for hh in range(2):
                            col = d * 8 + j * 2 + hh
                            mk = K["maskhh"][:, hh:hh + 1]
                            P.op("pool", lambda e, col=col, mk=mk, vw=view(d, "A", j): e.tensor_scalar(out=Abd[:, :, col], in0=vw, scalar1=mk, scalar2=None, op0=ALU.mult), reads=[f"scA{d}", "maskhh"], writes=["Abd"])
                            P.op("pool", lambda e, col=col, mk=mk, vw=view(d, "R", j): e.tensor_scalar(out=Rbd[:, :, col], in0=vw, scalar1=mk, scalar2=None, op0=ALU.mult), reads=[f"scR{d}", "maskhh"], writes=["Rbd"])
                            P.op("pool", lambda e, col=col, mk=mk, vw=view(d, "B", j): e.tensor_scalar(out=BKbd[:, :, col], in0=vw, scalar1=mk, scalar2=None, op0=ALU.mult), reads=[f"scB{d}", "maskhh"], writes=["BKbd"])
                            P.op("pool", lambda e, col=col, mk=mk, vw=view(d, "K", j): e.tensor_scalar(out=BKbd[:, :, 16 + col], in0=vw, scalar1=mk, scalar2=None, op0=ALU.mult), reads=[f"scK{d}", "maskhh"], writes=["BKbd"])
                        P.op("pool", lambda e, d=d, j=j, vw=view(d, "W", j): e.tensor_copy(out=Wt[:, :, d * 4 + j], in_=vw), reads=[f"scW{d}"], writes=["Wt"])
                for d in range(2):
                    r0 = s0 if d == 0 else ((4096 + s0) if s0 < 256 else (s0 - 256))
                    for j in range(4):
                        P.dma("sp" if d == 0 else "act", lambda e, d=d, j=j, r0=r0: e.dma_start(
                            out=RH[16 + d * 8 + 2 * j:16 + d * 8 + 2 * j + 2, :, d * 256 + j * 64:d * 256 + (j + 1) * 64],
                            in_=VT[d][r0:r0 + NS, 2 * j * 64:(2 * j + 2) * 64].rearrange("t (h v) -> h t v", h=2)),
                            reads=[f"VTM{j}" if d == 0 else f"VTMR{j}"], writes=["RHv"])
                for g in range(NS // 4):
                    pt = PS[g % 2]
                    for q in range(4):
                        s = g * 4 + q
                        P.op("pe", lambda e, pt=pt, q=q, s=s: e.transpose(pt[:32, q * 128:(q + 1) * 128], BKbd[:, s, :], K["ident"][:]), reads=["BKbd", "ident"], writes=[f"ps{g % 2}"])
                    P.op("act", lambda e, pt=pt, g=g: e.activation(out=LT[:, g * 4:(g + 1) * 4, :], in_=pt[:32, :].rearrange("p (q c) -> p q c", c=128), func=AF.Copy), reads=[f"ps{g % 2}"], writes=["LT"])
                for s in range(NS):
                    pu, pm, py = PS[2 + step % 2], PS[4 + step % 2], PS[6 + step % 2]
                    ku, km, ky = f"ps{2 + step % 2}", f"ps{4 + step % 2}", f"ps{6 + step % 2}"
                    P.op("pe", lambda e, pu=pu, s=s: e.matmul(pu[:16, :], lhsT=Abd[:, s, :], rhs=MA[:, :], start=True, stop=True), reads=["Abd", "MA"], writes=[ku])
                    P.op("pool", lambda e, s=s: e.tensor_tensor(out=MB[:, :].rearrange("p (g v) -> p g v", v=64), in0=MA[:, :].rearrange("p (g v) -> p g v", v=64),
                                                                  in1=Wt[:, s, :].unsqueeze(2).broadcast_to([128, 8, 64]), op=ALU.mult), reads=["MA", "Wt"], writes=["MB"])
                    P.op("dve", lambda e, pu=pu, s=s: e.tensor_tensor(out=RH[0:16, s, :], in0=pu[:16, :], in1=K["mask16"][:], op=ALU.mult), reads=[ku, "mask16"], writes=["RHu"])
                    P.op("pe", lambda e, pm=pm, s=s: e.matmul(pm[:, :], lhsT=LT[:, s, :], rhs=RH[:, s, :], start=True, stop=True), reads=["LT", "RHu", "RHv", "RH"], writes=[km])
                    P.op("dve", lambda e, pm=pm: e.tensor_tensor(out=MA[:, :], in0=MB[:, :], in1=pm[:, :], op=ALU.add), reads=["MB", km], writes=["MA"])
                    if step == 0 and "dbgMA" in C.dump:
                        for nm, src_, shp in (("dbgMA", MA[:, :], [128, 512]), ("dbgLT", LT[:, 0, :], [32, 128]), ("dbgRH", RH[:, 0, :], [32, 512]), ("dbgR", Rbd[:, 0, :], [128, 16]), ("dbgBK", BKbd[:, 0, :], [128, 32])):
                            dd_ = C.dram(nm, shp)
                            P.dma("sp", lambda e, dd_=dd_, src_=src_: e.dma_start(out=dd_[:, :], in_=src_), reads=["MA", "LT", "RHu", "RHv", "Rbd", "BKbd"], writes=[nm])
                    if need_y:
                        yi = step % 2
                        P.op("pe", lambda e, py=py, s=s: e.matmul(py[:16, :], lhsT=Rbd[:, s, :], rhs=MA[:, :], start=True, stop=True), reads=["Rbd", "MA"], writes=[ky])
                        P.op("dve", lambda e, py=py, yi=yi: e.tensor_tensor(out=ym[yi][:], in0=py[:16, :], in1=K["mask16"][:], op=ALU.mult), reads=[ky, "mask16"], writes=[f"ym{yi}"])
                        P.op("pool", lambda e, yi=yi: e.tensor_tensor(out=ym[yi][:, 0:256], in0=ym[yi][:, 0:256], in1=ym[yi][:, 256:512], op=ALU.add), reads=[f"ym{yi}"], writes=[f"ym{yi}"])
                        P.op("pool", lambda e, yi=yi: e.tensor_tensor(out=ym[yi][:, 0:128], in0=ym[yi][:, 0:128], in1=ym[yi][:, 128:256], op=ALU.add), reads=[f"ym{yi}"], writes=[f"ym{yi}"])
                        P.op("pool", lambda e, yi=yi, s=s: e.tensor_tensor(out=Ys[:, s, :], in0=ym[yi][:, 0:64], in1=ym[yi][:, 64:128], op=ALU.add), reads=[f"ym{yi}"], writes=["Ysteps"])
                    step += 1
                if need_y:
                    for d in range(2):
                        P.dma("sp", lambda e, d=d, s0=s0: e.dma_start(out=YD[d][s0:s0 + NS, :].rearrange("t (h v) -> h t v", h=8), in_=Ys[d * 8:(d + 1) * 8, :, :]), reads=["Ysteps"], writes=[f"rwYD{d}"])
        P.barrier()


def stage_rw_out(C, K, PS, l, need_ctx):
    nc, P = C.nc, C.P
    URW = C.dram("URW", [1792, T])
    Uv = URW.rearrange("(j p) t -> p j t", p=128)
    DR = {n: C.dram("rw" + n, [512, T]).rearrange("(j p) t -> p j t", p=128) for n in ("GG", "BON")}
    YD = [C.dram("rwYD0", [T, 512]), C.dram("rwYDR", [T, 512])]
    YRW = C.dram("YRW", [512, T], BF16).rearrange("(j p) t -> p j t", p=128)
    vec = C.inp("rw_vec_t", [L, 128, 4, 5])
    with ExitStack() as es:
        vt = sbt(es, nc, "rovec", [128, 4, 5], F32)
        P.dma("sp", lambda e: e.dma_start(out=vt[:], in_=vec[l]), writes=["rovec"])
        y0 = [sbt(es, nc, f"roy0_{i}", [128, 512], F32) for i in range(2)]
        y1 = [sbt(es, nc, f"roy1_{i}", [128, 512], F32) for i in range(2)]
        bon = sbt(es, nc, "robon", [128, 512], F32)
        vv = sbt(es, nc, "rov", [128, 512], F32)
        gg = sbt(es, nc, "rog", [128, 512], F32)
        yy = sbt(es, nc, "royy", [128, 512], F32)
        yc = sbt(es, nc, "royc", [128, 512], F32)
        sq = sbt(es, nc, "rosq", [128, 512], F32)
        rs = sbt(es, nc, "rors", [128, 512], F32)
        ob = [sbt(es, nc, f"roob{i}", [128, 512], BF16) for i in range(2)]
        for ci, (c0, ln) in enumerate(CHUNKS):
            if ci == 0 and not need_ctx:
                continue
            for bi in range(ln // 128):
                t0 = c0 + bi * 128
                sb = (255 - t0 - 127) if t0 < 256 else (4607 - t0 - 127)
                i = bi % 2
                P.dma("sp", lambda e, i=i, t0=t0: e.dma_start(out=y0[i][:], in_=YD[0][t0:t0 + 128, :]), reads=["rwYD0"], writes=[f"roy0_{i}"])
                P.dma("act", lambda e, i=i, sb=sb: e.dma_start(out=y1[i][:], in_=YD[1][sb:sb + 128, :]), reads=["rwYD1"], writes=[f"roy1_{i}"])
                for j in range(4):
                    js = slice(j * 128, (j + 1) * 128)
                    P.op("pe", lambda e, i=i, j=j, js=js, bi=bi: e.matmul(PS[j][:, bi * 128:(bi + 1) * 128], lhsT=y0[i][:, js], rhs=K["ident"][:], start=True, stop=False), reads=[f"roy0_{i}", "ident"], writes=[f"ps{j}"])
                    P.op("pe", lambda e, i=i, j=j, js=js, bi=bi: e.matmul(PS[j][:, bi * 128:(bi + 1) * 128], lhsT=y1[i][:, js], rhs=K["Jm"][:], start=False, stop=True), reads=[f"roy1_{i}", "Jm"], writes=[f"ps{j}"])
            for j in range(4):
                P.dma("sp", lambda e, j=j, c0=c0, ln=ln: e.dma_start(out=bon[:, :ln], in_=DR["BON"][:, j, c0:c0 + ln]), reads=["rwBON"], writes=["robon"])
                P.dma("act", lambda e, j=j, c0=c0, ln=ln: e.dma_start(out=vv[:, :ln], in_=Uv[:, 8 + j, c0:c0 + ln]), reads=[f"URW{8 + j}"], writes=["rov"])
                P.dma("sp", lambda e, j=j, c0=c0, ln=ln: e.dma_start(out=gg[:, :ln], in_=DR["GG"][:, j, c0:c0 + ln]), reads=["rwGG"], writes=["rog"])
                P.op("pool", lambda e, ln=ln: e.tensor_tensor(out=bon[:, :ln], in0=bon[:, :ln], in1=vv[:, :ln], op=ALU.mult), reads=["robon", "rov"], writes=["robon"])
                P.op("dve", lambda e, j=j, ln=ln: e.tensor_tensor(out=yy[:, :ln], in0=PS[j][:, :ln], in1=bon[:, :ln], op=ALU.add), reads=[f"ps{j}", "robon"], writes=["royy"])
                P.op("pe", lambda e, ln=ln: e.matmul(PS[4][:, :ln], lhsT=K["BO"][:], rhs=yy[:, :ln], start=True, stop=True), reads=["BO", "royy"], writes=["ps4"])
                P.op("dve", lambda e, ln=ln: e.scalar_tensor_tensor(out=yc[:, :ln], in0=PS[4][:, :ln], scalar=-1.0 / 64, in1=yy[:, :ln], op0=ALU.mult, op1=ALU.add), reads=["ps4", "royy"], writes=["royc"])
                P.op("act", lambda e, ln=ln: e.activation(out=sq[:, :ln], in_=yc[:, :ln], func=AF.Square), reads=["royc"], writes=["rosq"])
                P.op("pe", lambda e, ln=ln: e.matmul(PS[5][:, :ln], lhsT=K["BO"][:], rhs=sq[:, :ln], start=True, stop=True), reads=["BO", "rosq"], writes=["ps5"])
                P.op("act", lambda e, ln=ln: e.activation(out=rs[:, :ln], in_=PS[5][:, :ln], func=AF.Sqrt, scale=1.0 / 64, bias=K["rwc"][:, 2:3]), reads=["ps5", "rwc"], writes=["rors"])
                P.op("dve", lambda e, ln=ln: e.reciprocal(out=rs[:, :ln], in_=rs[:, :ln]), reads=["rors"], writes=["rors"])
                P.op("pool", lambda e, ln=ln: e.tensor_tensor(out=yc[:, :ln], in0=yc[:, :ln], in1=rs[:, :ln], op=ALU.mult), reads=["royc", "rors"], writes=["royc"])
                P.op("dve", lambda e, j=j, ln=ln: e.tensor_scalar(out=yc[:, :ln], in0=yc[:, :ln], scalar1=vt[:, j, 3:4], scalar2=vt[:, j, 4:5], op0=ALU.mult, op1=ALU.add), reads=["royc", "rovec"], writes=["royc"])
                P.op("pool", lambda e, j=j, ln=ln: e.tensor_tensor(out=ob[j % 2][:, :ln], in0=yc[:, :ln], in1=gg[:, :ln], op=ALU.mult), reads=["royc", "rog"], writes=[f"roob{j % 2}"])
                P.dma("sp", lambda e, j=j, c0=c0, ln=ln: e.dma_start(out=YRW[:, j, c0:c0 + ln], in_=ob[j % 2][:, :ln]), reads=[f"roob{j % 2}"], writes=["Y1"])
        P.barrier()
```

```python
import math
from contextlib import ExitStack
import numpy as np
import ml_dtypes
import concourse.bass as bass
import concourse.mybir as mybir
from concourse.bass_utils import run_bass_kernel_spmd

F32 = mybir.dt.float32
BF16 = mybir.dt.bfloat16
I32 = mybir.dt.int32
AF = mybir.ActivationFunctionType
ALU = mybir.AluOpType
AX = mybir.AxisListType

L = 2
D = 1024
TC = 256
TL = 4096
T = TC + TL
IN_COLS = 6912
NE = 32
CHUNKS = [(0, 256)] + [(256 + 512 * i, 512) for i in range(8)]
ENGS = ("pe", "dve", "act", "pool", "sp")
NDMA_SEM = 8


class Prog:
    CHECK_DMA = False
    DRAIN_BEFORE_DMA = False
    DBG = 0
    dma_multi = []

    def __init__(self, nc):
        self.nc = nc
        self.ops = {e: [] for e in ENGS}
        self.cnt = {}
        self.seen = {e: {} for e in ENGS}
        self.lw = {}
        self.lr = {}
        self.dma_rr = {e: 0 for e in ENGS}
        self.flushed = {}
        self.act_scratch = None
        self.sem_names = []
        for e in ENGS:
            self._mk(f"c_{e}")
            for i in range(NDMA_SEM):
                self._mk(f"d_{e}{i}")

    def _mk(self, name):
        self.cnt[name] = 0
        self.sem_names.append(name)

    def _deps(self, eng, reads, writes):
        need = {}

        def add(tok):
            if tok is None:
                return
            s, v = tok
            if need.get(s, 0) < v:
                need[s] = v
        for k in reads:
            add(self.lw.get(k))
        for k in writes:
            add(self.lw.get(k))
            for t in self.lr.get(k, ()):
                add(t)
        waits = []
        seen = self.seen[eng]
        for s, v in need.items():
            if s == "c_pe" and eng == "pe":
                continue
            if seen.get(s, 0) >= v:
                continue
            seen[s] = v
            waits.append((s, v))
        return waits

    def _commit(self, tok, reads, writes):
        for k in writes:
            self.lw[k] = tok
            self.lr[k] = []
        for k in reads:
            lst = self.lr.setdefault(k, [])
            lst.append(tok)
            if len(lst) > 12:
                m = {}
                for s, v in lst:
                    m[s] = max(m.get(s, 0), v)
                self.lr[k] = list(m.items())

    def op(self, eng, fn, reads=(), writes=()):
        waits = self._deps(eng, reads, writes)
        s = f"c_{eng}"
        self.cnt[s] += 1
        tok = (s, self.cnt[s])
        self.ops[eng].append((waits, fn, s, 1))
        self._commit(tok, reads, writes)
        return tok

    def _flush_producers(self, reads):
        if not Prog.DRAIN_BEFORE_DMA:
            return
        for k in reads:
            tok = self.lw.get(k)
            if tok is None or not tok[0].startswith("c_") or tok[0] == "c_pe":
                continue
            pe_ = tok[0][2:]
            if self.flushed.get(pe_, 0) >= tok[1]:
                ftok = (tok[0], self.flushed[pe_])
            else:
                s = tok[0]
                self.cnt[s] += 1
                ftok = (s, self.cnt[s])
                self.ops[pe_].append(([], lambda e: e.drain(), s, 1))
                self.flushed[pe_] = self.cnt[s]
            self.lw[k] = ftok

    def dma(self, eng, fn, reads=(), writes=()):
        if eng == "act":
            eng = "sp"
        self._flush_producers(reads)
        waits = self._deps(eng, reads, writes)
        i = self.dma_rr[eng]
        self.dma_rr[eng] = (i + 1) % NDMA_SEM
        s = f"d_{eng}{i}"
        if self.cnt[s] > 0 and self.seen[eng].get(s, 0) < self.cnt[s]:
            self.seen[eng][s] = self.cnt[s]
            waits = waits + [(s, self.cnt[s])]
        self.cnt[s] += 16
        tok = (s, self.cnt[s])
        self.ops[eng].append((waits, fn, s, 16))
        self._commit(tok, reads, writes)
        return tok

    def barrier(self):
        for e in ENGS:
            waits = []
            for s, v in self.cnt.items():
                if v == 0 or self.seen[e].get(s, 0) >= v:
                    continue
                if s == f"c_{e}" and e == "pe":
                    pass
                self.seen[e][s] = v
                waits.append((s, v))
            self.ops[e].append((waits, None, None, 0))
        self.lw = {}
        self.lr = {}

    def emit(self):
        nc = self.nc
        with ExitStack() as es:
            sems = {n: es.enter_context(nc.semaphore(n)) for n in self.sem_names}
            block = es.enter_context(nc.Block())
            handles = {"pe": block.tensor, "dve": block.vector, "act": block.scalar,
                       "pool": block.gpsimd, "sp": block.sync}
            for e in ENGS:
                ops = self.ops[e]
                if not ops:
                    continue

                def body(eh, ops=ops, e=e):
                    if e == "act" and self.act_scratch is not None:
                        eh = _ActProxy(eh, self.act_scratch)
                    for waits, fn, s, inc in ops:
                        for (ws, wv) in waits:
                            eh.wait_ge(sems[ws], wv)
                        if fn is not None:
                            if inc == 16 and Prog.CHECK_DMA:
                                n_before = nc.n_instructions()
                                ins = fn(eh)
                                dn = nc.n_instructions() - n_before
                                if dn != 1:
                                    Prog.dma_multi.append((dn, str(ins)[:200]))
                                ins.then_inc(sems[s], inc)
                            else:
                                fn(eh).then_inc(sems[s], inc)
                handles[e](body)


class _ActProxy:
    def __init__(self, eh, scr):
        self._eh = eh
        self._scr = scr
        self._last = None

    def activation(self, *a, **kw):
        f = kw.get("func")
        if f != self._last:
            self._last = f
            for _ in range(2):
                self._eh.activation(out=self._scr[:, 0:64], in_=self._scr[:, 64:128], func=f)
        return self._eh.activation(*a, **kw)

    def __getattr__(self, n):
        return getattr(self._eh, n)


class Ctx:
    def __init__(self, nc, feed=(), dump=()):
        self.nc = nc
        self.P = Prog(nc)
        self.feed = set(feed)
        self.dump = set(dump)
        self.drams = {}
        self.ext_in = set()
        self.ext_out = set()

    def dram(self, name, shape, dtype=F32, kind=None):
        if name in self.drams:
            return self.drams[name]
        if kind is None:
            kind = "ExternalInput" if name in self.feed else ("ExternalOutput" if name in self.dump else "Internal")
        t = self.nc.dram_tensor(name, list(shape), dtype, kind=kind).ap()
        self.drams[name] = t
        if kind == "ExternalInput":
            self.ext_in.add(name)
        elif kind == "ExternalOutput":
            self.ext_out.add(name)
        return t

    def inp(self, name, shape, dtype=F32):
        return self.dram(name, shape, dtype, kind="ExternalInput")


_SBT_UID = [0]


def sbt(es, nc, name, shape, dtype=F32):
    _SBT_UID[0] += 1
    return es.enter_context(nc.sbuf_tensor(f"{name}_u{_SBT_UID[0]}", list(shape), dtype))


def stage_consts(C, es):
    nc, P = C.nc, C.P
    K = {}
    K["ones_bf"] = sbt(es, nc, "ones_bf", [128, 128], BF16)
    K["ident"] = sbt(es, nc, "ident", [128, 128], F32)
    K["sel"] = sbt(es, nc, "sel", [32, NE * 128], F32)
    K["cond"] = sbt(es, nc, "cond", [128, 8, 2], F32)
    P.op("pool", lambda e: e.memset(K["ones_bf"][:], 1.0), writes=["ones_bf"])
    identd = C.inp("ident_in", [128, 128])
    seld = C.inp("sel_in", [32, NE * 128])
    ccT = C.inp("ccT", [128, 16])
    P.dma("sp", lambda e: e.dma_start(out=K["ident"][:], in_=identd[:, :]), writes=["ident"])
    P.dma("sp", lambda e: e.dma_start(out=K["sel"][:], in_=seld[:, :]), writes=["sel"])
    P.dma("sp", lambda e: e.dma_start(out=K["cond"][:].rearrange("p j s -> p (j s)"), in_=ccT[:, :]), writes=["cond"])
    P.op("act", lambda e: e.activation(out=K["cond"][:], in_=K["cond"][:], func=AF.Silu), reads=["cond"], writes=["cond"])
    for l in range(L):
        K[f"ada{l}"] = sbt(es, nc, f"ada{l}", [128, 48, 2], F32)
        K[f"gs1_{l}"] = sbt(es, nc, f"gs1_{l}", [128, 8, 2], F32)
        K[f"gs2_{l}"] = sbt(es, nc, f"gs2_{l}", [128, 8, 2], F32)
    K["fing"] = sbt(es, nc, "fing", [128, 8], F32)
    fg = C.inp("final_g_t", [128, 8])
    P.dma("sp", lambda e: e.dma_start(out=K["fing"][:], in_=fg[:, :]), writes=["fing"])
    return K


def stage_ada(C, K, PS, l):
    nc, P = C.nc, C.P
    ada_w = C.inp("ada_w", [L, D, 6 * D])
    ada_b = C.inp("ada_b_t", [L, 128, 48])
    n1g = C.inp("norm1_g_t", [L, 128, 8])
    n2g = C.inp("norm2_g_t", [L, 128, 8])
    with ExitStack() as es:
        aw = [sbt(es, nc, f"aw{i}_{l}", [128, 8, 128], F32) for i in range(3)]
        bt = sbt(es, nc, f"adab_{l}", [128, 48], F32)
        g1t = sbt(es, nc, f"n1g_{l}", [128, 8], F32)
        g2t = sbt(es, nc, f"n2g_{l}", [128, 8], F32)
        tmp = sbt(es, nc, f"adatmp_{l}", [128, 8, 2], F32)
        P.dma("sp", lambda e: e.dma_start(out=bt[:], in_=ada_b[l]), writes=["adab"])
        P.dma("sp", lambda e: e.dma_start(out=g1t[:], in_=n1g[l]), writes=["n1g"])
        P.dma("sp", lambda e: e.dma_start(out=g2t[:], in_=n2g[l]), writes=["n2g"])
        pa = PS[0]
        wv = ada_w[l].rearrange("(k p) c -> p k c", p=128)
        for j in range(48):
            a = aw[j % 3]
            key = f"aw{j % 3}"
            P.dma("sp" if j % 2 == 0 else "act", lambda e, a=a, j=j: e.dma_start(out=a[:], in_=wv[:, :, j * 128:(j + 1) * 128]), writes=[key])
            for k in range(8):
                P.op("pe", lambda e, a=a, j=j, k=k: e.matmul(pa[:, 2 * j:2 * j + 2], lhsT=a[:, k, :], rhs=K["cond"][:, k, :],
                                                              start=(k == 0), stop=(k == 7)), reads=[key, "cond"], writes=["ps0"])
        ada = K[f"ada{l}"]
        P.op("dve", lambda e: e.tensor_tensor(out=ada[:], in0=pa[:, 0:96].rearrange("p (j s) -> p j s", s=2),
                                               in1=bt[:, :].unsqueeze(2).broadcast_to([128, 48, 2]), op=ALU.add),
             reads=["ps0", "adab"], writes=[f"ada{l}"])
        for (gs, sc0, gt, gk) in ((K[f"gs1_{l}"], 8, g1t, "n1g"), (K[f"gs2_{l}"], 32, g2t, "n2g")):
            P.op("dve", lambda e, sc0=sc0: e.tensor_scalar(out=tmp[:], in0=ada[:, sc0:sc0 + 8, :], scalar1=1.0, scalar2=None, op0=ALU.add),
                 reads=[f"ada{l}"], writes=["adatmp"])
            P.op("dve", lambda e, gs=gs, gt=gt: e.tensor_tensor(out=gs[:], in0=tmp[:], in1=gt[:, :].unsqueeze(2).broadcast_to([128, 8, 2]), op=ALU.mult),
                 reads=["adatmp", gk], writes=[f"gs{l}"])
        if "dbgADA" in C.dump:
            dd_ = C.dram("dbgADA", [128, 96]); dg_ = C.dram("dbgGS1", [128, 16])
            P.dma("sp", lambda e: e.dma_start(out=dd_[:, :], in_=ada[:].rearrange("p j s -> p (j s)")), reads=[f"ada{l}"], writes=["dbgADA"])
            P.dma("sp", lambda e: e.dma_start(out=dg_[:, :], in_=K[f"gs1_{l}"][:].rearrange("p j s -> p (j s)")), reads=[f"gs{l}"], writes=["dbgGS1"])
        P.barrier()


def stage_resid_init(C, K, PS):
    nc, P = C.nc, C.P
    xT = C.inp("xT", [D, T])
    posT = C.inp("posT", [D, T])
    R = C.dram("R", [D, T])
    with ExitStack() as es:
        a = [sbt(es, nc, f"ri_a{i}", [128, T], F32) for i in range(2)]
        b = [sbt(es, nc, f"ri_b{i}", [128, T], F32) for i in range(2)]
        for j in range(8):
            i = j % 2
            P.dma("sp", lambda e, i=i, j=j: e.dma_start(out=a[i][:], in_=xT[j * 128:(j + 1) * 128, :]), writes=[f"ri_a{i}"])
            P.dma("act", lambda e, i=i, j=j: e.dma_start(out=b[i][:], in_=posT[j * 128:(j + 1) * 128, :]), writes=[f"ri_b{i}"])
            P.op("pool", lambda e, i=i: e.tensor_tensor(out=a[i][:], in0=a[i][:], in1=b[i][:], op=ALU.add), reads=[f"ri_a{i}", f"ri_b{i}"], writes=[f"ri_a{i}"])
            P.dma("sp", lambda e, i=i, j=j: e.dma_start(out=R[j * 128:(j + 1) * 128, :], in_=a[i][:]), reads=[f"ri_a{i}"], writes=[f"R{j}"])
        P.barrier()


def norm_chunk(C, K, xs, xs_key, rstd, rstd_key, sq, sq_key, ln, psum, pskey, tmp, tmp_keys=None):
    P = C.P
    ta, tb = tmp
    for j in range(8):
        P.op("pool", lambda e, j=j: e.tensor_tensor(out=sq[:, j, :ln], in0=xs[:, j, :ln], in1=xs[:, j, :ln], op=ALU.mult), reads=[xs_key], writes=[sq_key])
    for j in range(8):
        P.op("pe", lambda e, j=j: e.matmul(psum[:, :ln], lhsT=K["ones_bf"][:], rhs=sq[:, j, :ln], start=(j == 0), stop=(j == 7)),
             reads=[sq_key, "ones_bf"], writes=[pskey])
    P.op("act", lambda e: e.activation(out=rstd[:, :ln], in_=psum[:, :ln], func=AF.Sqrt, scale=1.0 / D, bias=K["eps"][:, 0:1]), reads=[pskey, "eps"], writes=[rstd_key])
    P.op("dve", lambda e: e.reciprocal(out=rstd[:, :ln], in_=rstd[:, :ln]), reads=[rstd_key], writes=[rstd_key])


def stage_norm1_inproj(C, K, PS, l, need_ctx):
    nc, P = C.nc, C.P
    R = C.dram("R", [D, T])
    Rv = R.rearrange("(j p) t -> p j t", p=128)
    w_in = C.inp("w_in", [L, D, IN_COLS])
    convw = C.inp("conv_t", [L, 128, 26, 4])
    UHY = C.dram("UHY", [T, 1536])
    URW = C.dram("URW", [1792, T])
    VTM = C.dram("VTM", [T, 512])
    VTMR = C.dram("VTMR", [T, 512])
    US5 = C.dram("US5", [512, T])
    GATE = C.dram("GATE", [3072, T], BF16)
    ada, gs1 = K[f"ada{l}"], K[f"gs1_{l}"]
    with ExitStack() as es:
        hT = sbt(es, nc, "hT", [128, 8, T], BF16)
        cw = sbt(es, nc, "convw", [128, 26, 4], F32)
        es_n = ExitStack()
        xs = [sbt(es_n, nc, f"n1xs{i}", [128, 8, 512], F32) for i in range(2)]
        sq = sbt(es_n, nc, "n1sq", [128, 8, 512], BF16)
        rstd = sbt(es_n, nc, "n1rstd", [128, 512], F32)
        tmp = [sbt(es_n, nc, f"n1tmp{i}", [128, 512], F32) for i in range(2)]
        ntmp = [sbt(es_n, nc, f"n1nt{i}", [128, 512], F32) for i in range(2)]
        P.dma("sp", lambda e: e.dma_start(out=cw[:], in_=convw[l]), writes=["convw"])
        for ci, (c0, ln) in enumerate(CHUNKS):
            s = 1 if ci == 0 else 0
            x = xs[ci % 2]
            tg = f"n1_{ci % 2}"
            P.dma("sp", lambda e, x=x, c0=c0, ln=ln: e.dma_start(out=x[:, :, :ln], in_=Rv[:, :, c0:c0 + ln]), reads=[f"R{j}" for j in range(8)], writes=[tg + "xs"])
            norm_chunk(C, K, x, tg + "xs", rstd, "n1rstd", sq, "n1sq", ln, PS[1], "ps1", ntmp)
            for j in range(8):
                t_ = tmp[j % 2]
                P.op("dve", lambda e, t_=t_, x=x, j=j, ln=ln, s=s: e.scalar_tensor_tensor(out=t_[:, :ln], in0=x[:, j, :ln], scalar=gs1[:, j, s:s + 1], in1=rstd[:, :ln], op0=ALU.mult, op1=ALU.mult),
                     reads=[tg + "xs", "n1rstd", f"gs{l}"], writes=[f"n1tmp{j % 2}"])
                P.op("act", lambda e, t_=t_, j=j, c0=c0, ln=ln, s=s: e.activation(out=hT[:, j, c0:c0 + ln], in_=t_[:, :ln], func=AF.Identity, bias=ada[:, j, s:s + 1], scale=1.0),
                     reads=[f"n1tmp{j % 2}", f"ada{l}"], writes=[f"hT{ci}"])
        P.barrier()
        es_n.close()
        wv = w_in[l].rearrange("(k p) c -> p k c", p=128)
        wst = [sbt(es, nc, f"ipw_s{i}", [128, 8, 128], F32) for i in range(2)]
        wb = [sbt(es, nc, f"ipw_b{i}", [128, 8, 128], BF16) for i in range(2)]
        zt = [sbt(es, nc, f"zt{i}", [128, T], F32) for i in range(2)]
        ut = [sbt(es, nc, "ut0", [128, T], F32)] * 2
        stg = [sbt(es, nc, "tstg0", [128, 34, 128], F32)] * 2
        sg2 = sbt(es, nc, "tstg2", [128, 34, 128], F32)
        gtb = [sbt(es, nc, f"gtb{i}", [128, T], BF16) for i in range(2)]
        nct = IN_COLS // 128
        psi = 2
        for ct in range(nct):
            i = ct % 2
            P.dma("sp" if i == 0 else "act", lambda e, i=i, ct=ct: e.dma_start(out=wst[i][:], in_=wv[:, :, ct * 128:(ct + 1) * 128]), writes=[f"ipw_s{i}"])
            P.op("pool", lambda e, i=i: e.tensor_copy(out=wb[i][:], in_=wst[i][:]), reads=[f"ipw_s{i}"], writes=[f"ipw_b{i}"])
            z = zt[i]
            for ci, (c0, ln) in enumerate(CHUNKS):
                ps = PS[2 + (psi % 4)]
                pk = f"ps{2 + (psi % 4)}"
                psi += 1
                for k in range(8):
                    P.op("pe", lambda e, ps=ps, i=i, k=k, c0=c0, ln=ln: e.matmul(ps[:, :ln], lhsT=wb[i][:, k, :], rhs=hT[:, k, c0:c0 + ln], start=(k == 0), stop=(k == 7)),
                         reads=[f"ipw_b{i}", f"hT{ci}"], writes=[pk])
                if ct < 30:
                    eng = "act" if ci % 2 else "dve"
                    if eng == "act":
                        P.op("act", lambda e, ps=ps, z=z, c0=c0, ln=ln: e.activation(out=z[:, c0:c0 + ln], in_=ps[:, :ln], func=AF.Copy), reads=[pk], writes=[f"zt{i}"])
                    else:
                        P.op("dve", lambda e, ps=ps, z=z, c0=c0, ln=ln: e.tensor_copy(out=z[:, c0:c0 + ln], in_=ps[:, :ln]), reads=[pk], writes=[f"zt{i}"])
                else:
                    P.op("act", lambda e, ps=ps, i=i, c0=c0, ln=ln: e.activation(out=gtb[i][:, c0:c0 + ln], in_=ps[:, :ln], func=AF.Sigmoid), reads=[pk], writes=[f"gtb{i}"])
            if ct < 26:
                u = ut[i]
                for (s0, sl) in ((0, TC), (TC, TL)):
                    P.op("dve", lambda e, u=u, z=z, s0=s0, sl=sl, ct=ct: e.tensor_scalar(out=u[:, s0:s0 + sl], in0=z[:, s0:s0 + sl], scalar1=cw[:, ct, 1:2], scalar2=cw[:, ct, 3:4], op0=ALU.mult, op1=ALU.add),
                         reads=[f"zt{i}", "convw"], writes=["ut0"])
                    P.op("dve", lambda e, u=u, z=z, s0=s0, sl=sl, ct=ct: e.scalar_tensor_tensor(out=u[:, s0 + 1:s0 + sl], in0=z[:, s0:s0 + sl - 1], scalar=cw[:, ct, 0:1], in1=u[:, s0 + 1:s0 + sl], op0=ALU.mult, op1=ALU.add),
                         reads=[f"zt{i}", "ut0", "convw"], writes=["ut0"])
                    P.op("dve", lambda e, u=u, z=z, s0=s0, sl=sl, ct=ct: e.scalar_tensor_tensor(out=u[:, s0:s0 + sl - 1], in0=z[:, s0 + 1:s0 + sl], scalar=cw[:, ct, 2:3], in1=u[:, s0:s0 + sl - 1], op0=ALU.mult, op1=ALU.add),
                         reads=[f"zt{i}", "ut0", "convw"], writes=["ut0"])
                tm_dst = None
                if ct < 12:
                    tm_dst = UHY[:, ct * 128:(ct + 1) * 128]
                    tmk = f"UHY{ct}"
                elif 20 <= ct < 24:
                    tm_dst = VTM[:, (ct - 20) * 128:(ct - 19) * 128]
                    tmk = f"VTM{ct - 20}"
                if ct >= 12:
                    P.dma("sp", lambda e, u=u, ct=ct: e.dma_start(out=URW[(ct - 12) * 128:(ct - 11) * 128, :], in_=u[:]), reads=["ut0"], writes=[f"URW{ct - 12}"])
                if tm_dst is not None:
                    sg = stg[i]
                    for b0 in range(0, 34, 4):
                        nb = min(4, 34 - b0)
                        ps = PS[6 + (b0 // 4) % 2]
                        pk = f"ps{6 + (b0 // 4) % 2}"
                        for bb in range(nb):
                            P.op("pe", lambda e, ps=ps, u=u, b0=b0, bb=bb: e.transpose(ps[:, bb * 128:(bb + 1) * 128], u[:, (b0 + bb) * 128:(b0 + bb + 1) * 128], K["ident"][:]),
                                 reads=["ut0", "ident"], writes=[pk])
                        P.op("act", lambda e, ps=ps, sg=sg, b0=b0, nb=nb: e.activation(out=sg[:, b0:b0 + nb, :], in_=ps[:, :nb * 128].rearrange("p (b c) -> p b c", c=128), func=AF.Copy),
                             reads=[pk], writes=["tstg0"])
                    for b0 in range(0, 34, 6):
                        nb = min(6, 34 - b0)
                        P.dma("sp", lambda e, sg=sg, tm_dst=tm_dst, b0=b0, nb=nb: e.dma_start(out=tm_dst[b0 * 128:(b0 + nb) * 128, :].rearrange("(b p) c -> p b c", p=128), in_=sg[:, b0:b0 + nb, :]), reads=["tstg0"], writes=[tmk])
                if 20 <= ct < 24:
                    vr_dst = VTMR[:, (ct - 20) * 128:(ct - 19) * 128]
                    for b0 in range(0, 34, 4):
                        nb = min(4, 34 - b0)
                        ps = PS[6 + (b0 // 4) % 2]
                        pk = f"ps{6 + (b0 // 4) % 2}"
                        for bb in range(nb):
                            P.op("pe", lambda e, ps=ps, sg=sg, b0=b0, bb=bb: e.matmul(ps[:, bb * 128:(bb + 1) * 128], lhsT=K["Jm"][:], rhs=sg[:, b0 + bb, :], start=True, stop=True),
                                 reads=["tstg0", "Jm"], writes=[pk])
                        for bb in range(nb):
                            P.op("act", lambda e, ps=ps, b0=b0, bb=bb: e.activation(out=sg2[:, 33 - (b0 + bb), :], in_=ps[:, bb * 128:(bb + 1) * 128], func=AF.Copy), reads=[pk], writes=["tstg2"])
                    for b0 in range(0, 34, 6):
                        nb = min(6, 34 - b0)
                        P.dma("sp", lambda e, vr_dst=vr_dst, b0=b0, nb=nb: e.dma_start(out=vr_dst[b0 * 128:(b0 + nb) * 128, :].rearrange("(b p) c -> p b c", p=128), in_=sg2[:, b0:b0 + nb, :]), reads=["tstg2"], writes=[f"VTMR{ct - 20}"])
            elif ct < 30:
                P.dma("sp", lambda e, z=z, ct=ct: e.dma_start(out=US5[(ct - 26) * 128:(ct - 25) * 128, :], in_=z[:]), reads=[f"zt{i}"], writes=[f"US5{ct - 26}"])
            else:
                P.dma("sp", lambda e, i=i, ct=ct: e.dma_start(out=GATE[(ct - 30) * 128:(ct - 29) * 128, :], in_=gtb[i][:]), reads=[f"gtb{i}"], writes=[f"GATE{ct - 30}"])
        P.barrier()


def stage_merge(C, K, PS, l, need_ctx):
    nc, P = C.nc, C.P
    R = C.dram("R", [D, T])
    Rv = R.rearrange("(j p) t -> p j t", p=128)
    GATE = C.dram("GATE", [3072, T], BF16)
    Gv = GATE.rearrange("(b j p) t -> p b j t", p=128, j=8)
    Y = [C.dram(n, [512, T], BF16) for n in ("YHY", "YRW", "YS5")]
    wbr = C.inp("w_branch", [L, 3, 512, D])
    wout = C.inp("w_out", [L, D, D])
    ada = K[f"ada{l}"]
    with ExitStack() as es:
        wbb = sbt(es, nc, "wbb", [128, 12, D], BF16)
        wob = sbt(es, nc, "wob", [128, 8, D], BF16)
        wst = [sbt(es, nc, f"mwst{i}", [128, D], F32) for i in range(2)]
        n = 0
        for b in range(3):
            for k in range(4):
                i = n % 2
                P.dma("sp", lambda e, i=i, b=b, k=k: e.dma_start(out=wst[i][:], in_=wbr[l, b, k * 128:(k + 1) * 128, :]), writes=[f"mwst{i}"])
                P.op("pool", lambda e, i=i, b=b, k=k: e.tensor_copy(out=wbb[:, b * 4 + k, :], in_=wst[i][:]), reads=[f"mwst{i}"], writes=["wbb"])
                n += 1
        for k in range(8):
            i = n % 2
            P.dma("sp", lambda e, i=i, k=k: e.dma_start(out=wst[i][:], in_=wout[l, k * 128:(k + 1) * 128, :]), writes=[f"mwst{i}"])
            P.op("pool", lambda e, i=i, k=k: e.tensor_copy(out=wob[:, k, :], in_=wst[i][:]), reads=[f"mwst{i}"], writes=["wob"])
            n += 1
        yb = [sbt(es, nc, f"myb{i}", [128, 12, 512], BF16) for i in range(2)]
        gb = [sbt(es, nc, f"mgb{i}", [128, 3, 8, 512], BF16) for i in range(2)]
        xr = [sbt(es, nc, f"mxr{i}", [128, 8, 512], F32) for i in range(2)]
        mT = sbt(es, nc, "mT", [128, 8, 512], BF16)
        t1 = [sbt(es, nc, f"mt1_{i}", [128, 512], F32) for i in range(2)]
        for ci, (c0, ln) in enumerate(CHUNKS):
            if ci == 0 and not need_ctx:
                continue
            s = 1 if ci == 0 else 0
            i = ci % 2
            for b in range(3):
                P.dma("sp", lambda e, i=i, b=b, c0=c0, ln=ln: e.dma_start(out=yb[i][:, b * 4:(b + 1) * 4, :ln], in_=Y[b].rearrange("(k p) t -> p k t", p=128)[:, :, c0:c0 + ln]),
                      reads=[f"Y{b}"], writes=[f"myb{i}"])
                P.dma("act", lambda e, i=i, b=b, c0=c0, ln=ln: e.dma_start(out=gb[i][:, b, :, :ln], in_=Gv[:, b, :, c0:c0 + ln]),
                      reads=[f"GATE{b * 8 + j}" for j in range(8)], writes=[f"mgb{i}"])
            P.dma("sp", lambda e, i=i, c0=c0, ln=ln: e.dma_start(out=xr[i][:, :, :ln], in_=Rv[:, :, c0:c0 + ln]), reads=[f"R{j}" for j in range(8)], writes=[f"mxr{i}"])
            for dt_ in range(8):
                for b in range(3):
                    ps = PS[b]
                    for k in range(4):
                        P.op("pe", lambda e, ps=ps, i=i, b=b, k=k, dt_=dt_, ln=ln: e.matmul(ps[:, :ln], lhsT=wbb[:, b * 4 + k, dt_ * 128:(dt_ + 1) * 128], rhs=yb[i][:, b * 4 + k, :ln], start=(k == 0), stop=(k == 3)),
                             reads=["wbb", f"myb{i}"], writes=[f"ps{b}"])
                ta, tb = t1
                P.op("dve", lambda e, i=i, dt_=dt_, ln=ln: e.tensor_tensor(out=ta[:, :ln], in0=PS[0][:, :ln], in1=gb[i][:, 0, dt_, :ln], op=ALU.mult), reads=["ps0", f"mgb{i}"], writes=["mt1_0"])
                P.op("dve", lambda e, i=i, dt_=dt_, ln=ln: e.tensor_tensor(out=tb[:, :ln], in0=PS[1][:, :ln], in1=gb[i][:, 1, dt_, :ln], op=ALU.mult), reads=["ps1", f"mgb{i}"], writes=["mt1_1"])
                P.op("pool", lambda e, ln=ln: e.tensor_tensor(out=ta[:, :ln], in0=ta[:, :ln], in1=tb[:, :ln], op=ALU.add), reads=["mt1_0", "mt1_1"], writes=["mt1_0"])
                P.op("dve", lambda e, i=i, dt_=dt_, ln=ln: e.tensor_tensor(out=tb[:, :ln], in0=PS[2][:, :ln], in1=gb[i][:, 2, dt_, :ln], op=ALU.mult), reads=["ps2", f"mgb{i}"], writes=["mt1_1"])
                P.op("pool", lambda e, dt_=dt_, ln=ln: e.tensor_tensor(out=mT[:, dt_, :ln], in0=ta[:, :ln], in1=tb[:, :ln], op=ALU.add), reads=["mt1_0", "mt1_1"], writes=[f"mT{dt_}"])
            for dt_ in range(8):
                ps = PS[4 + dt_ % 2]
                pk = f"ps{4 + dt_ % 2}"
                for k in range(8):
                    P.op("pe", lambda e, ps=ps, k=k, dt_=dt_, ln=ln: e.matmul(ps[:, :ln], lhsT=wob[:, k, dt_ * 128:(dt_ + 1) * 128], rhs=mT[:, k, :ln], start=(k == 0), stop=(k == 7)),
                         reads=["wob", f"mT{k}"], writes=[pk])
                P.op("dve", lambda e, ps=ps, i=i, dt_=dt_, ln=ln, s=s: e.scalar_tensor_tensor(out=xr[i][:, dt_, :ln], in0=ps[:, :ln], scalar=ada[:, 16 + dt_, s:s + 1], in1=xr[i][:, dt_, :ln], op0=ALU.mult, op1=ALU.add),
                     reads=[pk, f"mxr{i}", f"ada{l}"], writes=[f"mxr{i}"])
            P.dma("sp", lambda e, i=i, c0=c0, ln=ln: e.dma_start(out=Rv[:, :, c0:c0 + ln], in_=xr[i][:, :, :ln]), reads=[f"mxr{i}"], writes=[f"R{j}" for j in range(8)])
        P.barrier()


def stage_moe(C, K, PS, l, need_ctx, final):
    nc, P = C.nc, C.P
    R = C.dram("R", [D, T])
    Rv = R.rearrange("(j p) t -> p j t", p=128)
    H2 = C.dram("H2", [D, T], BF16)
    H2v = H2.rearrange("(j p) t -> p j t", p=128)
    rw = C.inp("router_w_t", [L, 128, 8, NE])
    rb = C.inp("router_b", [L, NE])
    w1 = C.inp("moe_w1", [L, NE, D, 2 * D])
    w2 = C.inp("moe_w2", [L, NE, D, D])
    b1 = C.inp("moe_b1_t", [L, 128, NE, 16])
    b2 = C.inp("moe_b2_t", [L, 128, NE, 8])
    outT = C.dram("outT", [D, TL], kind="ExternalOutput") if final else None
    ada, gs2 = K[f"ada{l}"], K[f"gs2_{l}"]
    chunks = [(ci, c0, ln) for ci, (c0, ln) in enumerate(CHUNKS) if not (ci == 0 and not need_ctx)]
    with ExitStack() as es:
        GT = sbt(es, nc, "GT", [32, T], F32)
        b1t = sbt(es, nc, "b1t", [128, NE, 16], F32)
        b2t = sbt(es, nc, "b2t", [128, NE, 8], F32)
        P.dma("sp", lambda e: e.dma_start(out=b1t[:], in_=b1[l]), writes=["b1t"])
        P.dma("sp", lambda e: e.dma_start(out=b2t[:], in_=b2[l]), writes=["b2t"])
        with ExitStack() as es2:
            xs = [sbt(es2, nc, f"n2xs{i}", [128, 8, 512], F32) for i in range(2)]
            sq = sbt(es2, nc, "n2sq", [128, 8, 512], BF16)
            rstd = sbt(es2, nc, "n2rstd", [128, 512], F32)
            hf = sbt(es2, nc, "n2hf", [128, 8, 512], F32)
            ntmp = [sbt(es2, nc, f"n2nt{i}", [128, 512], F32) for i in range(2)]
            hb = [sbt(es2, nc, f"n2hb{i}", [128, 8, 512], BF16) for i in range(2)]
            rwt = sbt(es2, nc, "rwt", [128, 8, NE], F32)
            rbt = sbt(es2, nc, "rbt", [128, NE], F32)
            lg = sbt(es2, nc, "lg", [128, NE], F32)
            m8 = sbt(es2, nc, "m8", [128, 8], F32)
            nm = sbt(es2, nc, "nm", [128, 1], F32)
            msk = sbt(es2, nc, "msk", [128, NE], F32)
            ex = sbt(es2, nc, "ex", [128, NE], F32)
            ssum = sbt(es2, nc, "ssum", [128, 1], F32)
            P.dma("sp", lambda e: e.dma_start(out=rwt[:], in_=rw[l]), writes=["rwt"])
            P.dma("sp", lambda e: e.dma_start(out=rbt[:], in_=rb[l:l + 1, :].partition_broadcast(128)), writes=["rbt"])
            for (ci, c0, ln) in chunks:
                s = 1 if ci == 0 else 0
                i = ci % 2
                x = xs[i]
                tg = f"n2_{i}"
                P.dma("sp", lambda e, x=x, c0=c0, ln=ln: e.dma_start(out=x[:, :, :ln], in_=Rv[:, :, c0:c0 + ln]), reads=[f"R{j}" for j in range(8)], writes=[tg + "xs"])
                norm_chunk(C, K, x, tg + "xs", rstd, "n2_rstd", sq, "n2sq", ln, PS[0], "ps0", ntmp)
                for j in range(8):
                    P.op("dve", lambda e, x=x, j=j, ln=ln, s=s: e.scalar_tensor_tensor(out=hf[:, j, :ln], in0=x[:, j, :ln], scalar=gs2[:, j, s:s + 1], in1=rstd[:, :ln], op0=ALU.mult, op1=ALU.mult),
                         reads=[tg + "xs", "n2_rstd", f"gs{l}"], writes=["n2hf"])
                    P.op("act", lambda e, j=j, ln=ln, s=s: e.activation(out=hf[:, j, :ln], in_=hf[:, j, :ln], func=AF.Identity, bias=ada[:, 24 + j, s:s + 1], scale=1.0),
                         reads=["n2hf", f"ada{l}"], writes=["n2hf"])
                    P.op("pool", lambda e, i=i, j=j, ln=ln: e.tensor_copy(out=hb[i][:, j, :ln], in_=hf[:, j, :ln]), reads=["n2hf"], writes=[f"n2hb{i}"])
                P.dma("sp", lambda e, i=i, c0=c0, ln=ln: e.dma_start(out=H2v[:, :, c0:c0 + ln], in_=hb[i][:, :, :ln]), reads=[f"n2hb{i}"], writes=["H2"])
                if ci == 0 and "dbgRS" in C.dump:
                    d1 = C.dram("dbgRS", [128, 512]); d2 = C.dram("dbgHF0", [128, 8, 512]); d3 = C.dram("dbgXS", [128, 8, 512])
                    P.dma("sp", lambda e: e.dma_start(out=d1[:, :], in_=rstd[:]), reads=["n2_rstd"], writes=["dbgRS"])
                    P.dma("sp", lambda e: e.dma_start(out=d2[:, :, :], in_=hf[:]), reads=["n2hf"], writes=["dbgHF0"])
                    P.dma("sp", lambda e, x=x: e.dma_start(out=d3[:, :, :], in_=x[:]), reads=[tg + "xs"], writes=["dbgXS"])
                for tb in range(ln // 128):
                    ps = PS[1 + tb % 2]
                    pk = f"ps{1 + tb % 2}"
                    for j in range(8):
                        P.op("pe", lambda e, ps=ps, j=j, tb=tb: e.matmul(ps[:, :NE], lhsT=hf[:, j, tb * 128:(tb + 1) * 128], rhs=rwt[:, j, :], start=(j == 0), stop=(j == 7)),
                             reads=["n2hf", "rwt"], writes=[pk])
                    P.op("dve", lambda e, ps=ps: e.tensor_tensor(out=lg[:], in0=ps[:, :NE], in1=rbt[:], op=ALU.add), reads=[pk, "rbt"], writes=["lg"])
                    P.op("dve", lambda e: e.max(out=m8[:], in_=lg[:]), reads=["lg"], writes=["m8"])
                    P.op("dve", lambda e: e.tensor_scalar(out=nm[:], in0=m8[:, 0:1], scalar1=-1.0, scalar2=None, op0=ALU.mult), reads=["m8"], writes=["nm"])
                    P.op("dve", lambda e: e.tensor_scalar(out=msk[:], in0=lg[:], scalar1=m8[:, 3:4], scalar2=None, op0=ALU.is_ge), reads=["lg", "m8"], writes=["msk"])
                    P.op("act", lambda e: e.activation(out=ex[:], in_=lg[:], func=AF.Exp, bias=nm[:, 0:1], scale=1.0), reads=["lg", "nm"], writes=["ex"])
                    P.op("dve", lambda e: e.tensor_tensor(out=ex[:], in0=ex[:], in1=msk[:], op=ALU.mult), reads=["ex", "msk"], writes=["ex"])
                    P.op("dve", lambda e: e.reduce_sum(out=ssum[:], in_=ex[:], axis=AX.X), reads=["ex"], writes=["ssum"])
                    P.op("dve", lambda e: e.reciprocal(out=ssum[:], in_=ssum[:]), reads=["ssum"], writes=["ssum"])
                    P.op("dve", lambda e: e.tensor_scalar(out=ex[:], in0=ex[:], scalar1=ssum[:, 0:1], scalar2=None, op0=ALU.mult), reads=["ex", "ssum"], writes=["ex"])
                    ps3 = PS[3]
                    P.op("pe", lambda e, ps3=ps3: e.transpose(ps3[:NE, :128], ex[:], K["ident"][:]), reads=["ex", "ident"], writes=["ps3"])
                    P.op("act", lambda e, ps3=ps3, c0=c0, tb=tb: e.activation(out=GT[:, c0 + tb * 128:c0 + (tb + 1) * 128], in_=ps3[:NE, :128], func=AF.Copy), reads=["ps3"], writes=["GT"])
            if "dbgGT" in C.dump:
                dg_ = C.dram("dbgGT", [32, T])
                for q0 in range(0, T, 256):
                    P.dma("sp", lambda e, q0=q0: e.dma_start(out=dg_[:, q0:q0 + 256], in_=GT[:, q0:q0 + 256]), reads=["GT"], writes=["dbgGT"])
            if "dbgLG" in C.dump:
                for nm_, t_ in (("dbgLG", lg), ("dbgEX", ex), ("dbgM8", m8)):
                    dd_ = C.dram(nm_, list(t_.shape))
                    P.dma("sp", lambda e, dd_=dd_, t_=t_: e.dma_start(out=dd_[:, :], in_=t_[:]), reads=["lg", "ex", "m8", "msk"], writes=[nm_])
                dd_ = C.dram("dbgHF", [128, 8, 512])
                P.dma("sp", lambda e, dd_=dd_: e.dma_start(out=dd_[:, :, :], in_=hf[:]), reads=["n2hf"], writes=["dbgHF"])
            P.barrier()
            if Prog.DBG & 8:
                return
        groups = [chunks[i:i + 2] for i in range(0, len(chunks), 2)]
        w1v = w1[l].rearrange("e (k p) c -> e p k c", p=128)
        w2v = w2[l].rearrange("e (k p) c -> e p k c", p=128)
        with ExitStack() as es3:
            w1b = [sbt(es3, nc, "w1b0", [128, 8, 2048], BF16)] * 2
            w2b = [sbt(es3, nc, "w2b0", [128, 8, 1024], BF16)] * 2
            wst = [sbt(es3, nc, f"xwst{i}", [128, 1024], F32) for i in range(2)]
            acc = sbt(es3, nc, "acc", [128, 8, 1024], F32)
            h2c = [sbt(es3, nc, f"h2c{i}", [128, 8, 512], BF16) for i in range(2)]
            actT = sbt(es3, nc, "actT", [128, 8, 512], BF16)
            gsb = sbt(es3, nc, "gsb", [128, 8, 512], BF16)
            gbs = sbt(es3, nc, "gbs", [128, 512], F32)
            glc = [sbt(es3, nc, f"glc{i}", [128, 512], F32) for i in range(2)]
            sig = [sbt(es3, nc, f"sig{i}", [128, 512], F32) for i in range(2)]
            upc = [sbt(es3, nc, f"upc{i}", [128, 512], F32) for i in range(2)]
            otmp = [sbt(es3, nc, f"otmp{i}", [128, 512], F32) for i in range(2)]
            xr = sbt(es3, nc, "moxr", [128, 8, 512], F32)
            fsq_t = sbt(es3, nc, "fsq", [128, 8, 512], BF16)
            frstd_t = sbt(es3, nc, "frstd", [128, 512], F32)
            wsi = 0
            psi = 0
            for grp in groups:
                goff = {}
                o = 0
                for (ci, c0, ln) in grp:
                    goff[ci] = o
                    o += ln
                glen = o
                P.op("pool", lambda e, glen=glen: e.memset(acc[:, :, :glen], 0.0), writes=["acc"])
                hci = 0
                for ex_ in range(NE):
                    wi = ex_ % 2
                    for k in range(8):
                        for hh in range(2):
                            si = wsi % 2
                            wsi += 1
                            P.dma("sp" if hh == 0 else "act", lambda e, si=si, ex_=ex_, k=k, hh=hh: e.dma_start(out=wst[si][:], in_=w1v[ex_, :, k, hh * 1024:(hh + 1) * 1024]), writes=[f"xwst{si}"])
                            P.op("pool", lambda e, si=si, wi=wi, k=k, hh=hh: e.tensor_copy(out=w1b[wi][:, k, hh * 1024:(hh + 1) * 1024], in_=wst[si][:]), reads=[f"xwst{si}"], writes=["w1b0"])
                    for k in range(8):
                        si = wsi % 2
                        wsi += 1
                        P.dma("sp" if k % 2 == 0 else "act", lambda e, si=si, ex_=ex_, k=k: e.dma_start(out=wst[si][:], in_=w2v[ex_, :, k, :]), writes=[f"xwst{si}"])
                        P.op("pool", lambda e, si=si, k=k: e.tensor_copy(out=w2b[0][:, k, :], in_=wst[si][:]), reads=[f"xwst{si}"], writes=["w2b0"])
                    for (ci, c0, ln) in grp:
                        hi = hci % 2
                        hci += 1
                        P.dma("sp", lambda e, hi=hi, c0=c0, ln=ln: e.dma_start(out=h2c[hi][:, :, :ln], in_=H2v[:, :, c0:c0 + ln]), reads=["H2"], writes=[f"h2c{hi}"])
                        psg = PS[7]
                        P.op("pe", lambda e, psg=psg, ex_=ex_, c0=c0, ln=ln: e.matmul(psg[:, :ln], lhsT=K["sel"][:, ex_ * 128:(ex_ + 1) * 128], rhs=GT[:, c0:c0 + ln], start=True, stop=True),
                             reads=["sel", "GT"], writes=["ps7"])
                        P.op("act", lambda e, psg=psg, ln=ln: e.activation(out=gbs[:, :ln], in_=psg[:, :ln], func=AF.Copy), reads=["ps7"], writes=["gbs"])
                        for ht in range(16):
                            ps = PS[psi % 6]
                            pk = f"ps{psi % 6}"
                            psi += 1
                            for k in range(8):
                                P.op("pe", lambda e, ps=ps, wi=wi, hi=hi, k=k, ht=ht, ln=ln: e.matmul(ps[:, :ln], lhsT=w1b[wi][:, k, ht * 128:(ht + 1) * 128], rhs=h2c[hi][:, k, :ln], start=(k == 0), stop=(k == 7)),
                                     reads=["w1b0", f"h2c{hi}"], writes=[pk])
                            q = ht % 2
                            if ht < 8:
                                P.op("dve", lambda e, ps=ps, q=q, ex_=ex_, ht=ht, ln=ln: e.tensor_scalar(out=glc[q][:, :ln], in0=ps[:, :ln], scalar1=b1t[:, ex_, ht:ht + 1], scalar2=7.0, op0=ALU.add, op1=ALU.min),
                                     reads=[pk, "b1t"], writes=[f"glc{q}"])
                                P.op("act", lambda e, q=q, ln=ln: e.activation(out=sig[q][:, :ln], in_=glc[q][:, :ln], func=AF.Sigmoid, scale=1.702), reads=[f"glc{q}"], writes=[f"sig{q}"])
                                P.op("pool", lambda e, q=q, ht=ht, ln=ln: e.tensor_tensor(out=gsb[:, ht, :ln], in0=glc[q][:, :ln], in1=sig[q][:, :ln], op=ALU.mult), reads=[f"glc{q}", f"sig{q}"], writes=[f"gsb{ht}"])
                            else:
                                P.op("dve", lambda e, ps=ps, q=q, ex_=ex_, ht=ht, ln=ln: e.tensor_scalar(out=upc[q][:, :ln], in0=ps[:, :ln], scalar1=b1t[:, ex_, ht:ht + 1], scalar2=7.0, op0=ALU.add, op1=ALU.min),
                                     reads=[pk, "b1t"], writes=[f"upc{q}"])
                                P.op("pool", lambda e, q=q, ln=ln: e.tensor_scalar(out=upc[q][:, :ln], in0=upc[q][:, :ln], scalar1=-7.0, scalar2=1.0, op0=ALU.max, op1=ALU.add), reads=[f"upc{q}"], writes=[f"upc{q}"])
                                P.op("pool", lambda e, q=q, ht=ht, ln=ln: e.tensor_tensor(out=actT[:, ht - 8, :ln], in0=upc[q][:, :ln], in1=gsb[:, ht - 8, :ln], op=ALU.mult), reads=[f"upc{q}", f"gsb{ht - 8}"], writes=[f"actT{ht - 8}"])
                        for dt_ in range(8):
                            ps = PS[psi % 6]
                            pk = f"ps{psi % 6}"
                            psi += 1
                            for k in range(8):
                                P.op("pe", lambda e, ps=ps, wi=wi, k=k, dt_=dt_, ln=ln: e.matmul(ps[:, :ln], lhsT=w2b[wi][:, k, dt_ * 128:(dt_ + 1) * 128], rhs=actT[:, k, :ln], start=(k == 0), stop=(k == 7)),
                                     reads=["w2b0", f"actT{k}"], writes=[pk])
                            q = dt_ % 2
                            o0 = goff[ci]
                            P.op("dve", lambda e, ps=ps, q=q, ex_=ex_, dt_=dt_, ln=ln: e.scalar_tensor_tensor(out=otmp[q][:, :ln], in0=ps[:, :ln], scalar=b2t[:, ex_, dt_:dt_ + 1], in1=gbs[:, :ln], op0=ALU.add, op1=ALU.mult),
                                 reads=[pk, "b2t", "gbs"], writes=[f"otmp{q}"])
                            P.op("pool", lambda e, q=q, dt_=dt_, o0=o0, ln=ln: e.tensor_tensor(out=acc[:, dt_, o0:o0 + ln], in0=acc[:, dt_, o0:o0 + ln], in1=otmp[q][:, :ln], op=ALU.add),
                                 reads=["acc", f"otmp{q}"], writes=["acc"])
                for (ci, c0, ln) in grp:
                    s = 1 if ci == 0 else 0
                    o0 = goff[ci]
                    P.dma("sp", lambda e, c0=c0, ln=ln: e.dma_start(out=xr[:, :, :ln], in_=Rv[:, :, c0:c0 + ln]), reads=[f"R{j}" for j in range(8)], writes=["moxs"])
                    for j in range(8):
                        P.op("dve", lambda e, j=j, o0=o0, ln=ln, s=s: e.scalar_tensor_tensor(out=xr[:, j, :ln], in0=acc[:, j, o0:o0 + ln], scalar=ada[:, 40 + j, s:s + 1], in1=xr[:, j, :ln], op0=ALU.mult, op1=ALU.add),
                             reads=["acc", "moxs", f"ada{l}"], writes=["moxs"])
                    if not final:
                        P.dma("sp", lambda e, c0=c0, ln=ln: e.dma_start(out=Rv[:, :, c0:c0 + ln], in_=xr[:, :, :ln]), reads=["moxs"], writes=[f"R{j}" for j in range(8)])
                    elif ci > 0:
                        norm_chunk(C, K, xr, "moxs", frstd_t, "morstd", fsq_t, "mosq", ln, PS[6], "ps6", (glc[0], glc[1]), ("glc0", "glc1"))
                        for j in range(8):
                            P.op("dve", lambda e, j=j, ln=ln: e.scalar_tensor_tensor(out=xr[:, j, :ln], in0=xr[:, j, :ln], scalar=K["fing"][:, j:j + 1], in1=frstd_t[:, :ln], op0=ALU.mult, op1=ALU.mult),
                                 reads=["moxs", "morstd", "fing"], writes=["moxs"])
                        P.dma("sp", lambda e, c0=c0, ln=ln: e.dma_start(out=outT.rearrange("(j p) t -> p j t", p=128)[:, :, c0 - TC:c0 - TC + ln], in_=xr[:, :, :ln]), reads=["moxs"], writes=["outT"])
            P.barrier()


def build_program(stages, feed=(), dump=()):
    nc = bass.Bass("TRN2", target_bir_lowering=False)
    C = Ctx(nc, feed, dump)
    P = C.P
    with ExitStack() as es:
        PS = [es.enter_context(nc.psum_tensor(f"ps{i}", [128, 512], F32)) for i in range(8)]
        K = stage_consts(C, es)
        rw_consts(C, K, es)
        K["actscr"] = sbt(es, nc, "actscr", [128, 128], F32)
        P.op("pool", lambda e: e.memset(K["actscr"][:], 1.0), writes=["actscr"])
        P.act_scratch = K["actscr"]
        K["eps"] = sbt(es, nc, "eps", [128, 1], F32)
        P.op("pool", lambda e: e.memset(K["eps"][:], 1e-6), writes=["eps"])
        P.barrier()
        for st in stages:
            st(C, K, PS)
        P.barrier()
        P.emit()
    return nc, C


def _vt(v, n):
    return np.ascontiguousarray(v.reshape(v.shape[0], n, 128).transpose(0, 2, 1))


def grid_pos_T():
    rows = TL // 64
    quarter = D // 4
    omega = 1.0 / (10000.0 ** (np.arange(quarter, dtype=np.float32) / np.float32(quarter)))
    rid, cid = np.meshgrid(np.arange(rows), np.arange(64), indexing="ij")

    def enc(p):
        ang = p.reshape(-1)[:, None].astype(np.float32) * omega.astype(np.float32)
        return np.concatenate([np.sin(ang), np.cos(ang)], axis=-1)
    pe = np.concatenate([enc(rid), enc(cid)], axis=-1).astype(np.float32)
    out = np.zeros((D, T), np.float32)
    out[:, TC:] = pe.T
    return out


def prep_shared(inp):
    f = np.float32
    S = {}
    S["ident_in"] = np.eye(128, dtype=f)
    sel = np.zeros((32, NE * 128), f)
    for e in range(NE):
        sel[e, e * 128:(e + 1) * 128] = 1.0
    S["sel_in"] = sel
    S["final_g_t"] = _vt(inp["final_g"][None], 8)[0]
    S["ada_w"] = inp["ada_w"]
    S["ada_b_t"] = _vt(inp["ada_b"], 48)
    S["norm1_g_t"] = _vt(inp["norm1_g"], 8)
    S["norm2_g_t"] = _vt(inp["norm2_g"], 8)
    S["posT"] = grid_pos_T()
    S["w_in"] = inp["w_in"]
    cw = np.concatenate([inp["hy_conv_w"], inp["rw_conv_w"]], axis=-1)
    cb = np.concatenate([inp["hy_conv_b"], inp["rw_conv_b"]], axis=-1)[:, None]
    c4 = np.concatenate([cw, cb], axis=1)
    S["conv_t"] = np.ascontiguousarray(c4.reshape(L, 4, 26, 128).transpose(0, 3, 2, 1))
    S["w_branch"] = inp["w_branch"]
    S["w_out"] = inp["w_out"]
    S["router_w_t"] = np.ascontiguousarray(inp["router_w"].reshape(L, 8, 128, NE).transpose(0, 2, 1, 3))
    S["router_b"] = inp["router_b"]
    S["moe_w1"] = inp["moe_w1"]
    S["moe_w2"] = inp["moe_w2"]
    S["moe_b1_t"] = np.ascontiguousarray(inp["moe_b1"].reshape(L, NE, 16, 128).transpose(0, 3, 1, 2))
    S["moe_b2_t"] = np.ascontiguousarray(inp["moe_b2"].reshape(L, NE, 8, 128).transpose(0, 3, 1, 2))
    def st_layout(a):
        return np.ascontiguousarray(a.reshape(L, 2, 16, 2, 64).transpose(0, 1, 3, 4, 2).reshape(L, 2, 128, 16))
    S["s5_a_re_t"] = st_layout(inp["s5_a_re"])
    S["s5_a_im_t"] = st_layout(inp["s5_a_im"])
    S["s5_ldt_t"] = st_layout(np.broadcast_to(inp["s5_log_dt"][..., None], (L, 2, 32, 64)))
    BT = np.zeros((L, 16, 32, 256), f)
    CT = np.zeros((L, 16, 128, 64), f)
    for ri, (bb, cc) in enumerate(((inp["s5_b_re"], inp["s5_c_re"]), (inp["s5_b_im"], inp["s5_c_im"]))):
        for gg in range(2):
            bg = bb.reshape(L, 16, 2, 64, 16)[:, :, gg]
            cg = cc.reshape(L, 16, 2, 16, 64)[:, :, gg]
            BT[:, :, gg * 16:(gg + 1) * 16, ri * 128 + gg * 64: ri * 128 + (gg + 1) * 64] = bg.transpose(0, 1, 3, 2)
            CT[:, :, gg * 64:(gg + 1) * 64, ri * 32 + gg * 16: ri * 32 + (gg + 1) * 16] = cg.transpose(0, 1, 3, 2)
    S["s5_BT"] = BT
    S["s5_CT"] = CT
    S["s5_d_t"] = _vt(inp["s5_d"], 4)
    S["s5_glu_w"] = inp["s5_glu_w"]
    S["s5_glu_b_t"] = _vt(inp["s5_glu_b"], 8)
    S["jiota"] = np.ascontiguousarray(np.broadcast_to(np.arange(512, dtype=f)[None], (128, 512)))
    S.update(hyena_consts(TL))
    S.update(hyena_consts(TC))
    for k in ("hy_w1", "hy_w2", "hy_w3", "hy_b3", "hy_bias"):
        S[k] = inp[k]
    S["hy_pv_t"] = np.ascontiguousarray(np.stack([inp["hy_b1"], inp["hy_f1"], inp["hy_b2"], inp["hy_f2"]], axis=-1))
    S["Jm_in"] = np.ascontiguousarray(np.eye(128, dtype=f)[::-1])
    bo = np.zeros((128, 128), f); bo[:64, :64] = 1.0; bo[64:, 64:] = 1.0
    S["BO_in"] = bo
    m16 = np.zeros((16, 512), f)
    for r_ in range(16):
        d_, j_ = r_ // 8, (r_ % 8) // 2
        m16[r_, d_ * 256 + j_ * 64: d_ * 256 + (j_ + 1) * 64] = 1.0
    S["mask16_in"] = m16
    mh = np.zeros((128, 2), f); mh[:64, 0] = 1.0; mh[64:, 1] = 1.0
    S["maskhh_in"] = mh
    rwc = np.zeros((128, 4), f); rwc[:, 0] = 1.0; rwc[:, 1] = -0.5; rwc[:, 2] = 64e-5
    S["rwc_in"] = rwc
    S["rw_vec_t"] = np.ascontiguousarray(np.stack([_vt(inp["rw_k_k"], 4), _vt(inp["rw_k_a"], 4), _vt(inp["rw_r_k"].reshape(L, 512), 4),
                                                    _vt(inp["rw_ln_g"], 4), _vt(inp["rw_ln_b"], 4)], axis=-1))
    S["rw_w0a0_t"] = np.ascontiguousarray(np.stack([_vt(inp["rw_w0"][:, 0], 4), _vt(inp["rw_w0"][:, 1], 4),
                                                     _vt(inp["rw_a0"][:, 0], 4), _vt(inp["rw_a0"][:, 1], 4)], axis=-1))
    for k in ("rw_w_up", "rw_a_up", "rw_g_up"):
        S[k] = inp[k]
    return S


def prep_core(inp, b):
    f = np.float32
    Cc = {}
    xcat = np.concatenate([inp["ctx"][b], inp["x"][b]], axis=0)
    Cc["xT"] = np.ascontiguousarray(xcat.T)
    cc = np.stack([inp["c"][b], inp["c_ctx"]], axis=-1)
    Cc["ccT"] = np.ascontiguousarray(cc.reshape(8, 128, 2).transpose(1, 0, 2).reshape(128, 16))
    return Cc


TWO_PI = 2.0 * math.pi


def emit_sincos(P, eng_sel, ang, ang_key, sin_t, cos_t, out_key, tmpf, tmpi, shape_sl, consts=None):
    sl = shape_sl
    for (dst, shift) in ((sin_t, 0.0), (cos_t, 0.5 * math.pi)):
        P.op("dve", lambda e, shift=shift: e.tensor_scalar(out=tmpi[sl], in0=ang[sl], scalar1=shift, scalar2=1.0 / TWO_PI, op0=ALU.add, op1=ALU.mult),
             reads=[ang_key], writes=[out_key + "_ti"])
        P.op("dve", lambda e: e.tensor_copy(out=tmpf[sl], in_=tmpi[sl]), reads=[out_key + "_ti"], writes=[out_key + "_tf"])
        P.op("dve", lambda e: e.scalar_tensor_tensor(out=tmpf[sl], in0=tmpf[sl], scalar=-TWO_PI, in1=ang[sl], op0=ALU.mult, op1=ALU.add),
             reads=[out_key + "_tf", ang_key], writes=[out_key + "_tf"])
        P.op("dve", lambda e, shift=shift: e.tensor_scalar(out=tmpf[sl], in0=tmpf[sl], scalar1=shift, scalar2=-3.1415925, op0=ALU.add, op1=ALU.max),
             reads=[out_key + "_tf"], writes=[out_key + "_tf"])
        P.op("dve", lambda e: e.tensor_scalar(out=tmpf[sl], in0=tmpf[sl], scalar1=3.1415925, scalar2=None, op0=ALU.min),
             reads=[out_key + "_tf"], writes=[out_key + "_tf"])
        P.op("act", lambda e, dst=dst: e.activation(out=dst[sl], in_=tmpf[sl], func=AF.Sin), reads=[out_key + "_tf"], writes=[out_key])


def stage_s5(C, K, PS, l, need_ctx):
    nc, P = C.nc, C.P
    US5 = C.dram("US5", [512, T])
    YPRE = C.dram("YS5PRE", [512, T])
    YS5 = C.dram("YS5", [512, T], BF16)
    a_re = C.inp("s5_a_re_t", [L, 2, 128, 16])
    a_im = C.inp("s5_a_im_t", [L, 2, 128, 16])
    ldt = C.inp("s5_ldt_t", [L, 2, 128, 16])
    BTd = C.inp("s5_BT", [L, 16, 32, 256])
    CTd = C.inp("s5_CT", [L, 16, 128, 64])
    dsk = C.inp("s5_d_t", [L, 128, 4])
    gw = C.inp("s5_glu_w", [L, 512, 1024])
    gbd = C.inp("s5_glu_b_t", [L, 128, 8])
    jio = C.inp("jiota", [128, 512])
    with ExitStack() as es:
        J = sbt(es, nc, "jio", [128, 512], F32)
        P.dma("sp", lambda e: e.dma_start(out=J[:], in_=jio[:, :]), writes=["jio"])
        sm = {}
        for nm in ("are", "aim", "dt", "rho", "phi", "sn", "cs", "lbr", "lbi", "den", "nr", "cr", "ci", "ncr", "t1", "t2"):
            sm[nm] = [sbt(es, nc, f"s5{nm}{d}", [128, 16], F32) for d in range(2)]
        smi = sbt(es, nc, "s5smi", [128, 16], I32)
        for d in range(2):
            g = lambda nm: sm[nm][d]
            P.dma("sp", lambda e, d=d: e.dma_start(out=sm["are"][d][:], in_=a_re[l, d]), writes=[f"are{d}"])
            P.dma("sp", lambda e, d=d: e.dma_start(out=sm["aim"][d][:], in_=a_im[l, d]), writes=[f"aim{d}"])
            P.dma("sp", lambda e, d=d: e.dma_start(out=sm["dt"][d][:], in_=ldt[l, d]), writes=[f"dt{d}"])
            P.op("act", lambda e, d=d: e.activation(out=sm["dt"][d][:], in_=sm["dt"][d][:], func=AF.Exp), reads=[f"dt{d}"], writes=[f"dt{d}"])
            P.op("dve", lambda e, d=d: e.tensor_tensor(out=sm["t1"][d][:], in0=sm["are"][d][:], in1=sm["dt"][d][:], op=ALU.mult), reads=[f"are{d}", f"dt{d}"], writes=[f"t1{d}"])
            P.op("act", lambda e, d=d: e.activation(out=sm["rho"][d][:], in_=sm["t1"][d][:], func=AF.Exp), reads=[f"t1{d}"], writes=[f"rho{d}"])
            P.op("dve", lambda e, d=d: e.tensor_tensor(out=sm["phi"][d][:], in0=sm["aim"][d][:], in1=sm["dt"][d][:], op=ALU.mult), reads=[f"aim{d}", f"dt{d}"], writes=[f"phi{d}"])
            emit_sincos(P, None, sm["phi"][d], f"phi{d}", sm["sn"][d], sm["cs"][d], f"sc{d}", sm["t2"][d], smi, slice(None))
            P.op("dve", lambda e, d=d: e.tensor_tensor(out=sm["lbr"][d][:], in0=sm["rho"][d][:], in1=sm["cs"][d][:], op=ALU.mult), reads=[f"rho{d}", f"sc{d}"], writes=[f"lbr{d}"])
            P.op("dve", lambda e, d=d: e.tensor_tensor(out=sm["lbi"][d][:], in0=sm["rho"][d][:], in1=sm["sn"][d][:], op=ALU.mult), reads=[f"rho{d}", f"sc{d}"], writes=[f"lbi{d}"])
            P.op("dve", lambda e, d=d: e.tensor_tensor(out=sm["den"][d][:], in0=sm["are"][d][:], in1=sm["are"][d][:], op=ALU.mult), reads=[f"are{d}"], writes=[f"den{d}"])
            P.op("dve", lambda e, d=d: e.tensor_tensor(out=sm["t1"][d][:], in0=sm["aim"][d][:], in1=sm["aim"][d][:], op=ALU.mult), reads=[f"aim{d}"], writes=[f"t1{d}"])
            P.op("dve", lambda e, d=d: e.tensor_tensor(out=sm["den"][d][:], in0=sm["den"][d][:], in1=sm["t1"][d][:], op=ALU.add), reads=[f"den{d}", f"t1{d}"], writes=[f"den{d}"])
            P.op("dve", lambda e, d=d: e.reciprocal(out=sm["den"][d][:], in_=sm["den"][d][:]), reads=[f"den{d}"], writes=[f"den{d}"])
            P.op("dve", lambda e, d=d: e.tensor_scalar(out=sm["nr"][d][:], in0=sm["lbr"][d][:], scalar1=-1.0, scalar2=None, op0=ALU.add), reads=[f"lbr{d}"], writes=[f"nr{d}"])
            P.op("dve", lambda e, d=d: e.tensor_tensor(out=sm["cr"][d][:], in0=sm["nr"][d][:], in1=sm["are"][d][:], op=ALU.mult), reads=[f"nr{d}", f"are{d}"], writes=[f"cr{d}"])
            P.op("dve", lambda e, d=d: e.tensor_tensor(out=sm["t1"][d][:], in0=sm["lbi"][d][:], in1=sm["aim"][d][:], op=ALU.mult), reads=[f"lbi{d}", f"aim{d}", f"den{d}"], writes=[f"t1{d}"])
            P.op("dve", lambda e, d=d: e.tensor_tensor(out=sm["cr"][d][:], in0=sm["cr"][d][:], in1=sm["t1"][d][:], op=ALU.add), reads=[f"cr{d}", f"t1{d}"], writes=[f"cr{d}"])
            P.op("dve", lambda e, d=d: e.tensor_tensor(out=sm["cr"][d][:], in0=sm["cr"][d][:], in1=sm["den"][d][:], op=ALU.mult), reads=[f"cr{d}", f"den{d}"], writes=[f"cr{d}"])
            P.op("dve", lambda e, d=d: e.tensor_tensor(out=sm["ci"][d][:], in0=sm["lbi"][d][:], in1=sm["are"][d][:], op=ALU.mult), reads=[f"lbi{d}", f"are{d}"], writes=[f"ci{d}"])
            P.op("dve", lambda e, d=d: e.tensor_tensor(out=sm["t1"][d][:], in0=sm["nr"][d][:], in1=sm["aim"][d][:], op=ALU.mult), reads=[f"nr{d}", f"aim{d}", f"cr{d}"], writes=[f"t1{d}"])
            P.op("dve", lambda e, d=d: e.tensor_tensor(out=sm["ci"][d][:], in0=sm["ci"][d][:], in1=sm["t1"][d][:], op=ALU.subtract), reads=[f"ci{d}", f"t1{d}"], writes=[f"ci{d}"])
            P.op("dve", lambda e, d=d: e.tensor_tensor(out=sm["ci"][d][:], in0=sm["ci"][d][:], in1=sm["den"][d][:], op=ALU.mult), reads=[f"ci{d}", f"den{d}"], writes=[f"ci{d}"])
            P.op("dve", lambda e, d=d: e.tensor_scalar(out=sm["ncr"][d][:], in0=sm["cr"][d][:], scalar1=-1.0, scalar2=None, op0=ALU.mult), reads=[f"cr{d}"], writes=[f"ncr{d}"])
        ang = sbt(es, nc, "s5ang", [128, 512], F32)
        tf = sbt(es, nc, "s5tf", [128, 512], F32)
        ti_ = sbt(es, nc, "s5ti", [128, 512], I32)
        sn = sbt(es, nc, "s5sn", [128, 512], F32)
        cs = sbt(es, nc, "s5cs", [128, 512], F32)
        trt = sbt(es, nc, "s5tr", [128, 512], F32)
        tit = sbt(es, nc, "s5tit", [128, 512], F32)
        rho_t = sbt(es, nc, "s5rhot", [128, 512], F32)
        u32 = sbt(es, nc, "s5u", [32, T], F32)
        u32r = sbt(es, nc, "s5ur", [32, T], F32)
        bt = sbt(es, nc, "s5bt", [32, 256], F32)
        ctf = sbt(es, nc, "s5ctf", [128, 64], F32)
        ctb = sbt(es, nc, "s5ctb", [128, 64], BF16)
        H = {(d, ri): sbt(es, nc, f"s5H{d}{ri}", [128, T], BF16) for d in range(2) for ri in range(2)}
        w = [sbt(es, nc, f"s5w{i}", [128, 512], F32) for i in range(6)]
        q = [sbt(es, nc, f"s5q{i}", [128, 512], F32) for i in range(2)]
        qi0 = [sbt(es, nc, f"s5qi{i}", [128, 1], F32) for i in range(2)]
        hl = [sbt(es, nc, f"s5hl{i}", [128, 1], F32) for i in range(2)]
        ns1 = sbt(es, nc, "s5ns1", [128, 1], F32)
        sm1 = sbt(es, nc, "s5sm1", [128, 1], F32)
        y32 = [sbt(es, nc, f"s5y32_{i}", [32, 512], F32) for i in range(2)]
        fchunks = list(CHUNKS)
        bchunks = [(TL, TC)] + [(512 * i, 512) for i in range(8)]
        for st in range(16):
            P.dma("sp", lambda e, st=st: e.dma_start(out=u32[:], in_=US5[32 * st:32 * st + 32, :]), reads=[f"US5{st // 4}"], writes=["s5u"])
            P.op("pool", lambda e: e.tensor_copy(out=u32r[:], in_=u32[:, ::-1]), reads=["s5u"], writes=["s5ur"])
            P.dma("act", lambda e, st=st: e.dma_start(out=bt[:], in_=BTd[l, st]), writes=["s5bt"])
            P.dma("act", lambda e, st=st: e.dma_start(out=ctf[:], in_=CTd[l, st]), writes=["s5ctf"])
            P.op("pool", lambda e: e.tensor_copy(out=ctb[:, 0:32], in_=ctf[:, 0:32]), reads=["s5ctf"], writes=["s5ctb"])
            P.op("pool", lambda e: e.tensor_scalar(out=ctb[:, 32:64], in0=ctf[:, 32:64], scalar1=-1.0, scalar2=None, op0=ALU.mult), reads=["s5ctf"], writes=["s5ctb"])
            for d in range(2):
                phi = sm["phi"][d][:, st:st + 1]
                P.op("dve", lambda e, phi=phi: e.tensor_scalar(out=ang[:], in0=J[:], scalar1=phi, scalar2=None, op0=ALU.mult), reads=["jio", f"phi{d}"], writes=["s5ang"])
                emit_sincos(P, None, ang, "s5ang", sn, cs, "s5sc", tf, ti_, slice(None))
                P.op("dve", lambda e, d=d, st=st: e.tensor_scalar(out=rho_t[:], in0=J[:], scalar1=0.0, scalar2=sm["rho"][d][:, st:st + 1], op0=ALU.mult, op1=ALU.add), reads=["jio", f"rho{d}"], writes=["s5rhot"])
                P.op("dve", lambda e, d=d, st=st: e.tensor_scalar(out=trt[:], in0=cs[:], scalar1=sm["cr"][d][:, st:st + 1], scalar2=None, op0=ALU.mult), reads=["s5sc", f"cr{d}"], writes=["s5tr"])
                P.op("dve", lambda e, d=d, st=st: e.scalar_tensor_tensor(out=trt[:], in0=sn[:], scalar=sm["ci"][d][:, st:st + 1], in1=trt[:], op0=ALU.mult, op1=ALU.add), reads=["s5sc", f"ci{d}", "s5tr"], writes=["s5tr"])
                P.op("dve", lambda e, d=d, st=st: e.tensor_scalar(out=tit[:], in0=cs[:], scalar1=sm["ci"][d][:, st:st + 1], scalar2=None, op0=ALU.mult), reads=["s5sc", f"ci{d}"], writes=["s5tit"])
                P.op("dve", lambda e, d=d, st=st: e.scalar_tensor_tensor(out=tit[:], in0=sn[:], scalar=sm["ncr"][d][:, st:st + 1], in1=tit[:], op0=ALU.mult, op1=ALU.add), reads=["s5sc", f"ncr{d}", "s5tit"], writes=["s5tit"])
                P.op("dve", lambda e: e.tensor_scalar(out=ns1[:], in0=sn[:, 1:2], scalar1=-1.0, scalar2=None, op0=ALU.mult), reads=["s5sc"], writes=["s5ns1"])
                usrc = u32 if d == 0 else u32r
                ukey = "s5u" if d == 0 else "s5ur"
                chunks = fchunks if d == 0 else bchunks
                for cidx, (c0, ln) in enumerate(chunks):
                    pX, pY = PS[(2 * cidx) % 4], PS[(2 * cidx + 1) % 4]
                    kX, kY = f"ps{(2 * cidx) % 4}", f"ps{(2 * cidx + 1) % 4}"
                    P.op("pe", lambda e, pX=pX, c0=c0, ln=ln, usrc=usrc: e.matmul(pX[:, :ln], lhsT=bt[:, 0:128], rhs=usrc[:, c0:c0 + ln], start=True, stop=True), reads=["s5bt", ukey], writes=[kX])
                    P.op("pe", lambda e, pY=pY, c0=c0, ln=ln, usrc=usrc: e.matmul(pY[:, :ln], lhsT=bt[:, 128:256], rhs=usrc[:, c0:c0 + ln], start=True, stop=True), reads=["s5bt", ukey], writes=[kY])
                    P.op("dve", lambda e, pX=pX, ln=ln: e.tensor_tensor(out=w[0][:, :ln], in0=pX[:, :ln], in1=trt[:, :ln], op=ALU.mult), reads=[kX, "s5tr"], writes=["s5w0"])
                    P.op("dve", lambda e, pY=pY, ln=ln: e.tensor_tensor(out=w[1][:, :ln], in0=pY[:, :ln], in1=tit[:, :ln], op=ALU.mult), reads=[kY, "s5tit"], writes=["s5w1"])
                    P.op("dve", lambda e, pY=pY, ln=ln: e.tensor_tensor(out=w[2][:, :ln], in0=pY[:, :ln], in1=trt[:, :ln], op=ALU.mult), reads=[kY, "s5tr"], writes=["s5w2"])
                    P.op("dve", lambda e, pX=pX, ln=ln: e.tensor_tensor(out=w[3][:, :ln], in0=pX[:, :ln], in1=tit[:, :ln], op=ALU.mult), reads=[kX, "s5tit"], writes=["s5w3"])
                    P.op("pool", lambda e, ln=ln: e.tensor_tensor(out=w[0][:, :ln], in0=w[0][:, :ln], in1=w[1][:, :ln], op=ALU.subtract), reads=["s5w0", "s5w1"], writes=["s5w0"])
                    P.op("pool", lambda e, ln=ln: e.tensor_tensor(out=w[2][:, :ln], in0=w[2][:, :ln], in1=w[3][:, :ln], op=ALU.add), reads=["s5w2", "s5w3"], writes=["s5w2"])
                    if cidx == 0:
                        P.op("pool", lambda e: e.memset(qi0[0][:], 0.0), writes=["s5qi0"])
                        P.op("pool", lambda e: e.memset(qi0[1][:], 0.0), writes=["s5qi1"])
                    else:
                        P.op("dve", lambda e: e.tensor_tensor(out=sm1[:], in0=hl[0][:], in1=cs[:, 1:2], op=ALU.mult), reads=["s5hl0", "s5sc"], writes=["s5sm1"])
                        P.op("dve", lambda e: e.scalar_tensor_tensor(out=qi0[0][:], in0=hl[1][:], scalar=ns1[:, 0:1], in1=sm1[:], op0=ALU.mult, op1=ALU.add), reads=["s5hl1", "s5ns1", "s5sm1"], writes=["s5qi0"])
                        P.op("dve", lambda e: e.tensor_tensor(out=sm1[:], in0=hl[1][:], in1=cs[:, 1:2], op=ALU.mult), reads=["s5hl1", "s5sc", "s5qi0"], writes=["s5sm1"])
                        P.op("dve", lambda e: e.scalar_tensor_tensor(out=qi0[1][:], in0=hl[0][:], scalar=sn[:, 1:2], in1=sm1[:], op0=ALU.mult, op1=ALU.add), reads=["s5hl0", "s5sc", "s5sm1"], writes=["s5qi1"])
                    P.op("dve", lambda e, ln=ln: e.tensor_tensor_scan(out=q[0][:, :ln], data0=rho_t[:, :ln], data1=w[0][:, :ln], initial=qi0[0][:, 0:1], op0=ALU.mult, op1=ALU.add), reads=["s5rhot", "s5w0", "s5qi0"], writes=["s5q0"])
                    P.op("dve", lambda e, ln=ln: e.tensor_tensor_scan(out=q[1][:, :ln], data0=rho_t[:, :ln], data1=w[2][:, :ln], initial=qi0[1][:, 0:1], op0=ALU.mult, op1=ALU.add), reads=["s5rhot", "s5w2", "s5qi1"], writes=["s5q1"])
                    P.op("pool", lambda e, ln=ln: e.tensor_tensor(out=w[1][:, :ln], in0=q[0][:, :ln], in1=cs[:, :ln], op=ALU.mult), reads=["s5q0", "s5sc", "s5w1"], writes=["s5w1"])
                    P.op("pool", lambda e, ln=ln: e.tensor_tensor(out=w[3][:, :ln], in0=q[1][:, :ln], in1=sn[:, :ln], op=ALU.mult), reads=["s5q1", "s5sc", "s5w3"], writes=["s5w3"])
                    P.op("pool", lambda e, ln=ln: e.tensor_tensor(out=w[4][:, :ln], in0=q[0][:, :ln], in1=sn[:, :ln], op=ALU.mult), reads=["s5q0", "s5sc"], writes=["s5w4"])
                    P.op("pool", lambda e, ln=ln: e.tensor_tensor(out=w[5][:, :ln], in0=q[1][:, :ln], in1=cs[:, :ln], op=ALU.mult), reads=["s5q1", "s5sc"], writes=["s5w5"])
                    P.op("pool", lambda e, ln=ln: e.tensor_tensor(out=w[1][:, :ln], in0=w[1][:, :ln], in1=w[3][:, :ln], op=ALU.subtract), reads=["s5w1", "s5w3"], writes=["s5w1"])
                    P.op("pool", lambda e, ln=ln: e.tensor_tensor(out=w[4][:, :ln], in0=w[4][:, :ln], in1=w[5][:, :ln], op=ALU.add), reads=["s5w4", "s5w5"], writes=["s5w4"])
                    P.op("act", lambda e, ln=ln: e.activation(out=hl[0][:], in_=w[1][:, ln - 1:ln], func=AF.Copy), reads=["s5w1"], writes=["s5hl0"])
                    P.op("act", lambda e, ln=ln: e.activation(out=hl[1][:], in_=w[4][:, ln - 1:ln], func=AF.Copy), reads=["s5w4"], writes=["s5hl1"])
                    if d == 0:
                        dr, di = H[(0, 0)][:, c0:c0 + ln], H[(0, 1)][:, c0:c0 + ln]
                    else:
                        n0 = T - c0 - ln
                        dr, di = H[(1, 0)][:, n0:n0 + ln][:, ::-1], H[(1, 1)][:, n0:n0 + ln][:, ::-1]
                    P.op("act", lambda e, dr=dr, ln=ln: e.activation(out=dr, in_=w[1][:, :ln], func=AF.Copy), reads=["s5w1"], writes=[f"s5H{d}0"])
                    P.op("act", lambda e, di=di, ln=ln: e.activation(out=di, in_=w[4][:, :ln], func=AF.Copy), reads=["s5w4"], writes=[f"s5H{d}1"])
            for ci, (c0, ln) in enumerate(CHUNKS):
                if ci == 0 and not need_ctx:
                    continue
                ps = PS[4 + ci % 2]
                pk = f"ps{4 + ci % 2}"
                n = 0
                for d in range(2):
                    for ri in range(2):
                        P.op("pe", lambda e, ps=ps, d=d, ri=ri, c0=c0, ln=ln, n=n: e.matmul(ps[:32, :ln], lhsT=ctb[:, ri * 32:(ri + 1) * 32], rhs=H[(d, ri)][:, c0:c0 + ln], start=(n == 0), stop=(n == 3)),
                             reads=["s5ctb", f"s5H{d}{ri}"], writes=[pk])
                        n += 1
                yy = y32[ci % 2]
                P.op("act", lambda e, ps=ps, yy=yy, ln=ln: e.activation(out=yy[:, :ln], in_=ps[:32, :ln], func=AF.Copy), reads=[pk], writes=[f"s5y32_{ci % 2}"])
                P.dma("sp", lambda e, yy=yy, st=st, c0=c0, ln=ln: e.dma_start(out=YPRE[32 * st:32 * st + 32, c0:c0 + ln], in_=yy[:, :ln]), reads=[f"s5y32_{ci % 2}"], writes=["YPRE"])
        P.barrier()
    with ExitStack() as es:
        gwst = [sbt(es, nc, f"s5gws{i}", [128, 1024], F32) for i in range(2)]
        gwb = sbt(es, nc, "s5gwb", [128, 4, 1024], BF16)
        gbt = sbt(es, nc, "s5gbt", [128, 8], F32)
        dt_ = sbt(es, nc, "s5dsk", [128, 4], F32)
        P.dma("sp", lambda e: e.dma_start(out=gbt[:], in_=gbd[l]), writes=["s5gbt"])
        P.dma("sp", lambda e: e.dma_start(out=dt_[:], in_=dsk[l]), writes=["s5dsk"])
        for k in range(4):
            P.dma("sp", lambda e, k=k: e.dma_start(out=gwst[k % 2][:], in_=gw[l, k * 128:(k + 1) * 128, :]), writes=[f"s5gws{k % 2}"])
            P.op("pool", lambda e, k=k: e.tensor_copy(out=gwb[:, k, :], in_=gwst[k % 2][:]), reads=[f"s5gws{k % 2}"], writes=["s5gwb"])
        yp = [sbt(es, nc, f"s5yp{i}", [128, 4, 512], F32) for i in range(2)]
        uu = [sbt(es, nc, f"s5uu{i}", [128, 4, 512], F32) for i in range(2)]
        yg = sbt(es, nc, "s5yg", [128, 4, 512], BF16)
        lin = [sbt(es, nc, f"s5lin{i}", [128, 512], F32) for i in range(2)]
        sg = [sbt(es, nc, f"s5sg{i}", [128, 512], F32) for i in range(2)]
        yo = [sbt(es, nc, f"s5yo{i}", [128, 4, 512], BF16) for i in range(2)]
        YPv = YPRE.rearrange("(k p) t -> p k t", p=128)
        USv = US5.rearrange("(k p) t -> p k t", p=128)
        YSv = YS5.rearrange("(k p) t -> p k t", p=128)
        for ci, (c0, ln) in enumerate(CHUNKS):
            if ci == 0 and not need_ctx:
                continue
            i = ci % 2
            P.dma("sp", lambda e, i=i, c0=c0, ln=ln: e.dma_start(out=yp[i][:, :, :ln], in_=YPv[:, :, c0:c0 + ln]), reads=["YPRE"], writes=[f"s5yp{i}"])
            P.dma("act", lambda e, i=i, c0=c0, ln=ln: e.dma_start(out=uu[i][:, :, :ln], in_=USv[:, :, c0:c0 + ln]), reads=[f"US5{k}" for k in range(4)], writes=[f"s5uu{i}"])
            for k in range(4):
                P.op("dve", lambda e, i=i, k=k, ln=ln: e.scalar_tensor_tensor(out=yp[i][:, k, :ln], in0=uu[i][:, k, :ln], scalar=dt_[:, k:k + 1], in1=yp[i][:, k, :ln], op0=ALU.mult, op1=ALU.add),
                     reads=[f"s5yp{i}", f"s5uu{i}", "s5dsk"], writes=[f"s5yp{i}"])
                P.op("act", lambda e, i=i, k=k, ln=ln: e.activation(out=yg[:, k, :ln], in_=yp[i][:, k, :ln], func=AF.Gelu), reads=[f"s5yp{i}"], writes=[f"s5yg{k}"])
            for ot in range(4):
                pl, pg = PS[ot % 2], PS[2 + ot % 2]
                kl, kg = f"ps{ot % 2}", f"ps{2 + ot % 2}"
                for k in range(4):
                    P.op("pe", lambda e, pl=pl, k=k, ot=ot, ln=ln: e.matmul(pl[:, :ln], lhsT=gwb[:, k, ot * 128:(ot + 1) * 128], rhs=yg[:, k, :ln], start=(k == 0), stop=(k == 3)), reads=["s5gwb", f"s5yg{k}"], writes=[kl])
                for k in range(4):
                    P.op("pe", lambda e, pg=pg, k=k, ot=ot, ln=ln: e.matmul(pg[:, :ln], lhsT=gwb[:, k, (ot + 4) * 128:(ot + 5) * 128], rhs=yg[:, k, :ln], start=(k == 0), stop=(k == 3)), reads=["s5gwb", f"s5yg{k}"], writes=[kg])
                P.op("act", lambda e, pl=pl, ot=ot, ln=ln: e.activation(out=lin[ot % 2][:, :ln], in_=pl[:, :ln], func=AF.Identity, bias=gbt[:, ot:ot + 1], scale=1.0), reads=[kl, "s5gbt"], writes=[f"s5lin{ot % 2}"])
                P.op("act", lambda e, pg=pg, ot=ot, ln=ln: e.activation(out=sg[ot % 2][:, :ln], in_=pg[:, :ln], func=AF.Sigmoid, bias=gbt[:, ot + 4:ot + 5], scale=1.0), reads=[kg, "s5gbt"], writes=[f"s5sg{ot % 2}"])
                P.op("dve", lambda e, i=i, ot=ot, ln=ln: e.tensor_tensor(out=yo[i][:, ot, :ln], in0=lin[ot % 2][:, :ln], in1=sg[ot % 2][:, :ln], op=ALU.mult), reads=[f"s5lin{ot % 2}", f"s5sg{ot % 2}"], writes=[f"s5yo{i}"])
            P.dma("sp", lambda e, i=i, c0=c0, ln=ln: e.dma_start(out=YSv[:, :, c0:c0 + ln], in_=yo[i][:, :, :ln]), reads=[f"s5yo{i}"], writes=["Y2"])
        P.barrier()


def hyena_consts(n):
    f64 = np.float64
    N2 = 2 * n
    nch = n // 128
    t = np.arange(n, dtype=f64)
    th = 2 * np.pi * (np.arange(n, dtype=f64) + 0.5) / N2
    ang = np.outer(t, th)
    Cm, Sm = np.cos(ang), np.sin(ang)

    def tile_fwd(M):
        return np.ascontiguousarray(M.reshape(nch, 128, nch, 128).transpose(2, 1, 0, 3)).astype(ml_dtypes.bfloat16)

    def tile_inv(M):
        return np.ascontiguousarray((M.T * (2.0 / N2)).reshape(nch, 128, nch, 128).transpose(2, 1, 0, 3)).astype(ml_dtypes.bfloat16)
    out = {f"hyCf{n}": tile_fwd(Cm), f"hySf{n}": tile_fwd(Sm), f"hyCi{n}": tile_inv(Cm), f"hySi{n}": tile_inv(Sm)}
    f32 = np.float32
    tt = np.linspace(0.0, 1.0, n, dtype=f32)[:, None]
    bands = np.linspace(1e-4, 15, 16, dtype=f32)
    a2 = (f32(2 * math.pi) * np.arange(n, dtype=f32) / f32(n))[:, None] * bands
    feats = np.concatenate([tt, np.cos(a2), -np.sin(a2)], axis=-1).astype(f32)
    out[f"hyfeat{n}"] = np.ascontiguousarray(feats.T)
    deltas = np.abs(np.linspace(math.log(1e-2) / 1.5, math.log(1e-2) / 0.3, 512, dtype=f32))
    out[f"hywin{n}"] = np.exp(-tt * deltas).astype(f32)
    return out


def emit_sin(P, src, src_key, dst, dst_key, tf, ti, sl):
    P.op("dve", lambda e: e.tensor_scalar(out=ti[sl], in0=src[sl], scalar1=1.0 / TWO_PI, scalar2=None, op0=ALU.mult), reads=[src_key], writes=[dst_key + "_ti"])
    P.op("dve", lambda e: e.tensor_copy(out=tf[sl], in_=ti[sl]), reads=[dst_key + "_ti"], writes=[dst_key + "_tf"])
    P.op("dve", lambda e: e.scalar_tensor_tensor(out=tf[sl], in0=tf[sl], scalar=-TWO_PI, in1=src[sl], op0=ALU.mult, op1=ALU.add), reads=[dst_key + "_tf", src_key], writes=[dst_key + "_tf"])
    P.op("dve", lambda e: e.tensor_scalar(out=tf[sl], in0=tf[sl], scalar1=-3.1415925, scalar2=3.1415925, op0=ALU.max, op1=ALU.min), reads=[dst_key + "_tf"], writes=[dst_key + "_tf"])
    P.op("act", lambda e: e.activation(out=dst[sl], in_=tf[sl], func=AF.Sin), reads=[dst_key + "_tf"], writes=[dst_key])


def stage_hyena(C, K, PS, l, n, row0):
    nc, P = C.nc, C.P
    nch = n // 128
    UHY = C.dram("UHY", [T, 1536])
    YHY = C.dram("YHY", [512, T], BF16)
    Cf = C.inp(f"hyCf{n}", [nch, 128, nch, 128], BF16)
    Sf = C.inp(f"hySf{n}", [nch, 128, nch, 128], BF16)
    Ci = C.inp(f"hyCi{n}", [nch, 128, nch, 128], BF16)
    Si = C.inp(f"hySi{n}", [nch, 128, nch, 128], BF16)
    featd = C.inp(f"hyfeat{n}", [33, n])
    wind = C.inp(f"hywin{n}", [n, 512])
    w1d = C.inp("hy_w1", [L, 33, 64]); w2d = C.inp("hy_w2", [L, 64, 64]); w3d = C.inp("hy_w3", [L, 64, 2048])
    pv = C.inp("hy_pv_t", [L, 64, 4])
    b3d = C.inp("hy_b3", [L, 2048])
    biasd = C.inp("hy_bias", [L, 2, 512])
    FA = [C.dram(f"hyFA{o}_{n}", [n, 512], BF16) for o in range(2)]
    FD = [C.dram(f"hyFD{o}_{n}", [n, 512], BF16) for o in range(2)]
    HR = [C.dram(f"hyHR{o}_{n}", [n, 512]) for o in range(2)]
    HI = [C.dram(f"hyHI{o}_{n}", [n, 512]) for o in range(2)]
    tg = f"hy{n}"
    with ExitStack() as es:
        ft_ = sbt(es, nc, tg + "feat", [33, n], F32)
        w1 = sbt(es, nc, tg + "w1", [33, 64], F32); w2 = sbt(es, nc, tg + "w2", [64, 64], F32); w3 = sbt(es, nc, tg + "w3", [64, 2048], F32)
        pvt = sbt(es, nc, tg + "pv", [64, 4], F32)
        b3 = sbt(es, nc, tg + "b3", [1, 2048], F32)
        on1 = sbt(es, nc, tg + "on1", [1, 128], F32)
        a1 = sbt(es, nc, tg + "a1", [64, 512], F32); h1 = sbt(es, nc, tg + "h1", [64, 512], F32); h2 = sbt(es, nc, tg + "h2", [64, 512], F32)
        tf = sbt(es, nc, tg + "tf", [64, 512], F32); ti = sbt(es, nc, tg + "ti", [64, 512], I32)
        win = [sbt(es, nc, tg + f"win{i}", [128, 512], F32) for i in range(2)]
        fw = [sbt(es, nc, tg + f"fw{i}", [128, 512], F32) for i in range(2)]
        bw = [sbt(es, nc, tg + f"bw{i}", [128, 512], F32) for i in range(2)]
        ao = [sbt(es, nc, tg + f"ao{i}", [128, 512], BF16) for i in range(2)]
        do = [sbt(es, nc, tg + f"do{i}", [128, 512], BF16) for i in range(2)]
        P.dma("sp", lambda e: e.dma_start(out=ft_[:], in_=featd[:, :]), writes=[tg + "feat"])
        P.dma("sp", lambda e: e.dma_start(out=w1[:], in_=w1d[l]), writes=[tg + "w"])
        P.dma("sp", lambda e: e.dma_start(out=w2[:], in_=w2d[l]), writes=[tg + "w"])
        P.dma("sp", lambda e: e.dma_start(out=w3[:], in_=w3d[l]), writes=[tg + "w"])
        P.dma("sp", lambda e: e.dma_start(out=pvt[:], in_=pv[l]), writes=[tg + "w"])
        P.dma("sp", lambda e: e.dma_start(out=b3[:], in_=b3d[l:l + 1, :]), writes=[tg + "w"])
        P.op("pool", lambda e: e.memset(on1[:], 1.0), writes=[tg + "on1"])
        P.barrier()
        for c5 in range(max(1, n // 512)):
            ln = min(512, n)
            p0 = c5 * 512
            sl = (slice(None), slice(0, ln))
            P.op("pe", lambda e, p0=p0, ln=ln: e.matmul(PS[0][:64, :ln], lhsT=w1[:, :], rhs=ft_[:, p0:p0 + ln], start=True, stop=True), reads=[tg + "feat"], writes=["ps0"])
            P.op("dve", lambda e, ln=ln: e.tensor_scalar(out=a1[:, :ln], in0=PS[0][:64, :ln], scalar1=pvt[:, 0:1], scalar2=pvt[:, 1:2], op0=ALU.add, op1=ALU.mult), reads=["ps0"], writes=[tg + "a1"])
            emit_sin(P, a1, tg + "a1", h1, tg + "h1", tf, ti, sl)
            P.op("pe", lambda e, ln=ln: e.matmul(PS[1][:64, :ln], lhsT=w2[:, :], rhs=h1[:, :ln], start=True, stop=True), reads=[tg + "h1"], writes=["ps1"])
            P.op("dve", lambda e, ln=ln: e.tensor_scalar(out=a1[:, :ln], in0=PS[1][:64, :ln], scalar1=pvt[:, 2:3], scalar2=pvt[:, 3:4], op0=ALU.add, op1=ALU.mult), reads=["ps1", tg + "h1_tf"], writes=[tg + "a1"])
            emit_sin(P, a1, tg + "a1", h2, tg + "h2", tf, ti, sl)
            for bl in range(ln // 128):
                blk = c5 * 4 + bl
                i = blk % 2
                P.dma("act", lambda e, i=i, blk=blk: e.dma_start(out=win[i][:], in_=wind[blk * 128:(blk + 1) * 128, :]), writes=[tg + f"win{i}"])
                for cc in range(4):
                    ps = PS[2 + cc]
                    P.op("pe", lambda e, ps=ps, bl=bl, cc=cc: e.matmul(ps[:, :], lhsT=h2[:, bl * 128:(bl + 1) * 128], rhs=w3[:, cc * 512:(cc + 1) * 512], start=True, stop=False), reads=[tg + "h2"], writes=[f"ps{2 + cc}"])
                    P.op("pe", lambda e, ps=ps, cc=cc: e.matmul(ps[:, :], lhsT=on1[:, :], rhs=b3[:, cc * 512:(cc + 1) * 512], start=False, stop=True), reads=[tg + "on1"], writes=[f"ps{2 + cc}"])
                for o in range(2):
                    P.op("dve", lambda e, i=i, o=o: e.tensor_tensor(out=fw[o][:], in0=PS[2 + 2 * o][:, :], in1=win[i][:], op=ALU.mult), reads=[f"ps{2 + 2 * o}", tg + f"win{i}"], writes=[tg + f"fw{o}"])
                    P.op("dve", lambda e, i=i, o=o: e.tensor_tensor(out=bw[o][:], in0=PS[3 + 2 * o][:, :], in1=win[i][:], op=ALU.mult), reads=[f"ps{3 + 2 * o}", tg + f"win{i}"], writes=[tg + f"bw{o}"])
                    if blk == 0:
                        P.op("dve", lambda e, o=o: e.memset(bw[o][0:1, :], 0.0), reads=[tg + f"bw{o}"], writes=[tg + f"bw{o}"])
                    P.op("pool", lambda e, o=o: e.tensor_tensor(out=ao[o][:], in0=fw[o][:], in1=bw[o][:], op=ALU.add), reads=[tg + f"fw{o}", tg + f"bw{o}"], writes=[tg + f"ao{o}"])
                    P.op("pool", lambda e, o=o: e.tensor_tensor(out=do[o][:], in0=fw[o][:], in1=bw[o][:], op=ALU.subtract), reads=[tg + f"fw{o}", tg + f"bw{o}"], writes=[tg + f"do{o}"])
                    P.dma("sp", lambda e, o=o, blk=blk: e.dma_start(out=FA[o][blk * 128:(blk + 1) * 128, :], in_=ao[o][:]), reads=[tg + f"ao{o}"], writes=[tg + "FA"])
                    P.dma("sp", lambda e, o=o, blk=blk: e.dma_start(out=FD[o][blk * 128:(blk + 1) * 128, :], in_=do[o][:]), reads=[tg + f"do{o}"], writes=[tg + "FD"])
        P.barrier()
    with ExitStack() as es:
        X = sbt(es, nc, tg + "X", [128, nch, 512], BF16)
        X2 = sbt(es, nc, tg + "X2", [128, nch, 512], BF16)
        cm = [sbt(es, nc, tg + f"cm{i}", [128, nch, 128], BF16) for i in range(2)]
        smm = [sbt(es, nc, tg + f"sm{i}", [128, nch, 128], BF16) for i in range(2)]
        ho = [sbt(es, nc, tg + f"ho{i}", [128, 512], F32) for i in range(4)]
        for o in range(2):
            for ch in range(nch):
                P.dma("sp", lambda e, o=o, ch=ch: e.dma_start(out=X[:, ch, :], in_=FA[o][ch * 128:(ch + 1) * 128, :]), reads=[tg + "FA"], writes=[tg + "X"])
                P.dma("sp" if Prog.DBG & 4 else "act", lambda e, o=o, ch=ch: e.dma_start(out=X2[:, ch, :], in_=FD[o][ch * 128:(ch + 1) * 128, :]), reads=[tg + "FD"], writes=[tg + "X2"])
            for ft in range(nch):
                i = ft % 2
                P.dma("sp", lambda e, i=i, ft=ft: e.dma_start(out=cm[i][:], in_=Cf[ft]), writes=[tg + f"cm{i}"])
                P.dma("sp" if Prog.DBG & 4 else "act", lambda e, i=i, ft=ft: e.dma_start(out=smm[i][:], in_=Sf[ft]), writes=[tg + f"sm{i}"])
                pr, pi_ = PS[2 * i], PS[2 * i + 1]
                for ch in range(nch):
                    P.op("pe", lambda e, pr=pr, i=i, ch=ch: e.matmul(pr[:, :], lhsT=cm[i][:, ch, :], rhs=X[:, ch, :], start=(ch == 0), stop=(ch == nch - 1)), reads=[tg + f"cm{i}", tg + "X"], writes=[f"ps{2 * i}"])
                for ch in range(nch):
                    P.op("pe", lambda e, pi_=pi_, i=i, ch=ch: e.matmul(pi_[:, :], lhsT=smm[i][:, ch, :], rhs=X2[:, ch, :], start=(ch == 0), stop=(ch == nch - 1)), reads=[tg + f"sm{i}", tg + "X2"], writes=[f"ps{2 * i + 1}"])
                if Prog.DBG & 1:
                    P.barrier()
                P.op("dve", lambda e, pr=pr, i=i: e.tensor_copy(out=ho[2 * i][:], in_=pr[:, :]), reads=[f"ps{2 * i}"], writes=[tg + f"ho{2 * i}"])
                P.op("dve", lambda e, pi_=pi_, i=i: e.tensor_copy(out=ho[2 * i + 1][:], in_=pi_[:, :]), reads=[f"ps{2 * i + 1}"], writes=[tg + f"ho{2 * i + 1}"])
                if Prog.DBG & 2:
                    P.barrier()
                P.dma("sp", lambda e, i=i, o=o, ft=ft: e.dma_start(out=HR[o][ft * 128:(ft + 1) * 128, :], in_=ho[2 * i][:]), reads=[tg + f"ho{2 * i}"], writes=[tg + "HR"])
                P.dma("sp", lambda e, i=i, o=o, ft=ft: e.dma_start(out=HI[o][ft * 128:(ft + 1) * 128, :], in_=ho[2 * i + 1][:]), reads=[tg + f"ho{2 * i + 1}"], writes=[tg + "HI"])
        P.barrier()
    with ExitStack() as es:
        cX = sbt(es, nc, tg + "cX", [128, nch, 512], BF16)
        Yr = sbt(es, nc, tg + "Yr", [128, nch, 512], BF16)
        Yi = sbt(es, nc, tg + "Yi", [128, nch, 512], BF16)
        ccm = [sbt(es, nc, tg + f"ccm{i}", [128, nch, 128], BF16) for i in range(2)]
        csm = [sbt(es, nc, tg + f"csm{i}", [128, nch, 128], BF16) for i in range(2)]
        hr = [sbt(es, nc, tg + f"hr{i}", [128, 512], F32) for i in range(2)]
        hi = [sbt(es, nc, tg + f"hi{i}", [128, 512], F32) for i in range(2)]
        tt = [sbt(es, nc, tg + f"t{i}", [128, 512], F32) for i in range(4)]
        ux = [sbt(es, nc, tg + f"ux{i}", [128, 1536], F32) for i in range(2)]
        bb = [sbt(es, nc, tg + f"bb{o}", [128, 512], F32) for o in range(2)]
        yo = sbt(es, nc, tg + "yo", [128, 512], F32)
        yst = [sbt(es, nc, tg + f"yst{i}", [128, 4, 128], BF16) for i in range(2)]
        for o in range(2):
            P.dma("sp", lambda e, o=o: e.dma_start(out=bb[o][:], in_=biasd[l, o:o + 1, :].partition_broadcast(128)), writes=[tg + f"bb{o}"])
        for ch in range(nch):
            P.dma("pool", lambda e, ch=ch: e.dma_start(out=cX[:, ch, :], in_=UHY[row0 + ch * 128:row0 + (ch + 1) * 128, 1024:1536]), reads=[f"UHY{c_}" for c_ in range(8, 12)], writes=[tg + f"cX{ch}"])
        for o in range(2):
            for ft in range(nch):
                i = ft % 2
                P.dma("sp", lambda e, i=i, ft=ft: e.dma_start(out=ccm[i][:], in_=Cf[ft]), writes=[tg + f"ccm{i}"])
                P.dma("act", lambda e, i=i, ft=ft: e.dma_start(out=csm[i][:], in_=Sf[ft]), writes=[tg + f"csm{i}"])
                P.dma("sp", lambda e, i=i, o=o, ft=ft: e.dma_start(out=hr[i][:], in_=HR[o][ft * 128:(ft + 1) * 128, :]), reads=[tg + "HR"], writes=[tg + f"hr{i}"])
                P.dma("act", lambda e, i=i, o=o, ft=ft: e.dma_start(out=hi[i][:], in_=HI[o][ft * 128:(ft + 1) * 128, :]), reads=[tg + "HI"], writes=[tg + f"hi{i}"])
                pr, pi_ = PS[2 * i], PS[2 * i + 1]
                for ch in range(nch):
                    P.op("pe", lambda e, pr=pr, i=i, ch=ch: e.matmul(pr[:, :], lhsT=ccm[i][:, ch, :], rhs=cX[:, ch, :], start=(ch == 0), stop=(ch == nch - 1)), reads=[tg + f"ccm{i}", tg + f"cX{ch}"], writes=[f"ps{2 * i}"])
                for ch in range(nch):
                    P.op("pe", lambda e, pi_=pi_, i=i, ch=ch: e.matmul(pi_[:, :], lhsT=csm[i][:, ch, :], rhs=cX[:, ch, :], start=(ch == 0), stop=(ch == nch - 1)), reads=[tg + f"csm{i}", tg + f"cX{ch}"], writes=[f"ps{2 * i + 1}"])
                P.op("dve", lambda e, pr=pr, i=i: e.tensor_tensor(out=tt[0][:], in0=pr[:, :], in1=hr[i][:], op=ALU.mult), reads=[f"ps{2 * i}", tg + f"hr{i}"], writes=[tg + "t0"])
                P.op("dve", lambda e, pi_=pi_, i=i: e.tensor_tensor(out=tt[1][:], in0=pi_[:, :], in1=hi[i][:], op=ALU.mult), reads=[f"ps{2 * i + 1}", tg + f"hi{i}"], writes=[tg + "t1"])
                P.op("dve", lambda e, pr=pr, i=i: e.tensor_tensor(out=tt[2][:], in0=pr[:, :], in1=hi[i][:], op=ALU.mult), reads=[f"ps{2 * i}", tg + f"hi{i}"], writes=[tg + "t2"])
                P.op("dve", lambda e, pi_=pi_, i=i: e.tensor_tensor(out=tt[3][:], in0=pi_[:, :], in1=hr[i][:], op=ALU.mult), reads=[f"ps{2 * i + 1}", tg + f"hr{i}"], writes=[tg + "t3"])
                P.op("pool", lambda e, ft=ft: e.tensor_tensor(out=Yr[:, ft, :], in0=tt[0][:], in1=tt[1][:], op=ALU.subtract), reads=[tg + "t0", tg + "t1"], writes=[tg + f"Yr{ft}"])
                P.op("pool", lambda e, ft=ft: e.tensor_tensor(out=Yi[:, ft, :], in0=tt[2][:], in1=tt[3][:], op=ALU.add), reads=[tg + "t2", tg + "t3"], writes=[tg + f"Yi{ft}"])
            for tb in range(nch):
                i = tb % 2
                P.dma("sp", lambda e, i=i, tb=tb: e.dma_start(out=ccm[i][:], in_=Ci[tb]), writes=[tg + f"ccm{i}"])
                P.dma("act", lambda e, i=i, tb=tb: e.dma_start(out=csm[i][:], in_=Si[tb]), writes=[tg + f"csm{i}"])
                P.dma("sp", lambda e, i=i, tb=tb: e.dma_start(out=ux[i][:], in_=UHY[row0 + tb * 128:row0 + (tb + 1) * 128, :]), reads=[f"UHY{c_}" for c_ in range(12)], writes=[tg + f"ux{i}"])
                ps = PS[4 + i]
                pk = f"ps{4 + i}"
                for ch in range(nch):
                    P.op("pe", lambda e, ps=ps, i=i, ch=ch: e.matmul(ps[:, :], lhsT=ccm[i][:, ch, :], rhs=Yr[:, ch, :], start=(ch == 0), stop=False), reads=[tg + f"ccm{i}", tg + f"Yr{ch}"], writes=[pk])
                for ch in range(nch):
                    P.op("pe", lambda e, ps=ps, i=i, ch=ch: e.matmul(ps[:, :], lhsT=csm[i][:, ch, :], rhs=Yi[:, ch, :], start=False, stop=(ch == nch - 1)), reads=[tg + f"csm{i}", tg + f"Yi{ch}"], writes=[pk])
                if o == 0:
                    P.op("pool", lambda e, i=i: e.tensor_tensor(out=tt[0][:], in0=ux[i][:, 1024:1536], in1=bb[0][:], op=ALU.mult), reads=[tg + f"ux{i}", tg + "bb0"], writes=[tg + "t0"])
                    P.op("dve", lambda e, ps=ps: e.tensor_tensor(out=tt[0][:], in0=ps[:, :], in1=tt[0][:], op=ALU.add), reads=[pk, tg + "t0"], writes=[tg + "t0"])
                    P.op("pool", lambda e, i=i, tb=tb: e.tensor_tensor(out=cX[:, tb, :], in0=tt[0][:], in1=ux[i][:, 0:512], op=ALU.mult), reads=[tg + "t0", tg + f"ux{i}"] + [tg + f"Yr{c_}" for c_ in range(nch)], writes=[tg + f"cX{tb}"])
                else:
                    P.op("pool", lambda e, tb=tb: e.tensor_tensor(out=tt[1][:], in0=cX[:, tb, :], in1=bb[1][:], op=ALU.mult), reads=[tg + f"cX{tb}", tg + "bb1"], writes=[tg + "t1"])
                    P.op("dve", lambda e, ps=ps: e.tensor_tensor(out=tt[1][:], in0=ps[:, :], in1=tt[1][:], op=ALU.add), reads=[pk, tg + "t1"], writes=[tg + "t1"])
                    P.op("pool", lambda e, i=i: e.tensor_tensor(out=yo[:], in0=tt[1][:], in1=ux[i][:, 512:1024], op=ALU.mult), reads=[tg + "t1", tg + f"ux{i}"], writes=[tg + "yo"])
                    pt = PS[6 + i]
                    for k in range(4):
                        P.op("pe", lambda e, pt=pt, k=k: e.transpose(pt[:, k * 128:(k + 1) * 128], yo[:, k * 128:(k + 1) * 128], K["ident"][:]), reads=[tg + "yo", "ident"], writes=[f"ps{6 + i}"])
                    P.op("act", lambda e, pt=pt, i=i: e.activation(out=yst[i][:], in_=pt[:, :].rearrange("p (k t) -> p k t", t=128), func=AF.Copy), reads=[f"ps{6 + i}"], writes=[tg + f"yst{i}"])
                    P.dma("sp", lambda e, i=i, tb=tb: e.dma_start(out=YHY.rearrange("(k p) t -> p k t", p=128)[:, :, row0 + tb * 128:row0 + (tb + 1) * 128], in_=yst[i][:]), reads=[tg + f"yst{i}"], writes=["Y0"])
        P.barrier()


def stage_zero_yrw(C, K, PS):
    nc, P = C.nc, C.P
    YRW = C.dram("YRW", [512, T], BF16)
    with ExitStack() as es:
        z = sbt(es, nc, "zrw", [128, T], BF16)
        P.op("pool", lambda e: e.memset(z[:], 0.0), writes=["zrw"])
        for k in range(4):
            P.dma("sp", lambda e, k=k: e.dma_start(out=YRW[k * 128:(k + 1) * 128, :], in_=z[:]), reads=["zrw"], writes=["Y1"])
        P.barrier()


def full_stages():
    st = []
    for l in range(L):
        need_ctx = l < L - 1
        st.append(lambda C, K, PS, l=l: stage_ada(C, K, PS, l))
        if l == 0:
            st.append(stage_resid_init)
        st.append(lambda C, K, PS, l=l, nctx=need_ctx: stage_norm1_inproj(C, K, PS, l, nctx))
        st.append(lambda C, K, PS, l=l: stage_hyena(C, K, PS, l, TL, TC))
        if need_ctx:
            st.append(lambda C, K, PS, l=l: stage_hyena(C, K, PS, l, TC, 0))
        st.append(lambda C, K, PS, l=l, nctx=need_ctx: stage_s5(C, K, PS, l, nctx))
        st.append(lambda C, K, PS, l=l: stage_rw_prep(C, K, PS, l))
        st.append(lambda C, K, PS, l=l, nctx=need_ctx: stage_rw_scan(C, K, PS, l, nctx))
        st.append(lambda C, K, PS, l=l, nctx=need_ctx: stage_rw_out(C, K, PS, l, nctx))
        st.append(lambda C, K, PS, l=l, nctx=need_ctx: stage_merge(C, K, PS, l, nctx))
        st.append(lambda C, K, PS, l=l, nctx=need_ctx: stage_moe(C, K, PS, l, nctx, l == L - 1))
    return st


def kernel(**inputs):
    inp = {k: np.asarray(v) for k, v in inputs.items()}
    B = inp["x"].shape[0]
    nc, C = build_program(full_stages())
    S = prep_shared(inp)
    in_maps = []
    for b in range(B):
        allin = {**S, **prep_core(inp, b)}
        in_maps.append({k: allin[k] for k in C.ext_in})
    res = run_bass_kernel_spmd(nc, in_maps, core_ids=list(range(B)))
    out = np.stack([np.ascontiguousarray(np.asarray(r["outT"]).T) for r in res.results], axis=0)
    return out.astype(np.float32)


NS = 32
SC = 256


def rw_consts(C, K, es):
    nc, P = C.nc, C.P
    for nm, shp in (("Jm", [128, 128]), ("BO", [128, 128]), ("mask16", [16, 512]), ("maskhh", [128, 2]), ("rwc", [128, 4])):
        K[nm] = sbt(es, nc, nm, shp, F32)
        src = C.inp(nm + "_in", shp)
        P.dma("sp", lambda e, nm=nm, src=src: e.dma_start(out=K[nm][:], in_=src[:, :]), writes=[nm])


def stage_rw_prep(C, K, PS, l):
    nc, P = C.nc, C.P
    URW = C.dram("URW", [1792, T])
    Uv = URW.rearrange("(j p) t -> p j t", p=128)
    names = ("AKK", "BD0", "BD1", "KD0", "KD1", "WD0", "WD1", "GG", "BON")
    DR = {n: C.dram("rw" + n, [512, T]).rearrange("(j p) t -> p j t", p=128) for n in names}
    vec = C.inp("rw_vec_t", [L, 128, 4, 5])
    w0a0 = C.inp("rw_w0a0_t", [L, 128, 4, 4])
    wupd = C.inp("rw_w_up", [L, 2, 64, 512])
    aupd = C.inp("rw_a_up", [L, 2, 64, 512])
    gupd = C.inp("rw_g_up", [L, 128, 512])
    with ExitStack() as es:
        vt = sbt(es, nc, "rwvec", [128, 4, 5], F32)
        wa = sbt(es, nc, "rww0a0", [128, 4, 4], F32)
        nw0 = sbt(es, nc, "rwnw0", [128, 4, 2], F32)
        wup = sbt(es, nc, "rwwup", [64, 2, 512], F32)
        aup = sbt(es, nc, "rwaup", [128, 2, 512], F32)
        gup = sbt(es, nc, "rwgup", [128, 512], F32)
        P.dma("sp", lambda e: e.dma_start(out=vt[:], in_=vec[l]), writes=["rwvec"])
        P.dma("sp", lambda e: e.dma_start(out=wa[:], in_=w0a0[l]), writes=["rww0a0"])
        for d in range(2):
            P.dma("sp", lambda e, d=d: e.dma_start(out=wup[:, d, :], in_=wupd[l, d]), writes=["rwwup"])
            P.dma("sp", lambda e, d=d: e.dma_start(out=aup[64:128, d, :], in_=aupd[l, d]), writes=["rwaup"])
        P.dma("sp", lambda e: e.dma_start(out=gup[:], in_=gupd[l]), writes=["rwgup"])
        P.op("dve", lambda e: e.tensor_scalar(out=nw0[:], in0=wa[:, :, 0:2], scalar1=-1.0, scalar2=None, op0=ALU.mult), reads=["rww0a0"], writes=["rwnw0"])
        xwa = sbt(es, nc, "rwxwa", [128, 512], F32)
        txw = sbt(es, nc, "rwtxw", [64, 512], F32)
        xg = sbt(es, nc, "rwxg", [128, 512], F32)
        rr = sbt(es, nc, "rwr", [128, 512], F32)
        kk_ = sbt(es, nc, "rwk", [128, 512], F32)
        kk0 = sbt(es, nc, "rwkk0", [128, 512], F32)
        sq = sbt(es, nc, "rwsq", [128, 512], F32)
        kkn = sbt(es, nc, "rwkkn", [128, 512], F32)
        akk = sbt(es, nc, "rwakk", [128, 512], F32)
        gg = sbt(es, nc, "rwgg", [128, 512], F32)
        e1 = sbt(es, nc, "rwe1", [128, 512], F32)
        dec = [sbt(es, nc, f"rwdec{d}", [128, 512], F32) for d in range(2)]
        aa = sbt(es, nc, "rwaa", [128, 512], F32)
        tt_ = sbt(es, nc, "rwtt", [128, 512], F32)
        kd = [sbt(es, nc, f"rwkd{d}", [128, 512], F32) for d in range(2)]
        bd = [sbt(es, nc, f"rwbd{d}", [128, 512], F32) for d in range(2)]
        bon = sbt(es, nc, "rwbon", [128, 512], F32)
        for ci, (c0, ln) in enumerate(CHUNKS):
            P.dma("sp", lambda e, c0=c0, ln=ln: e.dma_start(out=xwa[:, :ln], in_=Uv[:, 12, c0:c0 + ln]), reads=["URW12"], writes=["rwxwa"])
            P.dma("sp", lambda e, c0=c0, ln=ln: e.dma_start(out=xg[:, :ln], in_=Uv[:, 13, c0:c0 + ln]), reads=["URW13"], writes=["rwxg"])
            P.op("act", lambda e, ln=ln: e.activation(out=txw[:, :ln], in_=xwa[0:64, :ln], func=AF.Tanh), reads=["rwxwa"], writes=["rwtxw"])
            P.op("act", lambda e, ln=ln: e.activation(out=xg[:, :ln], in_=xg[:, :ln], func=AF.Sigmoid), reads=["rwxg"], writes=["rwxg"])
            for j in range(4):
                js = slice(j * 128, (j + 1) * 128)
                P.dma("sp", lambda e, j=j, c0=c0, ln=ln: e.dma_start(out=rr[:, :ln], in_=Uv[:, j, c0:c0 + ln]), reads=[f"URW{j}"], writes=["rwr"])
                P.dma("act", lambda e, j=j, c0=c0, ln=ln: e.dma_start(out=kk_[:, :ln], in_=Uv[:, 4 + j, c0:c0 + ln]), reads=[f"URW{4 + j}"], writes=["rwk"])
                P.op("pe", lambda e, js=js, ln=ln: e.matmul(PS[0][:, :ln], lhsT=gup[:, js], rhs=xg[:, :ln], start=True, stop=True), reads=["rwgup", "rwxg"], writes=["ps0"])
                P.op("act", lambda e, ln=ln: e.activation(out=gg[:, :ln], in_=PS[0][:, :ln], func=AF.Copy), reads=["ps0"], writes=["rwgg"])
                P.dma("sp", lambda e, j=j, c0=c0, ln=ln: e.dma_start(out=DR["GG"][:, j, c0:c0 + ln], in_=gg[:, :ln]), reads=["rwgg"], writes=["rwGG"])
                P.op("dve", lambda e, j=j, ln=ln: e.tensor_scalar(out=kk0[:, :ln], in0=kk_[:, :ln], scalar1=vt[:, j, 0:1], scalar2=None, op0=ALU.mult), reads=["rwk", "rwvec"], writes=["rwkk0"])
                P.op("act", lambda e, ln=ln: e.activation(out=sq[:, :ln], in_=kk0[:, :ln], func=AF.Square), reads=["rwkk0"], writes=["rwsq"])
                P.op("pe", lambda e, ln=ln: e.matmul(PS[1][:, :ln], lhsT=K["BO"][:], rhs=sq[:, :ln], start=True, stop=True), reads=["BO", "rwsq"], writes=["ps1"])
                P.op("dve", lambda e, ln=ln: e.tensor_scalar(out=sq[:, :ln], in0=PS[1][:, :ln], scalar1=1e-24, scalar2=None, op0=ALU.max), reads=["ps1"], writes=["rwsq"])
                P.op("act", lambda e, ln=ln: e.activation(out=sq[:, :ln], in_=sq[:, :ln], func=AF.Sqrt), reads=["rwsq"], writes=["rwsq"])
                P.op("dve", lambda e, ln=ln: e.reciprocal(out=sq[:, :ln], in_=sq[:, :ln]), reads=["rwsq"], writes=["rwsq"])
                P.op("dve", lambda e, ln=ln: e.tensor_tensor(out=kkn[:, :ln], in0=kk0[:, :ln], in1=sq[:, :ln], op=ALU.mult), reads=["rwkk0", "rwsq"], writes=["rwkkn"])
                P.op("pool", lambda e, ln=ln: e.tensor_scalar(out=akk[:, :ln], in0=kkn[:, :ln], scalar1=-1.0, scalar2=None, op0=ALU.mult), reads=["rwkkn"], writes=["rwakk"])
                P.dma("sp", lambda e, j=j, c0=c0, ln=ln: e.dma_start(out=DR["AKK"][:, j, c0:c0 + ln], in_=akk[:, :ln]), reads=["rwakk"], writes=["rwAKK"])
                for d in range(2):
                    P.op("pe", lambda e, d=d, js=js, ln=ln: e.matmul(PS[2][:, :ln], lhsT=wup[:, d, js], rhs=txw[:, :ln], start=True, stop=True), reads=["rwwup", "rwtxw"], writes=["ps2"])
                    P.op("act", lambda e, d=d, j=j, ln=ln: e.activation(out=e1[:, :ln], in_=PS[2][:, :ln], func=AF.Exp, bias=nw0[:, j, d:d + 1], scale=-1.0), reads=["ps2", "rwnw0"], writes=["rwe1"])
                    P.op("act", lambda e, ln=ln: e.activation(out=e1[:, :ln], in_=e1[:, :ln], func=AF.Ln, bias=K["rwc"][:, 0:1], scale=1.0), reads=["rwe1", "rwc"], writes=["rwe1"])
                    P.op("act", lambda e, ln=ln: e.activation(out=e1[:, :ln], in_=e1[:, :ln], func=AF.Exp, bias=K["rwc"][:, 1:2], scale=-1.0), reads=["rwe1", "rwc"], writes=["rwe1"])
                    P.op("act", lambda e, d=d, ln=ln: e.activation(out=dec[d][:, :ln], in_=e1[:, :ln], func=AF.Exp, scale=-1.0), reads=["rwe1"], writes=[f"rwdec{d}"])
                    P.dma("sp", lambda e, d=d, j=j, c0=c0, ln=ln: e.dma_start(out=DR[f"WD{d}"][:, j, c0:c0 + ln], in_=dec[d][:, :ln]), reads=[f"rwdec{d}"], writes=[f"rwWD{d}"])
                    P.op("pe", lambda e, d=d, js=js, ln=ln: e.matmul(PS[3][:, :ln], lhsT=aup[64:128, d, js], rhs=xwa[64:128, :ln], start=True, stop=True), reads=["rwaup", "rwxwa"], writes=["ps3"])
                    P.op("act", lambda e, d=d, j=j, ln=ln: e.activation(out=aa[:, :ln], in_=PS[3][:, :ln], func=AF.Sigmoid, bias=wa[:, j, 2 + d:3 + d], scale=1.0), reads=["ps3", "rww0a0"], writes=["rwaa"])
                    P.op("dve", lambda e, j=j, ln=ln: e.tensor_scalar(out=tt_[:, :ln], in0=aa[:, :ln], scalar1=-1.0, scalar2=vt[:, j, 1:2], op0=ALU.add, op1=ALU.mult), reads=["rwaa", "rwvec"], writes=["rwtt"])
                    P.op("dve", lambda e, d=d, ln=ln: e.scalar_tensor_tensor(out=kd[d][:, :ln], in0=tt_[:, :ln], scalar=1.0, in1=kk_[:, :ln], op0=ALU.add, op1=ALU.mult), reads=["rwtt", "rwk"], writes=[f"rwkd{d}"])
                    P.dma("sp", lambda e, d=d, j=j, c0=c0, ln=ln: e.dma_start(out=DR[f"KD{d}"][:, j, c0:c0 + ln], in_=kd[d][:, :ln]), reads=[f"rwkd{d}"], writes=[f"rwKD{d}"])
                    P.op("pool", lambda e, d=d, ln=ln: e.tensor_tensor(out=bd[d][:, :ln], in0=kkn[:, :ln], in1=aa[:, :ln], op=ALU.mult), reads=["rwkkn", "rwaa"], writes=[f"rwbd{d}"])
                    P.dma("act", lambda e, d=d, j=j, c0=c0, ln=ln: e.dma_start(out=DR[f"BD{d}"][:, j, c0:c0 + ln], in_=bd[d][:, :ln]), reads=[f"rwbd{d}"], writes=[f"rwBD{d}"])
                P.op("pool", lambda e, ln=ln: e.tensor_tensor(out=tt_[:, :ln], in0=kd[0][:, :ln], in1=kd[1][:, :ln], op=ALU.add), reads=["rwkd0", "rwkd1", "rwtt"], writes=["rwtt"])
                P.op("dve", lambda e, j=j, ln=ln: e.scalar_tensor_tensor(out=tt_[:, :ln], in0=rr[:, :ln], scalar=vt[:, j, 2:3], in1=tt_[:, :ln], op0=ALU.mult, op1=ALU.mult), reads=["rwr", "rwvec", "rwtt"], writes=["rwtt"])
                P.op("pe", lambda e, ln=ln: e.matmul(PS[4][:, :ln], lhsT=K["BO"][:], rhs=tt_[:, :ln], start=True, stop=True), reads=["BO", "rwtt"], writes=["ps4"])
                P.op("act", lambda e, ln=ln: e.activation(out=bon[:, :ln], in_=PS[4][:, :ln], func=AF.Copy), reads=["ps4"], writes=["rwbon"])
                P.dma("sp", lambda e, j=j, c0=c0, ln=ln: e.dma_start(out=DR["BON"][:, j, c0:c0 + ln], in_=bon[:, :ln]), reads=["rwbon"], writes=["rwBON"])
        P.barrier()


def stage_rw_scan(C, K, PS, l, need_ctx):
    nc, P = C.nc, C.P
    URW = C.dram("URW", [1792, T])
    Uv = URW.rearrange("(j p) t -> p j t", p=128)
    DR = {n: C.dram("rw" + n, [512, T]).rearrange("(j p) t -> p j t", p=128) for n in ("AKK", "BD0", "BD1", "KD0", "KD1", "WD0", "WD1")}
    VT = [C.dram("VTM", [T, 512]), C.dram("VTMR", [T, 512])]
    YD = [C.dram("rwYD0", [T, 512]), C.dram("rwYDR", [T, 512])]
    with ExitStack() as es:
        sct = {(d, nm): sbt(es, nc, f"sc{nm}{d}", [128, 4, SC], F32) for d in range(2) for nm in ("A", "R", "B", "K", "W")}
        Abd = sbt(es, nc, "Abd", [128, NS, 16], F32)
        Rbd = sbt(es, nc, "Rbd", [128, NS, 16], F32)
        BKbd = sbt(es, nc, "BKbd", [128, NS, 32], F32)
        Wt = sbt(es, nc, "Wt", [128, NS, 8], F32)
        LT = sbt(es, nc, "LT", [32, NS, 128], F32)
        RH = sbt(es, nc, "RH", [32, NS, 512], F32)
        MA = sbt(es, nc, "MA", [128, 512], F32)
        MB = sbt(es, nc, "MB", [128, 512], F32)
        ym = [sbt(es, nc, f"ym{i}", [16, 512], F32) for i in range(2)]
        Ys = sbt(es, nc, "Ysteps", [16, NS, 64], F32)
        P.op("pool", lambda e: e.memset(RH[:], 0.0), writes=["RH"])
        P.op("pool", lambda e: e.memset(MA[:], 0.0), writes=["MA"])
        srcs = {"A": lambda d: DR["AKK"], "R": lambda d: Uv, "B": lambda d: DR[f"BD{d}"], "K": lambda d: DR[f"KD{d}"], "W": lambda d: DR[f"WD{d}"]}
        skeys = {"A": lambda d: "rwAKK", "R": lambda d: None, "B": lambda d: f"rwBD{d}", "K": lambda d: f"rwKD{d}", "W": lambda d: f"rwWD{d}"}
        step = 0
        for s0sc in range(0, T, SC):
            n0 = [s0sc, (256 - s0sc - SC) if s0sc < 256 else (4608 - s0sc - SC)]
            for d in range(2):
                for nm in ("A", "R", "B", "K", "W"):
                    src = srcs[nm](d)
                    rk = [skeys[nm](d)] if skeys[nm](d) else [f"URW{j}" for j in range(4)]
                    P.dma("sp" if d == 0 else "act", lambda e, d=d, nm=nm, src=src, nd=n0[d]: e.dma_start(out=sct[(d, nm)][:], in_=src[:, 0:4, nd:nd + SC]),
                          reads=rk, writes=[f"sc{nm}{d}"])
            for i0 in range(0, SC, NS):
                s0 = s0sc + i0
                need_y = need_ctx or s0 >= 256

                def view(d, nm, j):
                    t_ = sct[(d, nm)]
                    if d == 0:
                        return t_[:, j, i0:i0 + NS]
                    return t_[:, j, SC - i0 - NS:SC - i0][:, ::-1]
                for d in range(2):
                    for j in range(4):
                        for hh in range(2):
                            col = d * 8 + j * 2 + hh
                            mk = K["maskhh"][:, hh:hh + 1]
                            P.op("pool", lambda e, col=col, mk=mk, vw=view(d, "A", j): e.tensor_scalar(out=Abd[:, :, col], in0=vw, scalar1=mk, scalar2=None, op0=ALU.mult), reads=[f"scA{d}", "maskhh"], writes=["Abd"])
                            P.op("pool", lambda e, col=col, mk=mk, vw=view(d, "R", j): e.tensor_scalar(out=Rbd[:, :, col], in0=vw, scalar1=mk, scalar2=None, op0=ALU.mult), reads=[f"scR{d}", "maskhh"], writes=["Rbd"])
                            P.op("pool", lambda e, col=col, mk=mk, vw=view(d, "B", j): e.tensor_scalar(out=BKbd[:, :, col], in0=vw, scalar1=mk, scalar2=None, op0=ALU.mult), reads=[f"scB{d}", "maskhh"], writes=["BKbd"])
                            P.op("pool", lambda e, col=col, mk=mk, vw=view(d, "K", j): e.tensor_scalar(out=BKbd[:, :, 16 + col], in0=vw, scalar1=mk, scalar2=None, op0=ALU.mult), reads=[f"scK{d}", "maskhh"], writes=["BKbd"])
                        P.op("pool", lambda e, d=d, j=j, vw=view(d, "W", j): e.tensor_copy(out=Wt[:, :, d * 4 + j], in_=vw), reads=[f"scW{d}"], writes=["Wt"])
                for d in range(2):
                    r0 = s0 if d == 0 else ((4096 + s0) if s0 < 256 else (s0 - 256))
                    for j in range(4):
                        P.dma("sp" if d == 0 else "act", lambda e, d=d, j=j, r0=r0: e.dma_start(
                            out=RH[16 + d * 8 + 2 * j:16 + d * 8 + 2 * j + 2, :, d * 256 + j * 64:d * 256 + (j + 1) * 64],
                            in_=VT[d][r0:r0 + NS, 2 * j * 64:(2 * j + 2) * 64].rearrange("t (h v) -> h t v", h=2)),
                            reads=[f"VTM{j}" if d == 0 else f"VTMR{j}"], writes=["RHv"])
                for g in range(NS // 4):
                    pt = PS[g % 2]
                    for q in range(4):
                        s = g * 4 + q
                        P.op("pe", lambda e, pt=pt, q=q, s=s: e.transpose(pt[:32, q * 128:(q + 1) * 128], BKbd[:, s, :], K["ident"][:]), reads=["BKbd", "ident"], writes=[f"ps{g % 2}"])
                    P.op("act", lambda e, pt=pt, g=g: e.activation(out=LT[:, g * 4:(g + 1) * 4, :], in_=pt[:32, :].rearrange("p (q c) -> p q c", c=128), func=AF.Copy), reads=[f"ps{g % 2}"], writes=["LT"])
                for s in range(NS):
                    pu, pm, py = PS[2 + step % 2], PS[4 + step % 2], PS[6 + step % 2]
                    ku, km, ky = f"ps{2 + step % 2}", f"ps{4 + step % 2}", f"ps{6 + step % 2}"
                    P.op("pe", lambda e, pu=pu, s=s: e.matmul(pu[:16, :], lhsT=Abd[:, s, :], rhs=MA[:, :], start=True, stop=True), reads=["Abd", "MA"], writes=[ku])
                    P.op("pool", lambda e, s=s: e.tensor_tensor(out=MB[:, :].rearrange("p (g v) -> p g v", v=64), in0=MA[:, :].rearrange("p (g v) -> p g v", v=64),
                                                                  in1=Wt[:, s, :].unsqueeze(2).broadcast_to([128, 8, 64]), op=ALU.mult), reads=["MA", "Wt"], writes=["MB"])
                    P.op("dve", lambda e, pu=pu, s=s: e.tensor_tensor(out=RH[0:16, s, :], in0=pu[:16, :], in1=K["mask16"][:], op=ALU.mult), reads=[ku, "mask16"], writes=["RHu"])
                    P.op("pe", lambda e, pm=pm, s=s: e.matmul(pm[:, :], lhsT=LT[:, s, :], rhs=RH[:, s, :], start=True, stop=True), reads=["LT", "RHu", "RHv", "RH"], writes=[km])
                    P.op("dve", lambda e, pm=pm: e.tensor_tensor(out=MA[:, :], in0=MB[:, :], in1=pm[:, :], op=ALU.add), reads=["MB", km], writes=["MA"])
                    if step == 0 and "dbgMA" in C.dump:
                        for nm, src_, shp in (("dbgMA", MA[:, :], [128, 512]), ("dbgLT", LT[:, 0, :], [32, 128]), ("dbgRH", RH[:, 0, :], [32, 512]), ("dbgR", Rbd[:, 0, :], [128, 16]), ("dbgBK", BKbd[:, 0, :], [128, 32])):
                            dd_ = C.dram(nm, shp)
                            P.dma("sp", lambda e, dd_=dd_, src_=src_: e.dma_start(out=dd_[:, :], in_=src_), reads=["MA", "LT", "RHu", "RHv", "Rbd", "BKbd"], writes=[nm])
                    if need_y:
                        yi = step % 2
                        P.op("pe", lambda e, py=py, s=s: e.matmul(py[:16, :], lhsT=Rbd[:, s, :], rhs=MA[:, :], start=True, stop=True), reads=["Rbd", "MA"], writes=[ky])
                        P.op("dve", lambda e, py=py, yi=yi: e.tensor_tensor(out=ym[yi][:], in0=py[:16, :], in1=K["mask16"][:], op=ALU.mult), reads=[ky, "mask16"], writes=[f"ym{yi}"])
                        P.op("pool", lambda e, yi=yi: e.tensor_tensor(out=ym[yi][:, 0:256], in0=ym[yi][:, 0:256], in1=ym[yi][:, 256:512], op=ALU.add), reads=[f"ym{yi}"], writes=[f"ym{yi}"])
                        P.op("pool", lambda e, yi=yi: e.tensor_tensor(out=ym[yi][:, 0:128], in0=ym[yi][:, 0:128], in1=ym[yi][:, 128:256], op=ALU.add), reads=[f"ym{yi}"], writes=[f"ym{yi}"])
                        P.op("pool", lambda e, yi=yi, s=s: e.tensor_tensor(out=Ys[:, s, :], in0=ym[yi][:, 0:64], in1=ym[yi][:, 64:128], op=ALU.add), reads=[f"ym{yi}"], writes=["Ysteps"])
                    step += 1
                if need_y:
                    for d in range(2):
                        P.dma("sp", lambda e, d=d, s0=s0: e.dma_start(out=YD[d][s0:s0 + NS, :].rearrange("t (h v) -> h t v", h=8), in_=Ys[d * 8:(d + 1) * 8, :, :]), reads=["Ysteps"], writes=[f"rwYD{d}"])
        P.barrier()


def stage_rw_out(C, K, PS, l, need_ctx):
    nc, P = C.nc, C.P
    URW = C.dram("URW", [1792, T])
    Uv = URW.rearrange("(j p) t -> p j t", p=128)
    DR = {n: C.dram("rw" + n, [512, T]).rearrange("(j p) t -> p j t", p=128) for n in ("GG", "BON")}
    YD = [C.dram("rwYD0", [T, 512]), C.dram("rwYDR", [T, 512])]
    YRW = C.dram("YRW", [512, T], BF16).rearrange("(j p) t -> p j t", p=128)
    vec = C.inp("rw_vec_t", [L, 128, 4, 5])
    with ExitStack() as es:
        vt = sbt(es, nc, "rovec", [128, 4, 5], F32)
        P.dma("sp", lambda e: e.dma_start(out=vt[:], in_=vec[l]), writes=["rovec"])
        y0 = [sbt(es, nc, f"roy0_{i}", [128, 512], F32) for i in range(2)]
        y1 = [sbt(es, nc, f"roy1_{i}", [128, 512], F32) for i in range(2)]
        bon = sbt(es, nc, "robon", [128, 512], F32)
        vv = sbt(es, nc, "rov", [128, 512], F32)
        gg = sbt(es, nc, "rog", [128, 512], F32)
        yy = sbt(es, nc, "royy", [128, 512], F32)
        yc = sbt(es, nc, "royc", [128, 512], F32)
        sq = sbt(es, nc, "rosq", [128, 512], F32)
        rs = sbt(es, nc, "rors", [128, 512], F32)
        ob = [sbt(es, nc, f"roob{i}", [128, 512], BF16) for i in range(2)]
        for ci, (c0, ln) in enumerate(CHUNKS):
            if ci == 0 and not need_ctx:
                continue
            for bi in range(ln // 128):
                t0 = c0 + bi * 128
                sb = (255 - t0 - 127) if t0 < 256 else (4607 - t0 - 127)
                i = bi % 2
                P.dma("sp", lambda e, i=i, t0=t0: e.dma_start(out=y0[i][:], in_=YD[0][t0:t0 + 128, :]), reads=["rwYD0"], writes=[f"roy0_{i}"])
                P.dma("act", lambda e, i=i, sb=sb: e.dma_start(out=y1[i][:], in_=YD[1][sb:sb + 128, :]), reads=["rwYD1"], writes=[f"roy1_{i}"])
                for j in range(4):
                    js = slice(j * 128, (j + 1) * 128)
                    P.op("pe", lambda e, i=i, j=j, js=js, bi=bi: e.matmul(PS[j][:, bi * 128:(bi + 1) * 128], lhsT=y0[i][:, js], rhs=K["ident"][:], start=True, stop=False), reads=[f"roy0_{i}", "ident"], writes=[f"ps{j}"])
                    P.op("pe", lambda e, i=i, j=j, js=js, bi=bi: e.matmul(PS[j][:, bi * 128:(bi + 1) * 128], lhsT=y1[i][:, js], rhs=K["Jm"][:], start=False, stop=True), reads=[f"roy1_{i}", "Jm"], writes=[f"ps{j}"])
            for j in range(4):
                P.dma("sp", lambda e, j=j, c0=c0, ln=ln: e.dma_start(out=bon[:, :ln], in_=DR["BON"][:, j, c0:c0 + ln]), reads=["rwBON"], writes=["robon"])
                P.dma("act", lambda e, j=j, c0=c0, ln=ln: e.dma_start(out=vv[:, :ln], in_=Uv[:, 8 + j, c0:c0 + ln]), reads=[f"URW{8 + j}"], writes=["rov"])
                P.dma("sp", lambda e, j=j, c0=c0, ln=ln: e.dma_start(out=gg[:, :ln], in_=DR["GG"][:, j, c0:c0 + ln]), reads=["rwGG"], writes=["rog"])
                P.op("pool", lambda e, ln=ln: e.tensor_tensor(out=bon[:, :ln], in0=bon[:, :ln], in1=vv[:, :ln], op=ALU.mult), reads=["robon", "rov"], writes=["robon"])
                P.op("dve", lambda e, j=j, ln=ln: e.tensor_tensor(out=yy[:, :ln], in0=PS[j][:, :ln], in1=bon[:, :ln], op=ALU.add), reads=[f"ps{j}", "robon"], writes=["royy"])
                P.op("pe", lambda e, ln=ln: e.matmul(PS[4][:, :ln], lhsT=K["BO"][:], rhs=yy[:, :ln], start=True, stop=True), reads=["BO", "royy"], writes=["ps4"])
                P.op("dve", lambda e, ln=ln: e.scalar_tensor_tensor(out=yc[:, :ln], in0=PS[4][:, :ln], scalar=-1.0 / 64, in1=yy[:, :ln], op0=ALU.mult, op1=ALU.add), reads=["ps4", "royy"], writes=["royc"])
                P.op("act", lambda e, ln=ln: e.activation(out=sq[:, :ln], in_=yc[:, :ln], func=AF.Square), reads=["royc"], writes=["rosq"])
                P.op("pe", lambda e, ln=ln: e.matmul(PS[5][:, :ln], lhsT=K["BO"][:], rhs=sq[:, :ln], start=True, stop=True), reads=["BO", "rosq"], writes=["ps5"])
                P.op("act", lambda e, ln=ln: e.activation(out=rs[:, :ln], in_=PS[5][:, :ln], func=AF.Sqrt, scale=1.0 / 64, bias=K["rwc"][:, 2:3]), reads=["ps5", "rwc"], writes=["rors"])
                P.op("dve", lambda e, ln=ln: e.reciprocal(out=rs[:, :ln], in_=rs[:, :ln]), reads=["rors"], writes=["rors"])
                P.op("pool", lambda e, ln=ln: e.tensor_tensor(out=yc[:, :ln], in0=yc[:, :ln], in1=rs[:, :ln], op=ALU.mult), reads=["royc", "rors"], writes=["royc"])
                P.op("dve", lambda e, j=j, ln=ln: e.tensor_scalar(out=yc[:, :ln], in0=yc[:, :ln], scalar1=vt[:, j, 3:4], scalar2=vt[:, j, 4:5], op0=ALU.mult, op1=ALU.add), reads=["royc", "rovec"], writes=["royc"])
                P.op("pool", lambda e, j=j, ln=ln: e.tensor_tensor(out=ob[j % 2][:, :ln], in0=yc[:, :ln], in1=gg[:, :ln], op=ALU.mult), reads=["royc", "rog"], writes=[f"roob{j % 2}"])
                P.dma("sp", lambda e, j=j, c0=c0, ln=ln: e.dma_start(out=YRW[:, j, c0:c0 + ln], in_=ob[j % 2][:, :ln]), reads=[f"roob{j % 2}"], writes=["Y1"])
        P.barrier()
```
